# Optimizing a Trainium2 kernel written in Bass

```python
import math, functools
import jax, jax.numpy as jnp
from jax import lax
import numpy as np

D_MODEL = 1024
BATCH = 8
SEQ = 2048
DEPTH = 4

GRID_W = 64
CTX_LEN = 256

NA_HEADS = 8
NA_HEAD_DIM = 64
NA_WIN_H = 8
NA_WIN_W = 16
DN_HEADS = 4
DN_HEAD_DIM = 128
DN_CONV = 5
DN_CHUNK = 64
SWA_Q_HEADS = 16
SWA_KV_HEADS = 2
SWA_HEAD_DIM = 64
SWA_WINDOW = 128
SWA_BLOCK = 128
ROPE_THETA = 10000.0
N_EXPERTS = 32
TOP_K = 4
D_EXPERT = 1024
SWIGLU_LIMIT = 7.0
SWIGLU_ALPHA = 1.702

LN_EPS = 1e-5
RMS_EPS = 1e-6
NEG_INF = -1e30
DEEPNORM_ALPHA = (2 * DEPTH) ** 0.25
DEEPNORM_BETA = (8 * DEPTH) ** -0.25

N_EVEN = (DEPTH + 1) // 2
N_ODD = DEPTH // 2
NA_WIDTH = NA_HEADS * NA_HEAD_DIM
DN_WIDTH = DN_HEADS * DN_HEAD_DIM
AB_SPLITS = (NA_WIDTH, 2 * NA_WIDTH, 3 * NA_WIDTH, 3 * NA_WIDTH + 3 * DN_WIDTH, 3 * NA_WIDTH + 4 * DN_WIDTH)
AB_IN = 3 * NA_WIDTH + 4 * DN_WIDTH + 4 * DN_HEADS
SWA_Q_WIDTH = SWA_Q_HEADS * SWA_HEAD_DIM
SWA_KV_WIDTH = SWA_KV_HEADS * SWA_HEAD_DIM
C_IN = SWA_Q_WIDTH + 2 * SWA_KV_WIDTH
F32 = jnp.float32

kernel_name = "hybrid_na_deltanet_swa_moe_diffusion_trunk"


def layer_norm(x, g, b):
    xf = x.astype(F32)
    mu = jnp.mean(xf, -1, keepdims=True)
    var = jnp.mean(jnp.square(xf - mu), -1, keepdims=True)
    return ((xf - mu) * lax.rsqrt(var + LN_EPS)).astype(x.dtype) * g + b


def softmax_parts(*logits):
    m = functools.reduce(jnp.maximum, [jnp.max(l, axis=-1, keepdims=True) for l in logits])
    e = [jnp.exp(l - m) for l in logits]
    denom = functools.reduce(jnp.add, [jnp.sum(t, axis=-1, keepdims=True) for t in e])
    return e, denom


def axial_rope_tables(n_tokens, head_dim):
    t = jnp.arange(n_tokens)
    n_freq = head_dim // 4
    inv = ROPE_THETA ** (-jnp.arange(n_freq, dtype=F32) / n_freq)
    row = (t // GRID_W).astype(F32)[:, None]
    col = (t % GRID_W).astype(F32)[:, None]
    ang = jnp.concatenate([row * inv, col * inv], -1)
    return jnp.cos(ang), jnp.sin(ang)


def apply_axial_rope(x, cos, sin):
    half = x.shape[-1] // 2
    x1, x2 = x[..., :half], x[..., half:]
    c = cos[:, None, :].astype(x.dtype)
    s = sin[:, None, :].astype(x.dtype)
    return jnp.concatenate([x1 * c - x2 * s, x2 * c + x1 * s], -1)


def context_attention(qc, kc, vc, sink=None):
    B, L, HQ, dh = qc.shape
    HKV = kc.shape[2]
    G = HQ // HKV
    qg = qc.reshape(B, L, HKV, G, dh) * dh ** -0.5
    s = jnp.einsum('bqkgd,blkd->bkgql', qg, kc, preferred_element_type=F32)
    if sink is None:
        parts = (s,)
    else:
        parts = (s, jnp.broadcast_to(sink.astype(F32).reshape(1, HKV, G, 1, 1), s.shape[:-1] + (1,)))
    e, denom = softmax_parts(*parts)
    o = jnp.einsum('bkgql,blkd->bkgqd', e[0], vc.astype(F32)) / denom
    return jnp.transpose(o, (0, 3, 1, 2, 4)).reshape(B, L, HQ * dh).astype(qc.dtype)


def neighbourhood_attention(q, k, v, kc, vc, rpb):
    B, N, H, dh = q.shape
    rows = N // GRID_W
    kh = min(NA_WIN_H, rows)
    r = jnp.arange(rows)
    key_rows = jnp.clip(r - kh // 2, 0, rows - kh)[:, None] + jnp.arange(kh)[None, :]
    col = jnp.arange(GRID_W)
    col_start = jnp.clip(col - NA_WIN_W // 2, 0, GRID_W - NA_WIN_W)
    col_in = (col[None, :] >= col_start[:, None]) & (col[None, :] < col_start[:, None] + NA_WIN_W)
    dy = key_rows - r[:, None] + NA_WIN_H - 1
    dx = jnp.clip(col[None, :] - col[:, None], 1 - NA_WIN_W, NA_WIN_W - 1) + NA_WIN_W - 1
    bias = rpb.astype(F32)[:, dy[:, None, :, None], dx[None, :, None, :]]
    qg = q.reshape(B, rows, GRID_W, H, dh) * dh ** -0.5
    kg = k.reshape(B, rows, GRID_W, H, dh)[:, key_rows]
    vg = v.reshape(B, rows, GRID_W, H, dh)[:, key_rows].reshape(B, rows, kh * GRID_W, H, dh)
    s_lat = jnp.einsum('brqhd,brjkhd->bhrqjk', qg, kg, preferred_element_type=F32) + bias
    s_lat = jnp.where(col_in[:, None, :], s_lat, NEG_INF).reshape(B, H, rows, GRID_W, kh * GRID_W)
    s_ctx = jnp.einsum('brqhd,blhd->bhrql', qg, kc, preferred_element_type=F32)
    (e_lat, e_ctx), denom = softmax_parts(s_lat, s_ctx)
    o = (jnp.einsum('bhrqj,brjhd->bhrqd', e_lat, vg.astype(F32))
         + jnp.einsum('bhrql,blhd->bhrqd', e_ctx, vc.astype(F32))) / denom
    return jnp.transpose(o, (0, 2, 3, 1, 4)).reshape(B, N, H * dh).astype(q.dtype)


def window_attention(q, k, v, kc, vc, sink):
    B, N, HQ, dh = q.shape
    HKV = k.shape[2]
    G = HQ // HKV
    P = SWA_BLOCK
    nb = N // P
    pad = ((0, 0), (P, P), (0, 0), (0, 0))
    band = jnp.arange(nb)[:, None] * P + jnp.arange(3 * P)[None, :]
    kb = jnp.pad(k, pad)[:, band]
    vb = jnp.pad(v, pad)[:, band]
    qpos = jnp.arange(nb)[:, None] * P + jnp.arange(P)[None, :]
    kpos = (band - P)[:, None, :]
    valid = (jnp.abs(qpos[:, :, None] - kpos) <= SWA_WINDOW) & (kpos >= 0) & (kpos < N)
    qb = q.reshape(B, nb, P, HKV, G, dh) * dh ** -0.5
    s_lat = jnp.einsum('bnqkgd,bnjkd->bkgnqj', qb, kb, preferred_element_type=F32)
    s_lat = jnp.where(valid, s_lat, NEG_INF)
    s_ctx = jnp.einsum('bnqkgd,blkd->bkgnql', qb, kc, preferred_element_type=F32)
    s_sink = jnp.broadcast_to(sink.astype(F32).reshape(1, HKV, G, 1, 1, 1), s_ctx.shape[:-1] + (1,))
    (e_lat, e_ctx, _), denom = softmax_parts(s_lat, s_ctx, s_sink)
    o = (jnp.einsum('bkgnqj,bnjkd->bkgnqd', e_lat, vb.astype(F32))
         + jnp.einsum('bkgnql,blkd->bkgnqd', e_ctx, vc.astype(F32))) / denom
    return jnp.transpose(o, (0, 3, 4, 1, 2, 5)).reshape(B, N, HQ * dh).astype(q.dtype)


def centred_depthwise_conv(x, w):
    K, C = w.shape
    return lax.conv_general_dilated(x, w[:, None, :].astype(x.dtype), window_strides=(1,),
                                    padding=[(K // 2, K // 2)], dimension_numbers=('NWC', 'WIO', 'NWC'),
                                    feature_group_count=C)


def l2norm(t):
    return t * lax.rsqrt(jnp.sum(t * t, -1, keepdims=True) + RMS_EPS)


def delta_inputs(qkv, ab, conv_w, a_log, dt_bias):
    B, T, _ = qkv.shape
    qkv = jax.nn.silu(centred_depthwise_conv(qkv, conv_w)).astype(F32)
    q, k, v = jnp.split(qkv, 3, axis=-1)
    to_heads = lambda t: jnp.swapaxes(t.reshape(B, T, DN_HEADS, DN_HEAD_DIM), 1, 2)
    q, k, v = to_heads(q), to_heads(k), to_heads(v)
    q = l2norm(q) * DN_HEAD_DIM ** -0.5
    k = l2norm(k)
    ab = ab.astype(F32).reshape(B, T, 2, 2, DN_HEADS)
    g = -jnp.exp(a_log.astype(F32)) * jax.nn.softplus(ab[:, :, 0] + dt_bias.astype(F32))
    beta = jax.nn.sigmoid(ab[:, :, 1])
    return q, k, v, jnp.transpose(g, (2, 0, 3, 1)), jnp.transpose(beta, (2, 0, 3, 1))


def gated_delta_chunked(q, k, v, g, beta, s0):
    B, H, T, dk = k.shape
    dv = v.shape[-1]
    C = DN_CHUNK
    nc = T // C
    q = q.reshape(B, H, nc, C, dk)
    k = k.reshape(B, H, nc, C, dk)
    v = v.reshape(B, H, nc, C, dv)
    g = jnp.cumsum(g.reshape(B, H, nc, C), -1)
    beta = beta.reshape(B, H, nc, C)
    kb = k * beta[..., None]
    vb = v * beta[..., None]
    incl = jnp.tril(jnp.ones((C, C), bool))
    strict = jnp.tril(jnp.ones((C, C), bool), -1)
    decay = jnp.where(incl, jnp.exp(jnp.where(incl, g[..., :, None] - g[..., None, :], 0.0)), 0.0)
    m = jnp.where(strict, jnp.einsum('bhnid,bhnjd->bhnij', kb, k) * decay, 0.0)
    eye = jnp.eye(C, dtype=F32)
    tmat = lax.linalg.triangular_solve(eye + m, jnp.broadcast_to(eye, m.shape), left_side=True,
                                       lower=True, unit_diagonal=True)
    u = jnp.einsum('bhnij,bhnjd->bhnid', tmat, vb)
    w = jnp.einsum('bhnij,bhnjd->bhnid', tmat, kb * jnp.exp(g)[..., None])
    a_intra = jnp.where(incl, jnp.einsum('bhnid,bhnjd->bhnij', q, k) * decay, 0.0)

    def step(s, xs):
        q_i, k_i, u_i, w_i, g_i, a_i = xs
        v_new = u_i - jnp.einsum('bhcd,bhde->bhce', w_i, s)
        o_i = jnp.einsum('bhcd,bhde->bhce', q_i * jnp.exp(g_i)[..., None], s) + jnp.einsum('bhij,bhje->bhie', a_i, v_new)
        g_last = g_i[..., -1:]
        s = s * jnp.exp(g_last)[..., None] + jnp.einsum('bhcd,bhce->bhde', k_i * jnp.exp(g_last - g_i)[..., None], v_new)
        return s, o_i

    xs = tuple(jnp.moveaxis(t, 2, 0) for t in (q, k, u, w, g, a_intra))
    s_final, o = lax.scan(step, s0, xs)
    return jnp.moveaxis(o, 0, 2).reshape(B, H, T, dv), s_final


def delta_bidirectional(q, k, v, g, beta, s0):
    o_f, s_f = gated_delta_chunked(q, k, v, g[0], beta[0], s0[0])
    flip = lambda t: jnp.flip(t, axis=2)
    o_b, s_b = gated_delta_chunked(flip(q), flip(k), flip(v), flip(g[1]), flip(beta[1]), s0[1])
    return o_f + flip(o_b), jnp.stack([s_f, s_b])


def gated_rmsnorm(o, z, w):
    B, H, T, dv = o.shape
    o = jnp.swapaxes(o, 1, 2)
    o = o * lax.rsqrt(jnp.mean(o * o, -1, keepdims=True) + RMS_EPS) * w.astype(F32)
    return (o * jax.nn.silu(z.astype(F32).reshape(B, T, H, dv))).reshape(B, T, H * dv)


def mixer_ab(h, hc, w_in, rpb, conv_w, a_log, dt_bias, norm_w, w_out, need_ctx):
    qa, ka, va, qkv_b, z, ab = jnp.split(h @ w_in, AB_SPLITS, axis=-1)
    qac, kac, vac, qkv_bc, zc, abc = jnp.split(hc @ w_in, AB_SPLITS, axis=-1)
    na = lambda t: t.reshape(t.shape[0], t.shape[1], NA_HEADS, NA_HEAD_DIM)
    o_a = neighbourhood_attention(na(qa), na(ka), na(va), na(kac), na(vac), rpb)
    B = h.shape[0]
    s0 = jnp.zeros((2, B, DN_HEADS, DN_HEAD_DIM, DN_HEAD_DIM), F32)
    o_bc, s_ctx = delta_bidirectional(*delta_inputs(qkv_bc, abc, conv_w, a_log, dt_bias), s0)
    o_b, _ = delta_bidirectional(*delta_inputs(qkv_b, ab, conv_w, a_log, dt_bias), s_ctx)
    o_b = gated_rmsnorm(o_b, z, norm_w).astype(h.dtype)
    out = jnp.concatenate([o_a, o_b], -1) @ w_out
    if not need_ctx:
        return out, None
    o_ac = context_attention(na(qac), na(kac), na(vac))
    o_bc = gated_rmsnorm(o_bc, zc, norm_w).astype(h.dtype)
    return out, jnp.concatenate([o_ac, o_bc], -1) @ w_out


def mixer_c(h, hc, w_in, sink, w_out, cos, sin, need_ctx):
    B, N, _ = h.shape
    L = hc.shape[1]
    p = h @ w_in
    q = apply_axial_rope(p[..., :SWA_Q_WIDTH].reshape(B, N, SWA_Q_HEADS, SWA_HEAD_DIM), cos, sin)
    k = apply_axial_rope(p[..., SWA_Q_WIDTH:SWA_Q_WIDTH + SWA_KV_WIDTH].reshape(B, N, SWA_KV_HEADS, SWA_HEAD_DIM), cos, sin)
    v = p[..., SWA_Q_WIDTH + SWA_KV_WIDTH:].reshape(B, N, SWA_KV_HEADS, SWA_HEAD_DIM)
    kvc = hc @ w_in[:, SWA_Q_WIDTH:]
    kc = kvc[..., :SWA_KV_WIDTH].reshape(B, L, SWA_KV_HEADS, SWA_HEAD_DIM)
    vc = kvc[..., SWA_KV_WIDTH:].reshape(B, L, SWA_KV_HEADS, SWA_HEAD_DIM)
    out = window_attention(q, k, v, kc, vc, sink) @ w_out
    if not need_ctx:
        return out, None
    qc = (hc @ w_in[:, :SWA_Q_WIDTH]).reshape(B, L, SWA_Q_HEADS, SWA_HEAD_DIM)
    return out, context_attention(qc, kc, vc, sink) @ w_out


def moe_ffn(t, w_router, b_router, w_gu, b_gu, w_down, b_down):
    logits = jnp.matmul(t, w_router, preferred_element_type=F32) + b_router.astype(F32)
    top_v, top_i = lax.top_k(logits, TOP_K)
    gates = jax.nn.softmax(top_v, axis=-1)
    combine = jnp.einsum('tk,tke->te', gates, jax.nn.one_hot(top_i, N_EXPERTS, dtype=F32))
    y = jnp.zeros(t.shape, F32)
    for e in range(N_EXPERTS):
        gu = t @ w_gu[e] + b_gu[e]
        gate = jnp.minimum(gu[:, :D_EXPERT], SWIGLU_LIMIT)
        up = jnp.clip(gu[:, D_EXPERT:], -SWIGLU_LIMIT, SWIGLU_LIMIT)
        hidden = gate * jax.nn.sigmoid(SWIGLU_ALPHA * gate) * (up + 1)
        y = y + combine[:, e:e + 1] * (hidden @ w_down[e] + b_down[e]).astype(F32)
    return y.astype(t.dtype)


def setup_inputs(seed: int = 0) -> dict:
    key = jax.random.key(seed)
    ks = jax.random.split(key, 24)
    nrm = lambda k, shape, s: jax.random.normal(k, shape, F32) * s
    D = D_MODEL
    dt = jnp.exp(jax.random.uniform(ks[12], (N_EVEN, 2, DN_HEADS), F32, math.log(1e-3), math.log(1e-1)))
    return {
        "x": nrm(ks[0], (BATCH, SEQ, D), 1.0),
        "c": nrm(ks[1], (BATCH, D), 1.0),
        "ctx": nrm(ks[2], (BATCH, CTX_LEN, D), 1.0),
        "c_ctx": nrm(ks[3], (D,), 1.0),
        "w_mod": nrm(ks[4], (DEPTH, D, 6 * D), 0.5 * D ** -0.5),
        "b_mod": nrm(ks[5], (DEPTH, 6 * D), 0.02),
        "ln_g": 1.0 + nrm(ks[6], (DEPTH, 2, D), 0.05),
        "ln_b": nrm(ks[7], (DEPTH, 2, D), 0.02),
        "w_in_ab": nrm(ks[8], (N_EVEN, D, AB_IN), D ** -0.5),
        "na_rpb": nrm(ks[9], (N_EVEN, NA_HEADS, 2 * NA_WIN_H - 1, 2 * NA_WIN_W - 1), 0.5),
        "dn_conv": nrm(ks[10], (N_EVEN, DN_CONV, 3 * DN_WIDTH), DN_CONV ** -0.5),
        "dn_a_log": jnp.log(jax.random.uniform(ks[11], (N_EVEN, 2, DN_HEADS), F32, 1.0, 16.0)),
        "dn_dt_bias": dt + jnp.log(-jnp.expm1(-dt)),
        "dn_norm_w": 1.0 + nrm(ks[13], (N_EVEN, DN_HEAD_DIM), 0.05),
        "w_out_ab": nrm(ks[14], (N_EVEN, NA_WIDTH + DN_WIDTH, D), DEEPNORM_BETA * (NA_WIDTH + DN_WIDTH) ** -0.5),
        "w_in_c": nrm(ks[15], (N_ODD, D, C_IN), D ** -0.5),
        "swa_sink": nrm(ks[16], (N_ODD, SWA_Q_HEADS), 1.0),
        "w_out_c": nrm(ks[17], (N_ODD, SWA_Q_WIDTH, D), DEEPNORM_BETA * SWA_Q_WIDTH ** -0.5),
        "w_router": nrm(ks[18], (DEPTH, D, N_EXPERTS), D ** -0.5),
        "b_router": nrm(ks[19], (DEPTH, N_EXPERTS), 0.01),
        "w_gu": nrm(ks[20], (DEPTH, N_EXPERTS, D, 2 * D_EXPERT), D ** -0.5),
        "b_gu": nrm(ks[21], (DEPTH, N_EXPERTS, 2 * D_EXPERT), 0.02),
        "w_down": nrm(ks[22], (DEPTH, N_EXPERTS, D_EXPERT, D), DEEPNORM_BETA * D_EXPERT ** -0.5),
        "b_down": nrm(ks[23], (DEPTH, N_EXPERTS, D), 0.02),
    }


def reference(x, c, ctx, c_ctx, w_mod, b_mod, ln_g, ln_b, w_in_ab, na_rpb, dn_conv, dn_a_log, dn_dt_bias,
              dn_norm_w, w_out_ab, w_in_c, swa_sink, w_out_c, w_router, b_router, w_gu, b_gu, w_down, b_down):
    B, N, D = x.shape
    L = ctx.shape[1]
    cos, sin = axial_rope_tables(N, SWA_HEAD_DIM)
    silu_c = jax.nn.silu(c)
    silu_cc = jax.nn.silu(c_ctx)
    h_lat, h_ctx = x, ctx
    for layer in range(DEPTH):
        last = layer == DEPTH - 1
        mod = (silu_c @ w_mod[layer] + b_mod[layer])[:, None, :]
        modc = silu_cc @ w_mod[layer] + b_mod[layer]
        sh1, sc1, g1, sh2, sc2, g2 = jnp.split(mod, 6, axis=-1)
        csh1, csc1, cg1, csh2, csc2, cg2 = jnp.split(modc, 6, axis=-1)
        a_lat = h_lat * (1 + sc1) + sh1
        a_ctx = h_ctx * (1 + csc1) + csh1
        i = layer // 2
        if layer % 2 == 0:
            m_lat, m_ctx = mixer_ab(a_lat, a_ctx, w_in_ab[i], na_rpb[i], dn_conv[i], dn_a_log[i], dn_dt_bias[i],
                                    dn_norm_w[i], w_out_ab[i], not last)
        else:
            m_lat, m_ctx = mixer_c(a_lat, a_ctx, w_in_c[i], swa_sink[i], w_out_c[i], cos, sin, not last)
        h_lat = layer_norm(DEEPNORM_ALPHA * h_lat + g1 * m_lat, ln_g[layer, 0], ln_b[layer, 0])
        f_lat = (h_lat * (1 + sc2) + sh2).reshape(B * N, D)
        if last:
            f = moe_ffn(f_lat, w_router[layer], b_router[layer], w_gu[layer], b_gu[layer], w_down[layer], b_down[layer])
        else:
            h_ctx = layer_norm(DEEPNORM_ALPHA * h_ctx + cg1 * m_ctx, ln_g[layer, 0], ln_b[layer, 0])
            f_ctx = (h_ctx * (1 + csc2) + csh2).reshape(B * L, D)
            f = moe_ffn(jnp.concatenate([f_lat, f_ctx], 0), w_router[layer], b_router[layer], w_gu[layer],
                        b_gu[layer], w_down[layer], b_down[layer])
            h_ctx = layer_norm(DEEPNORM_ALPHA * h_ctx + cg2 * f[B * N:].reshape(B, L, D), ln_g[layer, 1], ln_b[layer, 1])
        h_lat = layer_norm(DEEPNORM_ALPHA * h_lat + g2 * f[:B * N].reshape(B, N, D), ln_g[layer, 1], ln_b[layer, 1])
    return h_lat
```

```python
import numpy as np
import concourse.bass as bass
import concourse.mybir as mybir

F32 = mybir.dt.float32
BF16 = mybir.dt.bfloat16
I32 = mybir.dt.int32
ALU = mybir.AluOpType
AF = mybir.ActivationFunctionType
AX = mybir.AxisListType

ENGS = ["pe", "act", "dve", "pool", "sp"]
NSLOT = 8


class Prog:
    def __init__(self, nc):
        self.nc = nc
        self.ops = {e: [] for e in ENGS}
        self.last_w = {}
        self.readers = {}
        self.known = {e: {} for e in ENGS}
        self.ndma = {e: 0 for e in ENGS}
        self.sb_off = 16512
        self.sb_hi = 16512
        self.uid = 0
        self.psrr = 0
        self.epoch = 0
        self.epoch_start = {e: 0 for e in ENGS}

    def sb(self, shape, dtype=F32, name=None):
        self.uid += 1
        nm = f"{name or 't'}_{self.uid}"
        esz = 2 if dtype == BF16 else 4
        nbytes = int(np.prod(shape[1:])) * esz
        off = (self.sb_off + 63) // 64 * 64
        assert off + nbytes <= 229344, f"SBUF overflow {off + nbytes} for {nm}"
        t = self.nc.alloc_sbuf_tensor_at(nm, list(shape), dtype, offset=off)
        self.sb_off = off + nbytes
        self.sb_hi = max(self.sb_hi, self.sb_off)
        return t

    def mark(self):
        return self.sb_off

    def release(self, mark):
        self.barrier()
        self.sb_off = mark

    def dram(self, shape, dtype=F32, name=None):
        self.uid += 1
        return self.nc.dram_tensor(f"{name or 'd'}_{self.uid}", list(shape), dtype, kind="Internal")

    def _need(self, eng, ev):
        stream, val = ev
        k = self.known[eng]
        if k.get(stream, -1) >= val:
            return False
        k[stream] = val
        return True

    def op(self, eng, fn, r=(), w=(), dma=False):
        waits = []
        deps = []
        for key in r:
            ev = self.last_w.get(key)
            if ev is not None:
                deps.append(ev)
        for key in w:
            ev = self.last_w.get(key)
            if ev is not None:
                deps.append(ev)
            deps.extend(self.readers.get(key, ()))
        idx = len(self.ops[eng])
        if dma:
            n = self.ndma[eng]
            self.ndma[eng] += 1
            slot, cnt = n % NSLOT, n // NSLOT + 1
            myev = (("d", eng, slot), cnt)
            if cnt > 1:
                deps.append((("d", eng, slot), cnt - 1))
        else:
            myev = (("c", eng, self.epoch), idx)
        for ev in deps:
            stream, val = ev
            if stream[:2] == ("c", eng) and eng == "pe":
                continue
            if self._need(eng, ev):
                waits.append(ev)
        self.ops[eng].append(dict(fn=fn, waits=waits, dma=myev if dma else None, ep=self.epoch))
        for key in r:
            self.readers.setdefault(key, []).append(myev)
        for key in w:
            self.last_w[key] = myev
            self.readers[key] = []
        return myev

    def barrier(self):
        evs = []
        for e in ENGS:
            li = None
            for i in range(len(self.ops[e]) - 1, -1, -1):
                o = self.ops[e][i]
                if o["fn"] is not None and o["dma"] is None:
                    li = i
                    break
            if li is not None:
                evs.append((("c", e, self.ops[e][li]["ep"]), li))
            n = self.ndma[e]
            for s in range(min(n, NSLOT)):
                last = (n - 1 - s) // NSLOT * NSLOT + s
                evs.append((("d", e, s), last // NSLOT + 1))
        for e in ENGS:
            waits = []
            for ev in evs:
                if ev[0][:2] == ("c", e):
                    if e == "pe":
                        continue
                if self._need(e, ev):
                    waits.append(ev)
            if waits:
                self.ops[e].append(dict(fn=None, waits=waits, dma=None, ep=self.epoch))
        if any(len(self.ops[e]) - self.epoch_start[e] > 25000 for e in ENGS):
            self.epoch += 1
            self.epoch_start = {e: len(self.ops[e]) for e in ENGS}

    def emit(self):
        nc = self.nc
        sig = {e: set() for e in ENGS}
        for e in ENGS:
            for o in self.ops[e]:
                for (stream, val) in o["waits"]:
                    if stream[0] == "c":
                        sig[stream[1]].add(val)
        cnt = {}
        used_ep = set()
        for e in ENGS:
            c = {}
            m = {}
            for i in range(len(self.ops[e])):
                if i in sig[e]:
                    ep = self.ops[e][i]["ep"]
                    c[ep] = c.get(ep, 0) + 1
                    assert c[ep] < 65000, "semaphore overflow"
                    m[i] = c[ep]
                    used_ep.add((e, ep))
            cnt[e] = m
        from contextlib import ExitStack
        with ExitStack() as st:
            csem = {(e, ep): st.enter_context(nc.semaphore(f"c_{e}_{ep}")) for (e, ep) in sorted(used_ep)}
            dsem = {(e, s): st.enter_context(nc.semaphore(f"d_{e}_{s}"))
                    for e in ENGS if self.ndma[e] for s in range(min(NSLOT, self.ndma[e]))}
            block = st.enter_context(nc.Block())

            def run(e, eng):
                for i, o in enumerate(self.ops[e]):
                    for (stream, val) in o["waits"]:
                        if stream[0] == "c":
                            eng.wait_ge(csem[(stream[1], stream[2])], cnt[stream[1]][val])
                        else:
                            eng.wait_ge(dsem[(stream[1], stream[2])], 16 * val)
                    if o["fn"] is None:
                        continue
                    ins = o["fn"](eng)
                    if o["dma"] is not None:
                        (_, q, slot), _c = o["dma"]
                        ins.then_inc(dsem[(q, slot)], 16)
                    elif i in sig[e]:
                        ins.then_inc(csem[(e, o["ep"])], 1)

            @block.tensor
            def _(eng):
                run("pe", eng)

            @block.scalar
            def _(eng):
                run("act", eng)

            @block.vector
            def _(eng):
                run("dve", eng)

            @block.gpsimd
            def _(eng):
                run("pool", eng)

            @block.sync
            def _(eng):
                run("sp", eng)

    def dma(self, out, in_, r=(), w=(), q="sp", **kw):
        return self.op(q, lambda e: e.dma_start(out=out, in_=in_, **kw), r=r, w=w, dma=True)

    def mm(self, out, lhsT, rhs, start=True, stop=True, r=(), w=(), **kw):
        return self.op("pe", lambda e: e.matmul(out, lhsT, rhs, start=start, stop=stop, **kw), r=r, w=w)

    def tr(self, out, in_, ident, r=(), w=()):
        return self.op("pe", lambda e: e.transpose(out, in_, ident), r=r, w=w)

    def act(self, out, in_, func, r=(), w=(), **kw):
        return self.op("act", lambda e: e.activation(out, in_, func, **kw), r=r, w=w)

    def v(self, name, *args, r=(), w=(), **kw):
        return self.op("dve", lambda e: getattr(e, name)(*args, **kw), r=r, w=w)

    def g(self, name, *args, r=(), w=(), **kw):
        return self.op("pool", lambda e: getattr(e, name)(*args, **kw), r=r, w=w)

    def a(self, name, *args, r=(), w=(), **kw):
        return self.op("act", lambda e: getattr(e, name)(*args, **kw), r=r, w=w)

    def getps(self, nb=1):
        if nb == 2:
            b = (self.psrr + 1) // 2 * 2 % 8
            self.psrr = (b + 2) % 8
            return b * 512, [("ps", b), ("ps", b + 1)]
        b = self.psrr % 8
        self.psrr = (b + 1) % 8
        return b * 512, [("ps", b)]

    def finish(self):
        self.barrier()

import numpy as np


D = 1024; NLAT = 2048; LCTX = 256; T = 2304; NT = 18; NLT = 16
ALPHA = 8.0 ** 0.25
NEG = -1.0e30
LN_EPS = 1e-5
RMS_EPS = 1e-6
SB_ = 0


def host_consts():
    c = {}
    c["ident"] = np.eye(128, dtype=np.float32)
    c["ones"] = np.ones((128, 128), np.float32)
    i = np.arange(128)
    c["ut_incl"] = (i[None, :] >= i[:, None]).astype(np.float32)
    c["ut_strict"] = (i[None, :] > i[:, None]).astype(np.float32)
    c["lt_incl"] = (i[None, :] <= i[:, None]).astype(np.float32)
    c["lt_strict"] = (i[None, :] < i[:, None]).astype(np.float32)
    mp = np.where(i[None, :] >= i[:, None], 0.0, NEG).astype(np.float32)
    mn = np.where(i[None, :] <= i[:, None], 0.0, NEG).astype(np.float32)
    z = np.zeros((128, 128), np.float32); zc = np.zeros((128, 256), np.float32)
    c["swa_int"] = np.concatenate([mp, z, mn, zc], 1)
    c["swa_first"] = np.concatenate([z, mn, zc], 1)
    c["swa_last"] = np.concatenate([mp, z, zc], 1)
    qc = np.arange(64); cs = np.clip(qc - 8, 0, 48)
    colin = (qc[None, :] >= cs[:, None]) & (qc[None, :] < cs[:, None] + 16)
    cm = np.where(colin, 0.0, NEG).astype(np.float32)
    c["na_cm"] = np.tile(cm, (2, 10))
    t = np.arange(NLAT)
    inv = (10000.0 ** (-np.arange(16, dtype=np.float32) / 16)).astype(np.float32)
    row = (t // 64).astype(np.float32)[:, None]; col = (t % 64).astype(np.float32)[:, None]
    ang = np.concatenate([row * inv, col * inv], -1).astype(np.float32)
    cos = np.cos(ang).astype(np.float32); sin = np.sin(ang).astype(np.float32)
    cosF = np.ones((128, T), np.float32); sinF = np.zeros((128, T), np.float32)
    for pth in range(128):
        j = pth % 64; f = j % 32
        cosF[pth, :NLAT] = cos[:, f]
        sinF[pth, :NLAT] = (-sin[:, f]) if j < 32 else sin[:, f]
    c["cosF"] = cosF; c["sinF"] = sinF
    return c


CONST_SHAPES = {"ident": [128, 128], "ones": [128, 128], "ut_incl": [128, 128], "ut_strict": [128, 128],
                "lt_incl": [128, 128], "lt_strict": [128, 128], "swa_int": [128, 640], "swa_first": [128, 512],
                "swa_last": [128, 512], "na_cm": [128, 640], "cosF": [128, T], "sinF": [128, T]}

IN_SHAPES = {
    "x": [NLAT, D], "ctx": [LCTX, D], "c": [1, D], "c_ctx": [1, D],
    "w_mod": [4, D, 6 * D], "b_mod": [4, 6 * D], "ln_g": [4, 2, D], "ln_b": [4, 2, D],
    "w_in_ab": [2, D, 3600], "na_rpb": [2, 8, 15, 31], "dn_conv": [2, 5, 1536], "dn_a_log": [2, 2, 4],
    "dn_dt_bias": [2, 2, 4], "dn_norm_w": [2, 128], "w_out_ab": [2, D, D], "w_in_c": [2, D, 1280],
    "swa_sink": [2, 16], "w_out_c": [2, D, D], "w_router": [4, D, 32], "b_router": [4, 32],
    "w_gu": [4, 32, D, 2048], "b_gu": [4, 32, 2048], "w_down": [4, 32, D, D], "b_down": [4, 32, D],
}


class Ctx:
    pass


def setup(nc, out_shape=(NLAT, D), out_name="out"):
    p = Prog(nc)
    k = Ctx()
    k.p = p; k.nc = nc; k.wl = lambda l: l
    k.inp = {n: nc.dram_tensor(n, s, F32, kind="ExternalInput").ap() for n, s in IN_SHAPES.items()}
    k.cin = {n: nc.dram_tensor("k_" + n, s, F32, kind="ExternalInput").ap() for n, s in CONST_SHAPES.items()}
    k.out = nc.dram_tensor(out_name, list(out_shape), F32, kind="ExternalOutput").ap()
    k.ps = nc.alloc_psum_tensor("psall", [128, 4096], F32)
    k.ident = p.sb([128, 128], F32, "ident"); p.dma(k.ident[:], k.cin["ident"], w=["ident"])
    k.ones = p.sb([128, 128], F32, "ones"); p.dma(k.ones[:], k.cin["ones"], w=["ones"])
    k.epsln = p.sb([128, 1], F32, "epsln"); p.v("memset", k.epsln[:], LN_EPS, w=["epsln"])
    k.Hd = p.dram([T, D], F32, "Hd").ap()
    k.MIXd = p.dram([T, D], F32, "MIXd").ap()
    k.MODROW = p.dram([2, 6 * D], F32, "MODROW").ap()
    k.MODT = p.sb([128, 2, 48], F32, "MODT")
    k.S2 = p.sb([128, 8, 2], F32, "S2")
    return k


def psap(k, col0, n, parts=128, p0=0):
    return k.ps[p0:p0 + parts, col0:col0 + n]


def stage_init(k):
    p = k.p
    m = p.mark()
    bufs = [p.sb([128, 4, D], F32, "cp") for _ in range(2)]
    xv = k.inp["x"].rearrange("(n a p) d -> n p a d", p=128, a=4)
    hv = k.Hd[0:NLAT].rearrange("(n a p) d -> n p a d", p=128, a=4)
    for n in range(4):
        b = bufs[n % 2]; key = ("cp", n % 2)
        p.dma(b[:], xv[n], w=[key])
        p.dma(hv[n], b[:], r=[key], w=[("Hd", 4 * n + a) for a in range(4)], q="act")
    b = bufs[0]; key = ("cp", 0)
    cv = k.inp["ctx"].rearrange("(a p) d -> p a d", p=128)
    p.dma(b[:, 0:2, :], cv, w=[key])
    p.dma(k.Hd[NLAT:T].rearrange("(a p) d -> p a d", p=128), b[:, 0:2, :], r=[key], w=[("Hd", 16), ("Hd", 17)], q="act")
    cs = p.sb([8, 2, 128], F32, "cs")
    p.dma(cs[:, 0, :], k.inp["c"].rearrange("o (k p) -> (o k) p", p=128), w=["cs"])
    p.dma(cs[:, 1, :], k.inp["c_ctx"].rearrange("o (k p) -> (o k) p", p=128), w=["cs"])
    cs2 = p.sb([8, 2, 128], F32, "cs2")
    p.act(cs2[:], cs[:], AF.Silu, r=["cs"], w=["cs2"])
    for s in range(2):
        c0, keys = p.getps()
        p.tr(psap(k, c0, 8), cs2[:, s, :], k.ident[0:8, 0:8], r=["cs2", "ident"], w=keys)
        p.v("tensor_copy", k.S2[:, :, s], psap(k, c0, 8), r=keys, w=["S2"])
    p.release(m)


def stage_mod(k, l):
    p = k.p
    m = p.mark()
    wbuf = [p.sb([128, 8, 512], F32, "wm") for _ in range(2)]
    rows = p.sb([2, 6 * D], F32, "modrows")
    brow = p.sb([2, 6 * D], F32, "bmodrows")
    p.dma(brow[:], k.inp["b_mod"][l:l + 1, :].broadcast_to([2, 6 * D]), w=["brow"])
    wv = k.inp["w_mod"][l].rearrange("(kc p) n -> p kc n", p=128)
    for cb in range(12):
        wb = wbuf[cb % 2]; key = ("wm", cb % 2)
        p.dma(wb[:], wv[:, :, cb * 512:(cb + 1) * 512], w=[key], q="sp" if cb % 2 == 0 else "act")
        c0, keys = p.getps()
        for kc in range(8):
            p.mm(psap(k, c0, 512, 2), k.S2[:, kc, :], wb[:, kc, :], start=(kc == 0), stop=(kc == 7),
                 r=[key, "S2"], w=keys)
        p.v("tensor_tensor", rows[:, cb * 512:(cb + 1) * 512], psap(k, c0, 512, 2), brow[:, cb * 512:(cb + 1) * 512],
            ALU.add, r=keys + ["brow"], w=["rows"])
    p.dma(k.MODROW, rows[:], r=["rows"], w=["MODROW"])
    mt = p.sb([48, 2, 128], F32, "mt")
    for s in range(2):
        p.dma(mt[:, s, :], k.MODROW[s:s + 1, :].rearrange("o (j p) -> (o j) p", p=128), r=["MODROW"], w=["mt"])
    for s in range(2):
        c0, keys = p.getps()
        p.tr(psap(k, c0, 48), mt[:, s, :], k.ident[0:48, 0:48], r=["mt", "ident"], w=keys)
        p.v("tensor_copy", k.MODT[:, s, :], psap(k, c0, 48), r=keys, w=["MODT"])
    for j0 in (8, 32):
        p.v("tensor_scalar_add", k.MODT[:, :, j0:j0 + 8], k.MODT[:, :, j0:j0 + 8], 1.0, r=["MODT"], w=["MODT"])
    p.release(m)


def load_gate(k, tile, s, j, q="sp", key=None):
    k.p.dma(tile[:], k.MODROW[s:s + 1, j * D:(j + 1) * D].broadcast_to([128, D]), r=["MODROW"], w=[key], q=q)


def stage_AT(k, AT, sh_j, sc_j, ntiles, atkey="AT", XF=None):
    p = k.p
    m = p.mark()
    hb = [p.sb([128, D], F32, "hb") for _ in range(2)]
    for tt in range(ntiles):
        b = hb[tt % 2]; key = ("hb", tt % 2)
        s = 0 if tt < NLT else 1
        p.dma(b[:], k.Hd[tt * 128:(tt + 1) * 128, :], r=[("Hd", tt)], w=[key], q="sp" if tt % 2 == 0 else "act")
        for half in range(2):
            c0, keys = p.getps()
            for q4 in range(4):
                kc = half * 4 + q4
                p.tr(psap(k, c0 + q4 * 128, 128), b[:, kc * 128:(kc + 1) * 128], k.ident[:], r=[key, "ident"], w=keys)
            for q4 in range(4):
                kc = half * 4 + q4
                p.act(AT[:, kc, tt * 128:(tt + 1) * 128], psap(k, c0 + q4 * 128, 128), AF.Identity,
                      scale=k.MODT[:, s, sc_j * 8 + kc:sc_j * 8 + kc + 1], bias=k.MODT[:, s, sh_j * 8 + kc:sh_j * 8 + kc + 1],
                      r=keys + ["MODT"], w=[(atkey, tt)])
    p.release(m)


def ln_tile(k, y, ykey, out, outkey, lng, lnb, gkeys, scratch):
    p = k.p
    st, mv, rs = scratch
    for h in range(2):
        p.v("bn_stats", st[:, h, :], y[:, h * 512:(h + 1) * 512], r=[ykey], w=["lnst"])
    p.v("bn_aggr", mv[:], st[:].rearrange("p a b -> p (a b)"), r=["lnst"], w=["lnmv"])
    p.act(rs[:], mv[:, 1:2], AF.Sqrt, bias=k.epsln[:], scale=1.0, r=["lnmv", "epsln"], w=["lnrs"])
    p.v("reciprocal", rs[:], rs[:], r=["lnrs"], w=["lnrs"])
    p.v("tensor_scalar", y[:], y[:], mv[:, 0:1], rs[:], ALU.subtract, ALU.mult, r=[ykey, "lnmv", "lnrs"], w=[ykey])
    p.v("tensor_tensor", y[:], y[:], lng[:], ALU.mult, r=[ykey] + gkeys, w=[ykey])
    p.v("tensor_tensor", out[:], y[:], lnb[:], ALU.add, r=[ykey] + gkeys, w=[outkey])


TB5 = [(0, 512), (512, 512), (1024, 512), (1536, 512), (2048, 256)]


def attn_bufs(k):
    p = k.p
    b = Ctx()
    b.S = [p.sb([128, 896], F32, "aS") for _ in range(2)]
    b.P = [p.sb([128, 896], F32, "aP") for _ in range(2)]
    b.PT = [p.sb([128, 7, 128], BF16, "aPT") for _ in range(2)]
    b.sm = [p.sb([128, 8], F32, "asm") for _ in range(2)]
    b.i = 0
    return b


def attn_unit(k, b, qT, qkeys, segs, bias, bkeys, sink, skeys, out, okey):
    p = k.p
    i = b.i % 2; b.i += 1
    S = b.S[i]; P = b.P[i]; PT = b.PT[i]; sm = b.sm[i]
    kS = ("aS", i); kP = ("aP", i); kPT = ("aPT", i); ksm = ("asm", i)
    c0, keys = p.getps(2)
    col = 0
    vts = []
    for (kT, kkeys, vlist) in segs:
        n = kT.shape[-1]
        off = 0
        while off < n:
            take = min(n - off, 512 - (col % 512))
            p.mm(psap(k, c0 + col, take), qT, kT[:, off:off + take], r=qkeys + kkeys, w=keys)
            off += take; col += take
        vts.extend(vlist)
    W = col
    nt = W // 128
    if bias is not None:
        p.v("scalar_tensor_tensor", S[:, :W], psap(k, c0, W), 0.125, bias, ALU.mult, ALU.add, r=keys + bkeys, w=[kS])
    else:
        p.v("tensor_scalar_mul", S[:, :W], psap(k, c0, W), 0.125, r=keys, w=[kS])
    p.v("reduce_max", sm[:, 0:1], S[:, :W], AX.X, r=[kS], w=[ksm])
    if sink is not None:
        p.v("tensor_tensor", sm[:, 0:1], sm[:, 0:1], sink, ALU.max, r=[ksm] + skeys, w=[ksm])
    p.v("tensor_scalar_mul", sm[:, 1:2], sm[:, 0:1], -1.0, r=[ksm], w=[ksm])
    p.act(P[:, :W], S[:, :W], AF.Exp, bias=sm[:, 1:2], scale=1.0, accum_out=sm[:, 2:3], r=[kS, ksm], w=[kP, ksm])
    if sink is not None:
        p.act(sm[:, 3:4], sink, AF.Exp, bias=sm[:, 1:2], scale=1.0, r=[ksm] + skeys, w=[ksm])
        p.v("tensor_tensor", sm[:, 2:3], sm[:, 2:3], sm[:, 3:4], ALU.add, r=[ksm], w=[ksm])
    p.v("reciprocal", sm[:, 4:5], sm[:, 2:3], r=[ksm], w=[ksm])
    c1, keys1 = p.getps(2)
    for t in range(nt):
        p.tr(psap(k, c1 + t * 128, 128), P[:, t * 128:(t + 1) * 128], k.ident[:], r=[kP, "ident"], w=keys1)
    p.act(PT[:, :nt, :], psap(k, c1, W).rearrange("p (a b) -> p a b", b=128), AF.Copy, r=keys1, w=[kPT])
    c2, keys2 = p.getps()
    for t in range(nt):
        vap, vkey = vts[t]
        p.mm(psap(k, c2, 64), PT[:, t, :], vap, start=(t == 0), stop=(t == nt - 1), r=[kPT, vkey], w=keys2)
    p.v("tensor_scalar_mul", out, psap(k, c2, 64), sm[:, 4:5], r=keys2 + [ksm], w=[okey])


def swap_cols(k, dst, src, nh, rkeys, wkeys):
    dv = dst[:].rearrange("p kc (h two j) -> p kc h two j", two=2, j=32)
    sv = src[:].rearrange("p kc (h two j) -> p kc h two j", two=2, j=32)
    for kc in range(8):
        for two in range(2):
            k.p.g("tensor_copy", dv[:, kc, :, two, :], sv[:, kc, :, 1 - two, :], r=rkeys, w=wkeys)


def stage_mixer_c(k, l, need_ctx):
    p = k.p
    i = l // 2
    nq = NT if need_ctx else NLT
    m = p.mark()
    QT = p.sb([128, 8, T], BF16, "QT"); KT = p.sb([128, 2, T], BF16, "KT"); V = p.sb([128, NT, 128], BF16, "V")
    mA = p.mark()
    AT = p.sb([128, 8, T], BF16, "AT")
    stage_AT(k, AT, 0, 1, NT)
    atk = [("AT", t) for t in range(NT)]
    Wq = p.sb([128, 8, 1024], BF16, "Wq"); Wqs = p.sb([128, 8, 1024], BF16, "Wqs")
    Wk = p.sb([128, 8, 256], BF16, "Wk"); Wks = p.sb([128, 8, 256], BF16, "Wks"); Wv = p.sb([128, 8, 128], BF16, "Wv")
    cosF = p.sb([128, T], F32, "cosF"); sinF = p.sb([128, T], F32, "sinF")
    p.dma(cosF[:], k.cin["cosF"], w=["cosF"]); p.dma(sinF[:], k.cin["sinF"], w=["sinF"], q="act")
    w = k.inp["w_in_c"][i].rearrange("(kc p) n -> p kc n", p=128)
    for h in range(2):
        p.dma(Wq[:, :, h * 512:(h + 1) * 512], w[:, :, h * 512:(h + 1) * 512], w=["Wq"], q="pool")
    for g in range(2):
        for d in range(2):
            p.dma(Wk[:, :, g * 128 + d * 64:g * 128 + d * 64 + 64], w[:, :, 1024 + g * 64:1024 + g * 64 + 64], w=["Wk"], q="pool")
    p.dma(Wv[:], w[:, :, 1152:1280], w=["Wv"], q="pool")
    swap_cols(k, Wqs, Wq, 16, ["Wq"], ["Wqs"])
    swap_cols(k, Wks, Wk, 4, ["Wk"], ["Wks"])
    t1 = [p.sb([128, 512], F32, "t1") for _ in range(2)]
    t2 = [p.sb([128, 512], F32, "t2") for _ in range(2)]
    ri = 0
    for (Wa, Wb, ka, kb, dst, dkey, nch, ntok_lim) in ((Wq, Wqs, "Wq", "Wqs", QT, "QT", 8, nq * 128), (Wk, Wks, "Wk", "Wks", KT, "KT", 2, T)):
        for j in range(nch):
            for (t0, n) in TB5:
                if t0 >= ntok_lim:
                    continue
                ak = [("AT", t0 // 128 + a) for a in range(n // 128)]
                ca, kA = p.getps(); cb, kB = p.getps()
                for kc in range(8):
                    p.mm(psap(k, ca, n), Wa[:, kc, j * 128:(j + 1) * 128], AT[:, kc, t0:t0 + n], start=(kc == 0), stop=(kc == 7), r=[ka] + ak, w=kA)
                for kc in range(8):
                    p.mm(psap(k, cb, n), Wb[:, kc, j * 128:(j + 1) * 128], AT[:, kc, t0:t0 + n], start=(kc == 0), stop=(kc == 7), r=[kb] + ak, w=kB)
                r2 = ri % 2; ri += 1
                p.v("tensor_tensor", t1[r2][:, :n], psap(k, ca, n), cosF[:, t0:t0 + n], ALU.mult, r=kA + ["cosF"], w=[("t1", r2)])
                p.v("tensor_tensor", t2[r2][:, :n], psap(k, cb, n), sinF[:, t0:t0 + n], ALU.mult, r=kB + ["sinF"], w=[("t2", r2)])
                p.g("tensor_tensor", dst[:, j, t0:t0 + n], t1[r2][:, :n], t2[r2][:, :n], ALU.add, r=[("t1", r2), ("t2", r2)], w=[(dkey, j)])
    for tt in range(NT):
        c0, keys = p.getps()
        for kc in range(8):
            p.mm(psap(k, c0, 128), AT[:, kc, tt * 128:(tt + 1) * 128], Wv[:, kc, :], start=(kc == 0), stop=(kc == 7), r=["Wv", ("AT", tt)], w=keys)
        p.act(V[:, tt, :], psap(k, c0, 128), AF.Copy, r=keys, w=[("V", tt)])
    p.release(mA)
    MI = p.sb([128, 640], F32, "MI"); MF = p.sb([128, 512], F32, "MF"); ML = p.sb([128, 512], F32, "ML")
    p.dma(MI[:], k.cin["swa_int"], w=["MI"]); p.dma(MF[:], k.cin["swa_first"], w=["MF"]); p.dma(ML[:], k.cin["swa_last"], w=["ML"])
    SK = p.sb([128, 16], F32, "SK")
    p.dma(SK[:], k.inp["swa_sink"][i:i + 1, :].broadcast_to([128, 16]), w=["SK"])
    ab = attn_bufs(k)
    O = [p.sb([128, D], F32, "O") for _ in range(2)]
    for qt in range(nq):
        oi = qt % 2
        for h in range(16):
            j = h // 2; hp = h % 2; g = h // 8
            ps_ = slice(hp * 64, hp * 64 + 64)
            qT = QT[ps_, j, qt * 128:(qt + 1) * 128]
            ctxseg = (KT[ps_, g, NLAT:T], [("KT", g)], [(V[:, 16 + a, g * 64:(g + 1) * 64], ("V", 16 + a)) for a in range(2)])
            if qt < NLT:
                lo = max(qt - 1, 0); hi = min(qt + 1, NLT - 1)
                latseg = (KT[ps_, g, lo * 128:(hi + 1) * 128], [("KT", g)], [(V[:, t, g * 64:(g + 1) * 64], ("V", t)) for t in range(lo, hi + 1)])
                segs = [latseg, ctxseg]
                if qt == 0:
                    bias, bk = MF[:, :], ["MF"]
                elif qt == NLT - 1:
                    bias, bk = ML[:, :], ["ML"]
                else:
                    bias, bk = MI[:, :], ["MI"]
            else:
                segs = [ctxseg]; bias, bk = None, []
            attn_unit(k, ab, qT, [("QT", j)], segs, bias, bk, SK[:, h:h + 1], ["SK"], O[oi][:, h * 64:(h + 1) * 64], ("O", oi))
        p.dma(k.MIXd[qt * 128:(qt + 1) * 128, :], O[oi][:], r=[("O", oi)], w=[("MIXd", qt)], q="act")
    p.release(m)


def stage_outproj(k, l, w_out, ntiles, X2T, COMB):
    p = k.p
    m = p.mark()
    Wo = p.sb([128, 8, D], BF16, "Wo")
    wv = w_out.rearrange("(kc p) n -> p kc n", p=128)
    for h in range(2):
        p.dma(Wo[:, :, h * 512:(h + 1) * 512], wv[:, :, h * 512:(h + 1) * 512], w=[("Wo", h)], q="pool")
    Wr = p.sb([128, 8, 32], F32, "Wr")
    p.dma(Wr[:], k.inp["w_router"][l].rearrange("(kc p) n -> p kc n", p=128), w=["Wr"])
    br = p.sb([128, 32], F32, "br")
    p.dma(br[:], k.inp["b_router"][l:l + 1, :].broadcast_to([128, 32]), w=["br"])
    G1 = [p.sb([128, D], F32, "G1") for _ in range(2)]
    for s in range(2):
        load_gate(k, G1[s], s, 2, q="act", key=("G1", s))
    lng = p.sb([128, D], F32, "lng"); lnb = p.sb([128, D], F32, "lnb")
    p.dma(lng[:], k.inp["ln_g"][l, 0:1, :].broadcast_to([128, D]), w=["lng"], q="act")
    p.dma(lnb[:], k.inp["ln_b"][l, 0:1, :].broadcast_to([128, D]), w=["lnb"], q="act")
    mix = [p.sb([128, D], F32, "mix") for _ in range(2)]
    hb = [p.sb([128, D], F32, "hb2") for _ in range(2)]
    MT = [p.sb([128, 8, 128], BF16, "MT") for _ in range(2)]
    y = [p.sb([128, D], F32, "y") for _ in range(2)]
    hn = [p.sb([128, D], F32, "hn") for _ in range(2)]
    XF = [p.sb([128, 8, 128], F32, "XF") for _ in range(2)]
    st = p.sb([128, 2, 6], F32, "st"); mv = p.sb([128, 2], F32, "mv"); rs = p.sb([128, 1], F32, "rs")
    lg = p.sb([128, 32], F32, "lg"); m8 = p.sb([128, 8], F32, "m8"); msk = p.sb([128, 32], F32, "msk")
    ex = p.sb([128, 32], F32, "ex"); sm = p.sb([128, 1], F32, "sm"); nm = p.sb([128, 1], F32, "nm")
    for tt in range(ntiles):
        i = tt % 2; s = 0 if tt < NLT else 1
        p.dma(mix[i][:], k.MIXd[tt * 128:(tt + 1) * 128, :], r=[("MIXd", tt)], w=[("mix", i)])
        p.dma(hb[i][:], k.Hd[tt * 128:(tt + 1) * 128, :], r=[("Hd", tt)], w=[("hb2", i)], q="act")
        for half in range(2):
            c0, keys = p.getps()
            for q4 in range(4):
                kc = half * 4 + q4
                p.tr(psap(k, c0 + q4 * 128, 128), mix[i][:, kc * 128:(kc + 1) * 128], k.ident[:], r=[("mix", i), "ident"], w=keys)
            p.act(MT[i][:, half * 4:half * 4 + 4, :], psap(k, c0, 512).rearrange("p (a b) -> p a b", b=128), AF.Copy,
                  r=keys, w=[("MT", i)])
        c0, keys = p.getps(2)
        for half in range(2):
            for kc in range(8):
                p.mm(psap(k, c0 + half * 512, 512), MT[i][:, kc, :], Wo[:, kc, half * 512:(half + 1) * 512],
                     start=(kc == 0), stop=(kc == 7), r=[("MT", i), ("Wo", half)], w=keys)
        p.v("tensor_tensor", y[i][:], psap(k, c0, D), G1[s][:], ALU.mult, r=keys + [("G1", s)], w=[("y", i)])
        p.v("scalar_tensor_tensor", y[i][:], hb[i][:], ALPHA, y[i][:], ALU.mult, ALU.add, r=[("hb2", i), ("y", i)], w=[("y", i)])
        ln_tile(k, y[i], ("y", i), hn[i], ("hn", i), lng, lnb, ["lng", "lnb"], (st, mv, rs))
        p.dma(k.Hd[tt * 128:(tt + 1) * 128, :], hn[i][:], r=[("hn", i)], w=[("Hd", tt)])
        for half in range(2):
            c0, keys = p.getps()
            for q4 in range(4):
                kc = half * 4 + q4
                p.tr(psap(k, c0 + q4 * 128, 128), hn[i][:, kc * 128:(kc + 1) * 128], k.ident[:], r=[("hn", i), "ident"], w=keys)
            for q4 in range(4):
                kc = half * 4 + q4
                p.act(XF[i][:, kc, :], psap(k, c0 + q4 * 128, 128), AF.Identity,
                      scale=k.MODT[:, s, 32 + kc:33 + kc], bias=k.MODT[:, s, 24 + kc:25 + kc],
                      r=keys + ["MODT"], w=[("XF", i)])
        p.act(X2T[:, :, tt * 128:(tt + 1) * 128], XF[i][:], AF.Copy, r=[("XF", i)], w=[("X2T", tt)])
        c0, keys = p.getps()
        for kc in range(8):
            p.mm(psap(k, c0, 32), XF[i][:, kc, :], Wr[:, kc, :], start=(kc == 0), stop=(kc == 7), r=[("XF", i), "Wr"], w=keys)
        p.v("tensor_tensor", lg[:], psap(k, c0, 32), br[:], ALU.add, r=keys + ["br"], w=["lg"])
        p.v("max", m8[:], lg[:], r=["lg"], w=["m8"])
        p.v("tensor_scalar", msk[:], lg[:], m8[:, 3:4], None, ALU.is_ge, r=["lg", "m8"], w=["msk"])
        p.v("tensor_scalar_mul", nm[:], m8[:, 0:1], -1.0, r=["m8"], w=["nm"])
        p.act(ex[:], lg[:], AF.Exp, bias=nm[:], scale=1.0, r=["lg", "nm"], w=["ex"])
        p.v("tensor_tensor", ex[:], ex[:], msk[:], ALU.mult, r=["ex", "msk"], w=["ex"])
        p.v("reduce_sum", sm[:], ex[:], AX.X, r=["ex"], w=["sm"])
        p.v("reciprocal", sm[:], sm[:], r=["sm"], w=["sm"])
        p.v("tensor_scalar_mul", COMB[:, tt, :], ex[:], sm[:], r=["ex", "sm"], w=[("COMB", tt)])
    p.release(m)


def stage_moe(k, l, ntiles, X2T, COMB, out_dram, last):
    p = k.p
    m = p.mark()
    TB = [(t0, min(4, ntiles - t0)) for t0 in range(0, ntiles, 4)]
    F = p.sb([128, ntiles, D], F32, "F")
    BGU = p.sb([128, 16, 32], F32, "BGU")
    m1 = p.mark()
    bg_raw = p.sb([32, 2048], F32, "bgraw")
    p.dma(bg_raw[:], k.inp["b_gu"][l], w=["bgraw"])
    for c4 in range(4):
        c0, keys = p.getps()
        for q in range(4):
            c = c4 * 4 + q
            p.tr(psap(k, c0 + q * 32, 32), bg_raw[:, c * 128:(c + 1) * 128], k.ident[0:32, 0:32], r=["bgraw", "ident"], w=keys)
        p.v("tensor_copy", BGU[:, c4 * 4:c4 * 4 + 4, :], psap(k, c0, 128).rearrange("p (a b) -> p a b", b=32), r=keys, w=["BGU"])
    bd = p.sb([32, D], F32, "bd")
    p.dma(bd[:], k.inp["b_down"][l], w=["bd"])
    ct = p.sb([32, 128], F32, "ct")
    for tt in range(ntiles):
        c0, keys = p.getps()
        p.tr(psap(k, c0, 128, 32), COMB[:, tt, :], k.ident[:], r=[("COMB", tt), "ident"], w=keys)
        p.v("tensor_copy", ct[:], psap(k, c0, 128, 32), r=keys, w=["ct"])
        c0, keys = p.getps(2)
        for half in range(2):
            p.mm(psap(k, c0 + half * 512, 512), ct[:], bd[:, half * 512:(half + 1) * 512], r=["ct", "bd"], w=keys)
        p.act(F[:, tt, :], psap(k, c0, D), AF.Copy, r=keys, w=[("F", tt)])
    p.release(m1)
    m2 = p.mark()
    NB = 2
    Wg = [p.sb([128, 8, 512], BF16, "Wg") for _ in range(NB)]
    Wu = [p.sb([128, 8, 512], BF16, "Wu") for _ in range(NB)]
    Wd = [p.sb([128, 4, D], BF16, "Wd") for _ in range(NB)]
    HT = [p.sb([128, 4, 512], BF16, "HT") for _ in range(2)]
    tg = [p.sb([128, 512], F32, "tg") for _ in range(2)]
    ts_ = [p.sb([128, 512], F32, "ts") for _ in range(2)]
    tu = [p.sb([128, 512], F32, "tu") for _ in range(2)]
    tu2 = [p.sb([128, 512], F32, "tu2") for _ in range(2)]
    gi = 0; ci = 0; hi = 0
    for e in range(32):
        wgu = k.inp["w_gu"][k.wl(l), e].rearrange("(kc p) n -> p kc n", p=128)
        wdn = k.inp["w_down"][k.wl(l), e].rearrange("(j p) n -> p j n", p=128)
        for hh in range(2):
            b = gi % NB; gi += 1
            p.dma(Wg[b][:], wgu[:, :, hh * 512:(hh + 1) * 512], w=[("Wg", b)], q="pool")
            p.dma(Wu[b][:], wgu[:, :, 1024 + hh * 512:1024 + (hh + 1) * 512], w=[("Wu", b)], q="pool")
            p.dma(Wd[b][:], wdn[:, hh * 4:(hh + 1) * 4, :], w=[("Wd", b)], q="pool")
            for (t0, nt) in TB:
                ntok = nt * 128
                hb = hi % 2; hi += 1
                xkeys = [("X2T", t0 + a) for a in range(nt)]
                for j in range(4):
                    cidx = hh * 4 + j
                    cg, kg = p.getps(); cu, ku = p.getps()
                    for kc in range(8):
                        p.mm(psap(k, cg, ntok), Wg[b][:, kc, j * 128:(j + 1) * 128], X2T[:, kc, t0 * 128:t0 * 128 + ntok],
                             start=(kc == 0), stop=(kc == 7), r=[("Wg", b)] + xkeys, w=kg)
                    for kc in range(8):
                        p.mm(psap(k, cu, ntok), Wu[b][:, kc, j * 128:(j + 1) * 128], X2T[:, kc, t0 * 128:t0 * 128 + ntok],
                             start=(kc == 0), stop=(kc == 7), r=[("Wu", b)] + xkeys, w=ku)
                    c2 = ci % 2; ci += 1
                    p.v("tensor_scalar", tg[c2][:, :ntok], psap(k, cg, ntok), BGU[:, cidx, e:e + 1], 7.0, ALU.add, ALU.min,
                        r=kg + ["BGU"], w=[("tg", c2)])
                    p.act(ts_[c2][:, :ntok], tg[c2][:, :ntok], AF.Sigmoid, scale=1.702, r=[("tg", c2)], w=[("ts", c2)])
                    p.act(tu[c2][:, :ntok], psap(k, cu, ntok), AF.Identity, bias=BGU[:, 8 + cidx, e:e + 1], scale=1.0,
                          r=ku + ["BGU"], w=[("tu", c2)])
                    p.g("tensor_scalar", tu2[c2][:, :ntok], tu[c2][:, :ntok], 7.0, -7.0, ALU.min, ALU.max, r=[("tu", c2)], w=[("tu2", c2)])
                    p.g("tensor_scalar", tu2[c2][:, :ntok], tu2[c2][:, :ntok], 1.0, 1.0, ALU.add, ALU.mult, r=[("tu2", c2)], w=[("tu2", c2)])
                    p.g("tensor_tensor", tu2[c2][:, :ntok], tu2[c2][:, :ntok], tg[c2][:, :ntok], ALU.mult,
                        r=[("tu2", c2), ("tg", c2)], w=[("tu2", c2)])
                    p.v("tensor_tensor", HT[hb][:, j, :ntok], tu2[c2][:, :ntok], ts_[c2][:, :ntok], ALU.mult,
                        r=[("tu2", c2), ("ts", c2)], w=[("HT", hb, j)])
                for a in range(nt):
                    tt = t0 + a
                    for half in range(2):
                        cy, ky = p.getps()
                        for j in range(4):
                            p.mm(psap(k, cy, 512), HT[hb][:, j, a * 128:(a + 1) * 128], Wd[b][:, j, half * 512:(half + 1) * 512],
                                 start=(j == 0), stop=(j == 3), r=[("HT", hb, j), ("Wd", b)], w=ky)
                        p.v("scalar_tensor_tensor", F[:, tt, half * 512:(half + 1) * 512], psap(k, cy, 512), COMB[:, tt, e:e + 1],
                            F[:, tt, half * 512:(half + 1) * 512], ALU.mult, ALU.add, r=ky + [("COMB", tt), ("F", tt)], w=[("F", tt)])
    p.release(m2)
    G2 = [p.sb([128, D], F32, "G2") for _ in range(2)]
    for s in range(2):
        load_gate(k, G2[s], s, 5, q="act", key=("G2", s))
    lng = p.sb([128, D], F32, "lng2"); lnb = p.sb([128, D], F32, "lnb2")
    p.dma(lng[:], k.inp["ln_g"][l, 1:2, :].broadcast_to([128, D]), w=["lng2"], q="act")
    p.dma(lnb[:], k.inp["ln_b"][l, 1:2, :].broadcast_to([128, D]), w=["lnb2"], q="act")
    hb3 = [p.sb([128, D], F32, "hb3") for _ in range(2)]
    ho = [p.sb([128, D], F32, "ho") for _ in range(2)]
    st = p.sb([128, 2, 6], F32, "st2"); mv = p.sb([128, 2], F32, "mv2"); rs = p.sb([128, 1], F32, "rs2")
    for tt in range(ntiles):
        i = tt % 2; s = 0 if tt < NLT else 1
        p.dma(hb3[i][:], k.Hd[tt * 128:(tt + 1) * 128, :], r=[("Hd", tt)], w=[("hb3", i)])
        p.v("tensor_tensor", F[:, tt, :], F[:, tt, :], G2[s][:], ALU.mult, r=[("F", tt), ("G2", s)], w=[("F", tt)])
        p.v("scalar_tensor_tensor", F[:, tt, :], hb3[i][:], ALPHA, F[:, tt, :], ALU.mult, ALU.add, r=[("hb3", i), ("F", tt)], w=[("F", tt)])
        ln_tile(k, F[:, tt, :], ("F", tt), ho[i], ("ho", i), lng, lnb, ["lng2", "lnb2"], (st, mv, rs))
        if last:
            p.dma(out_dram[tt * 128:(tt + 1) * 128, :], ho[i][:], r=[("ho", i)], w=[("OUT", tt)], q="act")
        else:
            p.dma(k.Hd[tt * 128:(tt + 1) * 128, :], ho[i][:], r=[("ho", i)], w=[("Hd", tt)], q="act")
    p.release(m)


def na_class(qt):
    return 0 if qt == 0 else 1 if qt == 1 else 2 if qt <= 13 else 3 if qt == 14 else 4


def stage_mixer_ab(k, l, need_ctx):
    p = k.p
    i = l // 2
    nq = NT if need_ctx else NLT
    if not hasattr(k, "QKTd"):
        k.QKTd_h = p.dram([1024, T], BF16, "QKTd"); k.QKTd = k.QKTd_h.ap()
        k.Vd = p.dram([T, 512], BF16, "Vd").ap()
        k.QKVBd = p.dram([1536, T], F32, "QKVBd").ap()
        k.Zd = p.dram([T, 512], F32, "Zd").ap()
        k.ABd = p.dram([T, 16], F32, "ABd").ap()
        k.VZ_h = p.dram([120, 64, 128], F32, "VZ"); k.VZ = k.VZ_h.ap()
    m = p.mark()
    AT = p.sb([128, 8, T], BF16, "AT")
    stage_AT(k, AT, 0, 1, NT)
    W = p.sb([128, 8, 3600], BF16, "Wab")
    w = k.inp["w_in_ab"][i].rearrange("(kc p) n -> p kc n", p=128)
    for c0 in range(0, 3600, 512):
        c1 = min(c0 + 512, 3600)
        p.dma(W[:, :, c0:c1], w[:, :, c0:c1], w=["Wab"], q="pool")
    sb16 = [p.sb([128, 512], BF16, "st16") for _ in range(2)]
    sf32 = [p.sb([128, 512], F32, "st32") for _ in range(2)]
    ui = 0
    for ch in list(range(8)) + list(range(12, 24)):
        isq = ch < 8
        for (t0, n) in TB5:
            ak = [("AT", t0 // 128 + a) for a in range(n // 128)]
            c0, keys = p.getps()
            for kc in range(8):
                p.mm(psap(k, c0, n), W[:, kc, ch * 128:(ch + 1) * 128], AT[:, kc, t0:t0 + n], start=(kc == 0), stop=(kc == 7), r=["Wab"] + ak, w=keys)
            u = ui % 2; ui += 1
            if isq:
                st, sk = sb16[u], ("st16", u)
                dst, dk = k.QKTd[ch * 128:(ch + 1) * 128, t0:t0 + n], ("QKTd", ch)
            else:
                st, sk = sf32[u], ("st32", u)
                dst, dk = k.QKVBd[(ch - 12) * 128:(ch - 11) * 128, t0:t0 + n], ("QKVBd", ch - 12)
            if ui % 2:
                p.act(st[:, :n], psap(k, c0, n), AF.Copy, r=keys, w=[sk])
            else:
                p.v("tensor_copy", st[:, :n], psap(k, c0, n), r=keys, w=[sk])
            p.dma(dst, st[:, :n], r=[sk], w=[dk], q="sp" if ui % 2 else "act")
    sab = [p.sb([128, 16], F32, "stab") for _ in range(2)]
    for tt in range(NT):
        u = tt % 2
        for (cs, n, st, sk, dst, dk) in ((1024, 512, sb16[u], ("st16", u), k.Vd[tt * 128:(tt + 1) * 128, :], ("Vd", tt)),
                                         (3072, 512, sf32[u], ("st32", u), k.Zd[tt * 128:(tt + 1) * 128, :], ("Zd", tt)),
                                         (3584, 16, sab[u], ("stab", u), k.ABd[tt * 128:(tt + 1) * 128, :], ("ABd", tt))):
            c0, keys = p.getps()
            for kc in range(8):
                p.mm(psap(k, c0, n), AT[:, kc, tt * 128:(tt + 1) * 128], W[:, kc, cs:cs + n], start=(kc == 0), stop=(kc == 7), r=["Wab", ("AT", tt)], w=keys)
            p.act(st[:, :n], psap(k, c0, n), AF.Copy, r=keys, w=[sk])
            p.dma(dst, st[:, :n], r=[sk], w=[dk], q="sp")
    p.release(m)
    if getattr(k, "do_na", True):
        stage_na(k, i, nq)
    if getattr(k, "do_dn", True):
        stage_deltanet(k, i, nq)


def stage_na(k, i, nq):
    p = k.p
    m = p.mark()
    QT4 = p.sb([128, 4, T], BF16, "QT4"); KT4 = p.sb([128, 4, T], BF16, "KT4"); V = p.sb([128, NT, 512], BF16, "V")
    for j in range(4):
        p.dma(QT4[:, j, :], k.QKTd[j * 128:(j + 1) * 128, :], r=[("QKTd", j)], w=[("QT4", j)])
        p.dma(KT4[:, j, :], k.QKTd[(4 + j) * 128:(5 + j) * 128, :], r=[("QKTd", 4 + j)], w=[("KT4", j)], q="act")
    p.dma(V[:], k.Vd.rearrange("(t p) c -> p t c", p=128), r=[("Vd", t) for t in range(NT)], w=["V"])
    CM = p.sb([128, 640], F32, "CM"); p.dma(CM[:], k.cin["na_cm"], w=["CM"])
    rp = p.sb([120, 128], F32, "rp")
    p.v("memset", rp[:], 0.0, w=["rp"])
    p.dma(rp[:, 48:79], k.inp["na_rpb"][i].rearrange("h a b -> (h a) b"), w=["rp"])
    for r0 in range(0, 64, 16):
        p.dma(k.VZ[:, r0:r0 + 16, :], rp[:].unsqueeze(1).broadcast_to([120, 16, 128]), r=["rp"], w=["VZ"])
    BI = [[p.sb([128, 896], F32, "BI") for _ in range(8)] for _ in range(2)]

    def build_bias(cls):
        s = cls % 2
        qt = {0: 0, 1: 1, 2: 2, 3: 14, 4: 15}[cls]
        nk = 5 if cls == 2 else 4
        kbase = int(np.clip(2 * qt - 4, 0, 24))
        for h in range(8):
            t = BI[s][h]; key = ("BI", s, h)
            p.v("memset", t[:, 0:nk * 128], NEG, w=[key])
            p.v("memset", t[:, nk * 128:nk * 128 + 256], 0.0, w=[key])
            for qrl in range(2):
                qr = 2 * qt + qrl
                k0 = int(np.clip(qr - 4, 0, 24))
                a_start = k0 - qr + 7
                cstart = (k0 - kbase) * 64
                src = bass.AP(tensor=k.VZ_h, offset=(h * 15 + a_start) * 8192 + 63, ap=[[127, 64], [8192, 8], [1, 64]])
                dst = t[qrl * 64:(qrl + 1) * 64, cstart:cstart + 512].rearrange("p (a b) -> p a b", b=64)
                p.dma(dst, src, r=["VZ"], w=[key], q="sp" if qrl == 0 else "act")
            p.v("tensor_tensor", t[:, 0:nk * 128], t[:, 0:nk * 128], CM[:, 0:nk * 128], ALU.add, r=[key, "CM"], w=[key])

    build_bias(0); build_bias(1)
    ab = attn_bufs(k)
    O = [p.sb([128, 512], F32, "Ona") for _ in range(2)]
    for qt in range(nq):
        oi = qt % 2
        if qt == 1:
            build_bias(2)
        if qt == 2:
            build_bias(3)
        if qt == 14:
            build_bias(4)
        for h in range(8):
            j = h // 2; hp = h % 2
            ps_ = slice(hp * 64, hp * 64 + 64)
            qT = QT4[ps_, j, qt * 128:(qt + 1) * 128]
            ctxseg = (KT4[ps_, j, NLAT:T], [("KT4", j)], [(V[:, 16 + a, h * 64:(h + 1) * 64], "V") for a in range(2)])
            if qt < NLT:
                cls = na_class(qt); nk = 5 if cls == 2 else 4
                kt0 = int(np.clip(2 * qt - 4, 0, 24)) // 2
                latseg = (KT4[ps_, j, kt0 * 128:(kt0 + nk) * 128], [("KT4", j)], [(V[:, t, h * 64:(h + 1) * 64], "V") for t in range(kt0, kt0 + nk)])
                segs = [latseg, ctxseg]
                bias, bk = BI[cls % 2][h][:, 0:nk * 128 + 256], [("BI", cls % 2, h)]
            else:
                segs = [ctxseg]; bias, bk = None, []
            attn_unit(k, ab, qT, [("QT4", j)], segs, bias, bk, None, [], O[oi][:, h * 64:(h + 1) * 64], ("Ona", oi))
        p.dma(k.MIXd[qt * 128:(qt + 1) * 128, 0:512], O[oi][:], r=[("Ona", oi)], w=[("MIXd", qt)], q="act")
    p.release(m)


def stage_deltanet(k, i, nq):
    p = k.p
    m = p.mark()
    masks = {}
    for nme in ("ut_incl", "ut_strict", "lt_incl", "lt_strict"):
        masks[nme] = p.sb([128, 128], F32, nme)
        p.dma(masks[nme][:], k.cin[nme], w=[nme])
        EPSC = p.sb([128, 1], F32, "EPSC"); p.v("memset", EPSC[:], RMS_EPS, w=["EPSC"])
    cwr = p.sb([5, 1536], F32, "cwr"); p.dma(cwr[:], k.inp["dn_conv"][i], w=["cwr"])
    CW = p.sb([128, 12, 5], F32, "CW")
    for rc in range(12):
        c0, keys = p.getps()
        p.tr(psap(k, c0, 5), cwr[:, rc * 128:(rc + 1) * 128], k.ident[0:5, 0:5], r=["cwr", "ident"], w=keys)
        p.v("tensor_copy", CW[:, rc, :], psap(k, c0, 5), r=keys, w=["CW"])
    AB = p.sb([128, NT, 16], F32, "AB")
    p.dma(AB[:], k.ABd.rearrange("(t p) c -> p t c", p=128), r=[("ABd", t) for t in range(NT)], w=["AB"])
    ALB = p.sb([128, 8], F32, "ALB"); DTB = p.sb([128, 8], F32, "DTB")
    p.dma(ALB[:], k.inp["dn_a_log"][i:i + 1].rearrange("o a b -> o (a b)").broadcast_to([128, 8]), w=["ALB"])
    p.dma(DTB[:], k.inp["dn_dt_bias"][i:i + 1].rearrange("o a b -> o (a b)").broadcast_to([128, 8]), w=["DTB"])
    p.act(ALB[:], ALB[:], AF.Exp, r=["ALB"], w=["ALB"])
    p.v("tensor_scalar_mul", ALB[:], ALB[:], -1.0, r=["ALB"], w=["ALB"])
    Gg = p.sb([128, NT, 8], F32, "Gg"); BETA = p.sb([128, NT, 8], F32, "BETA")
    tA = p.sb([128, NT, 8], F32, "tA"); tB = p.sb([128, NT, 8], F32, "tB")
    for t in range(NT):
        p.v("tensor_tensor", Gg[:, t, :], AB[:, t, 0:8], DTB[:], ALU.add, r=["AB", "DTB"], w=["Gg"])
    p.act(tA[:], Gg[:], AF.Abs, r=["Gg"], w=["tA"])
    p.act(tA[:], tA[:], AF.Exp, scale=-1.0, r=["tA"], w=["tA"])
    p.act(tA[:], tA[:], AF.Ln, bias=1.0, scale=1.0, r=["tA"], w=["tA"])
    p.v("tensor_scalar_max", tB[:], Gg[:], 0.0, r=["Gg"], w=["tB"])
    p.v("tensor_tensor", tA[:], tA[:], tB[:], ALU.add, r=["tA", "tB"], w=["tA"])
    for t in range(NT):
        p.v("tensor_tensor", Gg[:, t, :], tA[:, t, :], ALB[:], ALU.mult, r=["tA", "ALB", "Gg"], w=["Gg"])
    p.act(BETA[:], AB[:, :, 8:16], AF.Sigmoid, r=["AB"], w=["BETA"])
    OB = p.sb([128, NT, 4, 128], F32, "OB")
    written = set()
    if getattr(k, "dn_stop", "") == "gates":
        p.dma(k.out[1536:1664, 0:NT * 8], Gg[:].rearrange("p a b -> p (a b)"), r=["Gg"], w=["dbg"])
        p.dma(k.out[1664:1792, 0:NT * 8], BETA[:].rearrange("p a b -> p (a b)"), r=["BETA"], w=["dbg"])
        p.dma(k.out[0:128, 0:60], CW[:].rearrange("p a b -> p (a b)"), r=["CW"], w=["dbg"])
        p.release(m)
        return
    mH = p.mark()
    for hp2 in range(2):
        heads = [2 * hp2, 2 * hp2 + 1]
        QN = {}; KN = {}; VV = {}
        for h in heads:
            QN[h] = p.sb([128, T], F32, "QN"); KN[h] = p.sb([128, T], F32, "KN"); VV[h] = p.sb([128, T], F32, "VV")
        mP = p.mark()
        XP = [p.sb([128, 2312], F32, "XP") for _ in range(2)]
        SQ = p.sb([128, T], F32, "SQ"); RS = p.sb([128, T], F32, "RS")
        for u in range(2):
            p.v("memset", XP[u][:], 0.0, w=[("XP", u)])
        xi = 0
        for h in heads:
            for kind, dstd in (("q", QN), ("k", KN), ("v", VV)):
                rc = {"q": 0, "k": 4, "v": 8}[kind] + h
                u = xi % 2; xi += 1
                xp = XP[u]; xk = ("XP", u)
                Y = dstd[h]; yk = (kind, h)
                p.dma(xp[:, 2:2050], k.QKVBd[rc * 128:(rc + 1) * 128, 0:NLAT], r=[("QKVBd", rc)], w=[xk])
                p.dma(xp[:, 2054:2310], k.QKVBd[rc * 128:(rc + 1) * 128, NLAT:T], r=[("QKVBd", rc)], w=[xk], q="act")
                for (y0, x0, n) in ((0, 0, NLAT), (NLAT, 2052, LCTX)):
                    p.v("tensor_scalar_mul", Y[:, y0:y0 + n], xp[:, x0:x0 + n], CW[:, rc, 0:1], r=[xk, "CW"], w=[yk])
                    for kk in range(1, 5):
                        p.v("scalar_tensor_tensor", Y[:, y0:y0 + n], xp[:, x0 + kk:x0 + kk + n], CW[:, rc, kk:kk + 1], Y[:, y0:y0 + n],
                            ALU.mult, ALU.add, r=[xk, "CW", yk], w=[yk])
                p.act(Y[:], Y[:], AF.Silu, r=[yk], w=[yk])
                if kind != "v" and not getattr(k, "dn_nol2", False):
                    p.act(SQ[:], Y[:], AF.Square, r=[yk], w=["SQ"])
                    for (t0, n) in TB5:
                        c0, keys = p.getps()
                        p.mm(psap(k, c0, n), k.ones[:], SQ[:, t0:t0 + n], r=["ones", "SQ"], w=keys)
                        p.act(RS[:, t0:t0 + n], psap(k, c0, n), AF.Sqrt, bias=EPSC[:], scale=1.0, r=keys + ["EPSC"], w=["RS"])
                        p.v("reciprocal", RS[:, t0:t0 + n], RS[:, t0:t0 + n], r=["RS"], w=["RS"])
                    p.v("scalar_tensor_tensor", Y[:], Y[:], (128.0 ** -0.5) if kind == "q" else 1.0, RS[:], ALU.mult, ALU.mult, r=[yk, "RS"], w=[yk])
        p.release(mP)
        if getattr(k, "dn_stop", "") == "phase1":
            dbg = k.out
            for h in heads:
                for nm_, dd in (("q", QN), ("k", KN), ("v", VV)):
                    r0 = ({"q": 0, "k": 4, "v": 8}[nm_] + h) * 128
                    p.dma(dbg[r0:r0 + 128, :], dd[h][:, 0:D], r=[(nm_, h)], w=["dbg"])
            p.dma(dbg[1536:1664, 0:NT * 8], Gg[:].rearrange("p a b -> p (a b)"), r=["Gg"], w=["dbg"])
            p.dma(dbg[1664:1792, 0:NT * 8], BETA[:].rearrange("p a b -> p (a b)"), r=["BETA"], w=["dbg"])
            p.release(mH)
            continue
        R = 2
        pools = {}
        cnt = {}
        jobid = [0]

        def tb(kind, shape=(128, 128)):
            kind = (kind, jobid[0])
            if kind not in pools:
                pools[kind] = [p.sb(list(shape), F32, "p" + kind[0]) for _ in range(R)]
                cnt[kind] = 0
            ix = cnt[kind] % R; cnt[kind] += 1
            return pools[kind][ix], (kind, hp2, ix)

        jobs = []
        for h in heads:
            for d in range(2):
                S = p.sb([128, 128], F32, "S")
                sk = ("S", h, d)
                p.v("memset", S[:], 0.0, w=[sk])
                order = [16, 17] + list(range(16)) if d == 0 else [17, 16] + list(range(15, -1, -1))
                jobs.append((h, d, S, sk, order))

        def unit(h, d, S, sk, order, step):
            c = order[step]
            incl = masks["ut_incl" if d == 0 else "lt_incl"]; ikey = "ut_incl" if d == 0 else "lt_incl"
            strict = masks["ut_strict" if d == 0 else "lt_strict"]; skey_ = "ut_strict" if d == 0 else "lt_strict"
            last = 127 if d == 0 else 0
            cs = slice(c * 128, (c + 1) * 128)
            qT = QN[h][:, cs]; kT = KN[h][:, cs]; vT = VV[h][:, cs]
            qk = [("q", h)]; kk_ = [("k", h)]; vk = [("v", h)]
            gcol = Gg[:, c, d * 4 + h:d * 4 + h + 1]; bcol = BETA[:, c, d * 4 + h:d * 4 + h + 1]
            ktm, ktk = tb("ktm"); vtm, vtk = tb("vtm")
            c0, ks = p.getps(); p.tr(psap(k, c0, 128), kT, k.ident[:], r=kk_ + ["ident"], w=ks)
            p.act(ktm[:], psap(k, c0, 128), AF.Copy, r=ks, w=[ktk])
            c0, ks = p.getps(); p.tr(psap(k, c0, 128), vT, k.ident[:], r=vk + ["ident"], w=ks)
            p.act(vtm[:], psap(k, c0, 128), AF.Copy, r=ks, w=[vtk])
            yield
            sm, smk = tb("sm", (128, 8))
            c0, ks = p.getps(); p.mm(psap(k, c0, 8), incl[:], Gg[:, c, :], r=[ikey, "Gg"], w=ks)
            p.v("tensor_copy", sm[:, 0:1], psap(k, c0 + d * 4 + h, 1), r=ks, w=[smk])
            Gt, Gtk = tb("Gt"); p.act(Gt[:], incl[:], AF.Identity, scale=gcol, r=[ikey, "Gg"], w=[Gtk])
            Bd, Bdk = tb("Bd"); p.act(Bd[:], k.ident[:], AF.Identity, scale=bcol, r=["ident", "BETA"], w=[Bdk])
            cB, kB = p.getps(); p.mm(psap(k, cB, 128), k.ones[:], Bd[:], r=["ones", Bdk], w=kB)
            BR, BRk = tb("BR"); p.act(BR[:], psap(k, cB, 128), AF.Copy, r=kB, w=[BRk])
            cR, kR = p.getps(); p.mm(psap(k, cR, 128), k.ones[:], Gt[:], r=["ones", Gtk], w=kR)
            D1, D1k = tb("D1"); p.v("tensor_scalar", D1[:], psap(k, cR, 128), sm[:, 0:1], 0.0, ALU.subtract, ALU.min, r=kR + [smk], w=[D1k])
            egr, egrk = tb("egr"); p.act(egr[:], psap(k, cR, 128), AF.Exp, r=kR, w=[egrk])
            p.act(sm[:, 2:3], psap(k, cR + last, 1), AF.Exp, r=kR, w=[smk])
            p.v("tensor_scalar", sm[:, 3:4], psap(k, cR + last, 1), sm[:, 0:1], None, ALU.subtract, r=kR + [smk], w=[smk])
            p.act(sm[:, 3:4], sm[:, 3:4], AF.Exp, r=[smk], w=[smk])
            p.act(sm[:, 1:2], sm[:, 0:1], AF.Exp, r=[smk], w=[smk])
            E, Ek = tb("E"); p.act(E[:], D1[:], AF.Exp, r=[D1k], w=[Ek])
            DTm, DTmk = tb("DTm"); p.v("tensor_tensor", DTm[:], E[:], incl[:], ALU.mult, r=[Ek, ikey], w=[DTmk])
            DTs, DTsk = tb("DTs"); p.v("tensor_tensor", DTs[:], E[:], strict[:], ALU.mult, r=[Ek, skey_], w=[DTsk])
            yield
            cG, kG = p.getps(); p.mm(psap(k, cG, 128), kT, kT, r=kk_, w=kG)
            N1, N1k = tb("N1"); p.v("tensor_tensor", N1[:], psap(k, cG, 128), DTs[:], ALU.mult, r=kG + [DTsk], w=[N1k])
            cA, kA = p.getps(); p.mm(psap(k, cA, 128), kT, qT, r=kk_ + qk, w=kA)
            ATt, ATk = tb("ATt"); p.v("tensor_tensor", ATt[:], psap(k, cA, 128), DTm[:], ALU.mult, r=kA + [DTmk], w=[ATk])
            Nl, Nlk = tb("Na"); p.v("tensor_tensor", Nl[:], N1[:], BR[:], ALU.mult, r=[BRk, N1k], w=[Nlk])
            yield
            Ml, Mlk = tb("Ma")
            c0, ks = p.getps(); p.tr(psap(k, c0, 128), Nl[:], k.ident[:], r=[Nlk, "ident"], w=ks)
            p.act(Ml[:], psap(k, c0, 128), AF.Copy, r=ks, w=[Mlk])
            yield
            y, yk2 = tb("ya", (128, 256))
            p.act(y[:, 0:128], vtm[:], AF.Identity, scale=bcol, r=[vtk, "BETA"], w=[yk2])
            p.v("tensor_tensor", sm[:, 4:5], sm[:, 1:2], bcol, ALU.mult, r=[smk, "BETA"], w=[smk])
            p.act(y[:, 128:256], ktm[:], AF.Identity, scale=sm[:, 4:5], r=[ktk, smk], w=[yk2])
            for lev in range(7):
                yield
                c0, ks = p.getps()
                p.mm(psap(k, c0, 256), Nl[:], y[:], r=[Nlk, yk2], w=ks)
                y2, y2k = tb("yb" if lev % 2 == 0 else "ya", (128, 256))
                p.v("tensor_tensor", y2[:], y[:], psap(k, c0, 256), ALU.subtract if lev == 0 else ALU.add, r=ks + [yk2], w=[y2k])
                y, yk2 = y2, y2k
                if lev < 6:
                    N2, N2k = tb("Nb" if lev % 2 == 0 else "Na")
                    c0, ks = p.getps(); p.mm(psap(k, c0, 128), Ml[:], Nl[:], r=[Mlk, Nlk], w=ks)
                    p.act(N2[:], psap(k, c0, 128), AF.Copy, r=ks, w=[N2k])
                    if lev < 5:
                        M2, M2k = tb("Mb" if lev % 2 == 0 else "Ma")
                        c0, ks = p.getps(); p.mm(psap(k, c0, 128), Nl[:], Ml[:], r=[Mlk, Nlk], w=ks)
                        p.act(M2[:], psap(k, c0, 128), AF.Copy, r=ks, w=[M2k])
                        Ml, Mlk = M2, M2k
                    Nl, Nlk = N2, N2k
            yield
            wT, wTk = tb("wT")
            c0, ks = p.getps(); p.tr(psap(k, c0, 128), y[:, 128:256], k.ident[:], r=[yk2, "ident"], w=ks)
            p.act(wT[:], psap(k, c0, 128), AF.Copy, r=ks, w=[wTk])
            yield
            qg, qgk = tb("qg"); p.v("tensor_tensor", qg[:], qT, egr[:], ALU.mult, r=qk + [egrk], w=[qgk])
            kd, kdk = tb("kd"); p.act(kd[:], ktm[:], AF.Identity, scale=sm[:, 3:4], r=[ktk, smk], w=[kdk])
            yield
            c0, ks = p.getps(); p.mm(psap(k, c0, 128), wT[:], S[:], r=[wTk, sk], w=ks)
            vn, vnk = tb("vn"); p.v("tensor_tensor", vn[:], y[:, 0:128], psap(k, c0, 128), ALU.subtract, r=ks + [yk2], w=[vnk])
            cO, kO = p.getps()
            p.mm(psap(k, cO, 128), qg[:], S[:], start=True, stop=False, r=[qgk, sk], w=kO)
            p.mm(psap(k, cO, 128), ATt[:], vn[:], start=False, stop=True, r=[ATk, vnk], w=kO)
            cS, kS_ = p.getps(); p.mm(psap(k, cS, 128), kd[:], vn[:], r=[kdk, vnk], w=kS_)
            p.v("scalar_tensor_tensor", S[:], S[:], sm[:, 2:3], psap(k, cS, 128), ALU.mult, ALU.add, r=[sk, smk] + kS_, w=[sk])
            okey = ("OB", c, h)
            if (c, h) not in written:
                written.add((c, h))
                p.act(OB[:, c, h, :], psap(k, cO, 128), AF.Copy, r=kO, w=[okey])
            else:
                p.v("tensor_tensor", OB[:, c, h, :], OB[:, c, h, :], psap(k, cO, 128), ALU.add, r=kO + [okey], w=[okey])

        for step in range(getattr(k, "dn_steps", NT)):
            gens = []
            for ji, job in enumerate(jobs):
                gens.append((ji, unit(*job, step)))
            while gens:
                for (ji, g) in list(gens):
                    jobid[0] = ji
                    try:
                        next(g)
                    except StopIteration:
                        gens.remove((ji, g))
        p.release(mH)
    if getattr(k, "dn_stop", "") != "":
        p.release(m)
        return
    NW = p.sb([128, 4, 128], F32, "NW")
    for h in range(4):
        p.dma(NW[:, h, :], k.inp["dn_norm_w"][i:i + 1, :].broadcast_to([128, 128]), w=["NW"])
    zt = [p.sb([128, 512], F32, "zt") for _ in range(2)]
    on = [p.sb([128, 4, 128], F32, "on") for _ in range(2)]
    junk = p.sb([128, 128], F32, "junk")
    ss = [p.sb([128, 4], F32, "ss4") for _ in range(2)]
    for tt in range(nq):
        u = tt % 2
        p.dma(zt[u][:], k.Zd[tt * 128:(tt + 1) * 128, :], r=[("Zd", tt)], w=[("zt", u)])
        p.act(zt[u][:], zt[u][:], AF.Silu, r=[("zt", u)], w=[("zt", u)])
        for h in range(4):
            p.act(junk[:], OB[:, tt, h, :], AF.Square, accum_out=ss[u][:, h:h + 1], r=[("OB", tt, h)], w=["junk", ("ss4", u)])
        p.v("tensor_scalar", ss[u][:], ss[u][:], 1.0 / 128, RMS_EPS, ALU.mult, ALU.add, r=[("ss4", u)], w=[("ss4", u)])
        p.act(ss[u][:], ss[u][:], AF.Sqrt, r=[("ss4", u)], w=[("ss4", u)])
        p.v("reciprocal", ss[u][:], ss[u][:], r=[("ss4", u)], w=[("ss4", u)])
        for h in range(4):
            p.v("tensor_scalar_mul", on[u][:, h, :], OB[:, tt, h, :], ss[u][:, h:h + 1], r=[("OB", tt, h), ("ss4", u)], w=[("on", u)])
        p.v("tensor_tensor", on[u][:], on[u][:], NW[:], ALU.mult, r=[("on", u), "NW"], w=[("on", u)])
        p.v("tensor_tensor", on[u][:].rearrange("p a b -> p (a b)"), on[u][:].rearrange("p a b -> p (a b)"), zt[u][:], ALU.mult,
            r=[("on", u), ("zt", u)], w=[("on", u)])
        p.dma(k.MIXd[tt * 128:(tt + 1) * 128, 512:1024], on[u][:].rearrange("p a b -> p (a b)"), r=[("on", u)], w=[("MIXd", tt)], q="act")
    p.release(m)


from concourse.bass_utils import run_bass_kernel_spmd

N_CORES = 8


def build_program(nc, layers=(0, 1, 2, 3)):
    k = setup(nc)
    p = k.p
    stage_init(k)
    for l in layers:
        last = l == 3
        stage_mod(k, l)
        if l % 2 == 0:
            stage_mixer_ab(k, l, not last)
        else:
            stage_mixer_c(k, l, not last)
        nt = NLT if last else NT
        m = p.mark()
        X2T = p.sb([128, 8, T], BF16, "X2T"); COMB = p.sb([128, NT, 32], F32, "COMB")
        stage_outproj(k, l, k.inp["w_out_c" if l % 2 else "w_out_ab"][l // 2], nt, X2T, COMB)
        stage_moe(k, l, nt, X2T, COMB, k.out, last)
        p.release(m)
    p.finish()
    p.emit()
    return k


_CACHE = {}


def kernel(**inputs):
    consts = host_consts()
    nc = bass.Bass("TRN2", target_bir_lowering=False)
    build_program(nc)
    shared = {}
    for n in IN_SHAPES:
        if n in ("x", "ctx", "c", "c_ctx"):
            continue
        shared[n] = np.ascontiguousarray(np.asarray(inputs[n], dtype=np.float32))
    for n, v in consts.items():
        shared["k_" + n] = np.ascontiguousarray(v)
    x = np.asarray(inputs["x"], dtype=np.float32); ctx = np.asarray(inputs["ctx"], dtype=np.float32)
    c = np.asarray(inputs["c"], dtype=np.float32); cc = np.asarray(inputs["c_ctx"], dtype=np.float32)
    in_maps = []
    for b in range(N_CORES):
        m = dict(shared)
        m["x"] = np.ascontiguousarray(x[b]); m["ctx"] = np.ascontiguousarray(ctx[b])
        m["c"] = np.ascontiguousarray(c[b:b + 1]); m["c_ctx"] = np.ascontiguousarray(cc[None, :])
        in_maps.append(m)
    res = run_bass_kernel_spmd(nc, in_maps, core_ids=list(range(N_CORES)))
    return np.stack([np.asarray(r["out"], dtype=np.float32) for r in res.results], 0)
```

```python
import numpy as np
import concourse.bass as bass
import concourse.mybir as mybir

F32 = mybir.dt.float32
BF16 = mybir.dt.bfloat16
I32 = mybir.dt.int32
ALU = mybir.AluOpType
AF = mybir.ActivationFunctionType
AX = mybir.AxisListType

ENGS = ["pe", "act", "dve", "pool", "sp"]
NSLOT = 8


class Prog:
    def __init__(self, nc):
        self.nc = nc
        self.ops = {e: [] for e in ENGS}
        self.last_w = {}
        self.readers = {}
        self.known = {e: {} for e in ENGS}
        self.ndma = {e: 0 for e in ENGS}
        self.sb_off = 16512
        self.sb_hi = 16512
        self.uid = 0
        self.psrr = 0
        self.epoch = 0
        self.epoch_start = {e: 0 for e in ENGS}

    def sb(self, shape, dtype=F32, name=None):
        self.uid += 1
        nm = f"{name or 't'}_{self.uid}"
        esz = 2 if dtype == BF16 else 4
        nbytes = int(np.prod(shape[1:])) * esz
        off = (self.sb_off + 63) // 64 * 64
        assert off + nbytes <= 229344, f"SBUF overflow {off + nbytes} for {nm}"
        t = self.nc.alloc_sbuf_tensor_at(nm, list(shape), dtype, offset=off)
        self.sb_off = off + nbytes
        self.sb_hi = max(self.sb_hi, self.sb_off)
        return t

    def mark(self):
        return self.sb_off

    def release(self, mark):
        self.barrier()
        self.sb_off = mark

    def dram(self, shape, dtype=F32, name=None):
        self.uid += 1
        return self.nc.dram_tensor(f"{name or 'd'}_{self.uid}", list(shape), dtype, kind="Internal")

    def _need(self, eng, ev):
        stream, val = ev
        k = self.known[eng]
        if k.get(stream, -1) >= val:
            return False
        k[stream] = val
        return True

    def op(self, eng, fn, r=(), w=(), dma=False):
        waits = []
        deps = []
        for key in r:
            ev = self.last_w.get(key)
            if ev is not None:
                deps.append(ev)
        for key in w:
            ev = self.last_w.get(key)
            if ev is not None:
                deps.append(ev)
            deps.extend(self.readers.get(key, ()))
        idx = len(self.ops[eng])
        if dma:
            n = self.ndma[eng]
            self.ndma[eng] += 1
            slot, cnt = n % NSLOT, n // NSLOT + 1
            myev = (("d", eng, slot), cnt)
            if cnt > 1:
                deps.append((("d", eng, slot), cnt - 1))
        else:
            myev = (("c", eng, self.epoch), idx)
        for ev in deps:
            stream, val = ev
            if stream[:2] == ("c", eng) and eng == "pe":
                continue
            if self._need(eng, ev):
                waits.append(ev)
        self.ops[eng].append(dict(fn=fn, waits=waits, dma=myev if dma else None, ep=self.epoch))
        for key in r:
            self.readers.setdefault(key, []).append(myev)
        for key in w:
            self.last_w[key] = myev
            self.readers[key] = []
        return myev

    def barrier(self):
        evs = []
        for e in ENGS:
            li = None
            for i in range(len(self.ops[e]) - 1, -1, -1):
                o = self.ops[e][i]
                if o["fn"] is not None and o["dma"] is None:
                    li = i
                    break
            if li is not None:
                evs.append((("c", e, self.ops[e][li]["ep"]), li))
            n = self.ndma[e]
            for s in range(min(n, NSLOT)):
                last = (n - 1 - s) // NSLOT * NSLOT + s
                evs.append((("d", e, s), last // NSLOT + 1))
        for e in ENGS:
            waits = []
            for ev in evs:
                if ev[0][:2] == ("c", e):
                    if e == "pe":
                        continue
                if self._need(e, ev):
                    waits.append(ev)
            if waits:
                self.ops[e].append(dict(fn=None, waits=waits, dma=None, ep=self.epoch))
        if any(len(self.ops[e]) - self.epoch_start[e] > 25000 for e in ENGS):
            self.epoch += 1
            self.epoch_start = {e: len(self.ops[e]) for e in ENGS}

    def emit(self):
        nc = self.nc
        sig = {e: set() for e in ENGS}
        for e in ENGS:
            for o in self.ops[e]:
                for (stream, val) in o["waits"]:
                    if stream[0] == "c":
                        sig[stream[1]].add(val)
        cnt = {}
        used_ep = set()
        for e in ENGS:
            c = {}
            m = {}
            for i in range(len(self.ops[e])):
                if i in sig[e]:
                    ep = self.ops[e][i]["ep"]
                    c[ep] = c.get(ep, 0) + 1
                    assert c[ep] < 65000, "semaphore overflow"
                    m[i] = c[ep]
                    used_ep.add((e, ep))
            cnt[e] = m
        from contextlib import ExitStack
        with ExitStack() as st:
            csem = {(e, ep): st.enter_context(nc.semaphore(f"c_{e}_{ep}")) for (e, ep) in sorted(used_ep)}
            dsem = {(e, s): st.enter_context(nc.semaphore(f"d_{e}_{s}"))
                    for e in ENGS if self.ndma[e] for s in range(min(NSLOT, self.ndma[e]))}
            block = st.enter_context(nc.Block())

            def run(e, eng):
                for i, o in enumerate(self.ops[e]):
                    for (stream, val) in o["waits"]:
                        if stream[0] == "c":
                            eng.wait_ge(csem[(stream[1], stream[2])], cnt[stream[1]][val])
                        else:
                            eng.wait_ge(dsem[(stream[1], stream[2])], 16 * val)
                    if o["fn"] is None:
                        continue
                    ins = o["fn"](eng)
                    if o["dma"] is not None:
                        (_, q, slot), _c = o["dma"]
                        ins.then_inc(dsem[(q, slot)], 16)
                    elif i in sig[e]:
                        ins.then_inc(csem[(e, o["ep"])], 1)

            @block.tensor
            def _(eng):
                run("pe", eng)

            @block.scalar
            def _(eng):
                run("act", eng)

            @block.vector
            def _(eng):
                run("dve", eng)

            @block.gpsimd
            def _(eng):
                run("pool", eng)

            @block.sync
            def _(eng):
                run("sp", eng)

    def dma(self, out, in_, r=(), w=(), q="sp", **kw):
        return self.op(q, lambda e: e.dma_start(out=out, in_=in_, **kw), r=r, w=w, dma=True)

    def mm(self, out, lhsT, rhs, start=True, stop=True, r=(), w=(), **kw):
        return self.op("pe", lambda e: e.matmul(out, lhsT, rhs, start=start, stop=stop, **kw), r=r, w=w)

    def tr(self, out, in_, ident, r=(), w=()):
        return self.op("pe", lambda e: e.transpose(out, in_, ident), r=r, w=w)

    def act(self, out, in_, func, r=(), w=(), **kw):
        return self.op("act", lambda e: e.activation(out, in_, func, **kw), r=r, w=w)

    def v(self, name, *args, r=(), w=(), **kw):
        return self.op("dve", lambda e: getattr(e, name)(*args, **kw), r=r, w=w)

    def g(self, name, *args, r=(), w=(), **kw):
        return self.op("pool", lambda e: getattr(e, name)(*args, **kw), r=r, w=w)

    def a(self, name, *args, r=(), w=(), **kw):
        return self.op("act", lambda e: getattr(e, name)(*args, **kw), r=r, w=w)

    def getps(self, nb=1):
        if nb == 2:
            b = (self.psrr + 1) // 2 * 2 % 8
            self.psrr = (b + 2) % 8
            return b * 512, [("ps", b), ("ps", b + 1)]
        b = self.psrr % 8
        self.psrr = (b + 1) % 8
        return b * 512, [("ps", b)]

    def finish(self):
        self.barrier()

import numpy as np


D = 1024; NLAT = 2048; LCTX = 256; T = 2304; NT = 18; NLT = 16
ALPHA = 8.0 ** 0.25
NEG = -1.0e30
LN_EPS = 1e-5
RMS_EPS = 1e-6
SB_ = 0


def host_consts():
    c = {}
    c["ident"] = np.eye(128, dtype=np.float32)
    c["ones"] = np.ones((128, 128), np.float32)
    i = np.arange(128)
    c["ut_incl"] = (i[None, :] >= i[:, None]).astype(np.float32)
    c["ut_strict"] = (i[None, :] > i[:, None]).astype(np.float32)
    c["lt_incl"] = (i[None, :] <= i[:, None]).astype(np.float32)
    c["lt_strict"] = (i[None, :] < i[:, None]).astype(np.float32)
    mp = np.where(i[None, :] >= i[:, None], 0.0, NEG).astype(np.float32)
    mn = np.where(i[None, :] <= i[:, None], 0.0, NEG).astype(np.float32)
    z = np.zeros((128, 128), np.float32); zc = np.zeros((128, 256), np.float32)
    c["swa_int"] = np.concatenate([mp, z, mn, zc], 1)
    c["swa_first"] = np.concatenate([z, mn, zc], 1)
    c["swa_last"] = np.concatenate([mp, z, zc], 1)
    qc = np.arange(64); cs = np.clip(qc - 8, 0, 48)
    colin = (qc[None, :] >= cs[:, None]) & (qc[None, :] < cs[:, None] + 16)
    cm = np.where(colin, 0.0, NEG).astype(np.float32)
    c["na_cm"] = np.tile(cm, (2, 10))
    t = np.arange(NLAT)
    inv = (10000.0 ** (-np.arange(16, dtype=np.float32) / 16)).astype(np.float32)
    row = (t // 64).astype(np.float32)[:, None]; col = (t % 64).astype(np.float32)[:, None]
    ang = np.concatenate([row * inv, col * inv], -1).astype(np.float32)
    cos = np.cos(ang).astype(np.float32); sin = np.sin(ang).astype(np.float32)
    cosF = np.ones((128, T), np.float32); sinF = np.zeros((128, T), np.float32)
    for pth in range(128):
        j = pth % 64; f = j % 32
        cosF[pth, :NLAT] = cos[:, f]
        sinF[pth, :NLAT] = (-sin[:, f]) if j < 32 else sin[:, f]
    c["cosF"] = cosF; c["sinF"] = sinF
    c["cm7"] = np.full((128, 512), -7.0, np.float32)
    return c


CONST_SHAPES = {"ident": [128, 128], "ones": [128, 128], "ut_incl": [128, 128], "ut_strict": [128, 128],
                "lt_incl": [128, 128], "lt_strict": [128, 128], "swa_int": [128, 640], "swa_first": [128, 512],
                "swa_last": [128, 512], "na_cm": [128, 640], "cosF": [128, T], "sinF": [128, T], "cm7": [128, 512]}

IN_SHAPES = {
    "x": [NLAT, D], "ctx": [LCTX, D], "c": [1, D], "c_ctx": [1, D],
    "w_mod": [4, D, 6 * D], "b_mod": [4, 6 * D], "ln_g": [4, 2, D], "ln_b": [4, 2, D],
    "w_in_ab": [2, D, 3600], "na_rpb": [2, 8, 15, 31], "dn_conv": [2, 5, 1536], "dn_a_log": [2, 2, 4],
    "dn_dt_bias": [2, 2, 4], "dn_norm_w": [2, 128], "w_out_ab": [2, D, D], "w_in_c": [2, D, 1280],
    "swa_sink": [2, 16], "w_out_c": [2, D, D], "w_router": [4, D, 32], "b_router": [4, 32],
    "w_gu": [4, 32, D, 2048], "b_gu": [4, 32, 2048], "w_down": [4, 32, D, D], "b_down": [4, 32, D],
}


class Ctx:
    pass


def setup(nc, out_shape=(NLAT, D), out_name="out"):
    p = Prog(nc)
    k = Ctx()
    k.p = p; k.nc = nc; k.wl = lambda l: l
    k.inp = {n: nc.dram_tensor(n, s, F32, kind="ExternalInput").ap() for n, s in IN_SHAPES.items()}
    k.cin = {n: nc.dram_tensor("k_" + n, s, F32, kind="ExternalInput").ap() for n, s in CONST_SHAPES.items()}
    k.out = nc.dram_tensor(out_name, list(out_shape), F32, kind="ExternalOutput").ap()
    k.ps = nc.alloc_psum_tensor("psall", [128, 4096], F32)
    k.ident = p.sb([128, 128], F32, "ident"); p.dma(k.ident[:], k.cin["ident"], w=["ident"])
    k.ones = p.sb([128, 128], F32, "ones"); p.dma(k.ones[:], k.cin["ones"], w=["ones"])
    k.epsln = p.sb([128, 1], F32, "epsln"); p.v("memset", k.epsln[:], LN_EPS, w=["epsln"])
    k.Hd = p.dram([T, D], F32, "Hd").ap()
    k.MIXd = p.dram([T, D], F32, "MIXd").ap()
    k.MODROW = p.dram([2, 6 * D], F32, "MODROW").ap()
    k.MODT = p.sb([128, 2, 48], F32, "MODT")
    k.S2 = p.sb([128, 8, 2], F32, "S2")
    return k


def psap(k, col0, n, parts=128, p0=0):
    return k.ps[p0:p0 + parts, col0:col0 + n]


def stage_init(k):
    p = k.p
    m = p.mark()
    bufs = [p.sb([128, 4, D], F32, "cp") for _ in range(2)]
    xv = k.inp["x"].rearrange("(n a p) d -> n p a d", p=128, a=4)
    hv = k.Hd[0:NLAT].rearrange("(n a p) d -> n p a d", p=128, a=4)
    for n in range(4):
        b = bufs[n % 2]; key = ("cp", n % 2)
        p.dma(b[:], xv[n], w=[key])
        p.dma(hv[n], b[:], r=[key], w=[("Hd", 4 * n + a) for a in range(4)], q="act")
    b = bufs[0]; key = ("cp", 0)
    cv = k.inp["ctx"].rearrange("(a p) d -> p a d", p=128)
    p.dma(b[:, 0:2, :], cv, w=[key])
    p.dma(k.Hd[NLAT:T].rearrange("(a p) d -> p a d", p=128), b[:, 0:2, :], r=[key], w=[("Hd", 16), ("Hd", 17)], q="act")
    cs = p.sb([8, 2, 128], F32, "cs")
    p.dma(cs[:, 0, :], k.inp["c"].rearrange("o (k p) -> (o k) p", p=128), w=["cs"])
    p.dma(cs[:, 1, :], k.inp["c_ctx"].rearrange("o (k p) -> (o k) p", p=128), w=["cs"])
    cs2 = p.sb([8, 2, 128], F32, "cs2")
    p.act(cs2[:], cs[:], AF.Silu, r=["cs"], w=["cs2"])
    for s in range(2):
        c0, keys = p.getps()
        p.tr(psap(k, c0, 8), cs2[:, s, :], k.ident[0:8, 0:8], r=["cs2", "ident"], w=keys)
        p.v("tensor_copy", k.S2[:, :, s], psap(k, c0, 8), r=keys, w=["S2"])
    p.release(m)


def stage_mod(k, l):
    p = k.p
    m = p.mark()
    wbuf = [p.sb([128, 8, 512], F32, "wm") for _ in range(2)]
    rows = p.sb([2, 6 * D], F32, "modrows")
    brow = p.sb([2, 6 * D], F32, "bmodrows")
    p.dma(brow[:], k.inp["b_mod"][l:l + 1, :].broadcast_to([2, 6 * D]), w=["brow"])
    wv = k.inp["w_mod"][l].rearrange("(kc p) n -> p kc n", p=128)
    for cb in range(12):
        wb = wbuf[cb % 2]; key = ("wm", cb % 2)
        p.dma(wb[:], wv[:, :, cb * 512:(cb + 1) * 512], w=[key], q="sp" if cb % 2 == 0 else "act")
        c0, keys = p.getps()
        for kc in range(8):
            p.mm(psap(k, c0, 512, 2), k.S2[:, kc, :], wb[:, kc, :], start=(kc == 0), stop=(kc == 7),
                 r=[key, "S2"], w=keys)
        p.v("tensor_tensor", rows[:, cb * 512:(cb + 1) * 512], psap(k, c0, 512, 2), brow[:, cb * 512:(cb + 1) * 512],
            ALU.add, r=keys + ["brow"], w=["rows"])
    p.dma(k.MODROW, rows[:], r=["rows"], w=["MODROW"])
    mt = p.sb([48, 2, 128], F32, "mt")
    for s in range(2):
        p.dma(mt[:, s, :], k.MODROW[s:s + 1, :].rearrange("o (j p) -> (o j) p", p=128), r=["MODROW"], w=["mt"])
    for s in range(2):
        c0, keys = p.getps()
        p.tr(psap(k, c0, 48), mt[:, s, :], k.ident[0:48, 0:48], r=["mt", "ident"], w=keys)
        p.v("tensor_copy", k.MODT[:, s, :], psap(k, c0, 48), r=keys, w=["MODT"])
    for j0 in (8, 32):
        p.v("tensor_scalar_add", k.MODT[:, :, j0:j0 + 8], k.MODT[:, :, j0:j0 + 8], 1.0, r=["MODT"], w=["MODT"])
    p.release(m)


def load_gate(k, tile, s, j, q="sp", key=None):
    k.p.dma(tile[:], k.MODROW[s:s + 1, j * D:(j + 1) * D].broadcast_to([128, D]), r=["MODROW"], w=[key], q=q)


def stage_AT(k, AT, sh_j, sc_j, ntiles, atkey="AT", XF=None):
    p = k.p
    m = p.mark()
    hb = [p.sb([128, D], F32, "hb") for _ in range(2)]
    for tt in range(ntiles):
        b = hb[tt % 2]; key = ("hb", tt % 2)
        s = 0 if tt < NLT else 1
        p.dma(b[:], k.Hd[tt * 128:(tt + 1) * 128, :], r=[("Hd", tt)], w=[key], q="sp" if tt % 2 == 0 else "act")
        for half in range(2):
            c0, keys = p.getps()
            for q4 in range(4):
                kc = half * 4 + q4
                p.tr(psap(k, c0 + q4 * 128, 128), b[:, kc * 128:(kc + 1) * 128], k.ident[:], r=[key, "ident"], w=keys)
            for q4 in range(4):
                kc = half * 4 + q4
                p.act(AT[:, kc, tt * 128:(tt + 1) * 128], psap(k, c0 + q4 * 128, 128), AF.Identity,
                      scale=k.MODT[:, s, sc_j * 8 + kc:sc_j * 8 + kc + 1], bias=k.MODT[:, s, sh_j * 8 + kc:sh_j * 8 + kc + 1],
                      r=keys + ["MODT"], w=[(atkey, tt)])
    p.release(m)


def ln_tile(k, y, ykey, out, outkey, lng, lnb, gkeys, scratch):
    p = k.p
    st, mv, rs = scratch
    for h in range(2):
        p.v("bn_stats", st[:, h, :], y[:, h * 512:(h + 1) * 512], r=[ykey], w=["lnst"])
    p.v("bn_aggr", mv[:], st[:].rearrange("p a b -> p (a b)"), r=["lnst"], w=["lnmv"])
    p.act(rs[:], mv[:, 1:2], AF.Sqrt, bias=k.epsln[:], scale=1.0, r=["lnmv", "epsln"], w=["lnrs"])
    p.v("reciprocal", rs[:], rs[:], r=["lnrs"], w=["lnrs"])
    p.v("tensor_scalar", y[:], y[:], mv[:, 0:1], rs[:], ALU.subtract, ALU.mult, r=[ykey, "lnmv", "lnrs"], w=[ykey])
    p.v("tensor_tensor", y[:], y[:], lng[:], ALU.mult, r=[ykey] + gkeys, w=[ykey])
    p.v("tensor_tensor", out[:], y[:], lnb[:], ALU.add, r=[ykey] + gkeys, w=[outkey])


TB5 = [(0, 512), (512, 512), (1024, 512), (1536, 512), (2048, 256)]


def attn_bufs(k):
    p = k.p
    b = Ctx()
    b.S = [p.sb([128, 896], F32, "aS") for _ in range(2)]
    b.P = [p.sb([128, 896], F32, "aP") for _ in range(2)]
    b.PT = [p.sb([128, 7, 128], BF16, "aPT") for _ in range(2)]
    b.sm = [p.sb([128, 8], F32, "asm") for _ in range(2)]
    b.i = 0
    return b


def attn_unit(k, b, qT, qkeys, segs, bias, bkeys, sink, skeys, out, okey):
    p = k.p
    i = b.i % 2; b.i += 1
    S = b.S[i]; P = b.P[i]; PT = b.PT[i]; sm = b.sm[i]
    kS = ("aS", i); kP = ("aP", i); kPT = ("aPT", i); ksm = ("asm", i)
    c0, keys = p.getps(2)
    col = 0
    vts = []
    for (kT, kkeys, vlist) in segs:
        n = kT.shape[-1]
        off = 0
        while off < n:
            take = min(n - off, 512 - (col % 512))
            p.mm(psap(k, c0 + col, take), qT, kT[:, off:off + take], r=qkeys + kkeys, w=keys)
            off += take; col += take
        vts.extend(vlist)
    W = col
    nt = W // 128
    if bias is not None:
        p.v("scalar_tensor_tensor", S[:, :W], psap(k, c0, W), 0.125, bias, ALU.mult, ALU.add, r=keys + bkeys, w=[kS])
    else:
        p.v("tensor_scalar_mul", S[:, :W], psap(k, c0, W), 0.125, r=keys, w=[kS])
    p.v("reduce_max", sm[:, 0:1], S[:, :W], AX.X, r=[kS], w=[ksm])
    if sink is not None:
        p.v("tensor_tensor", sm[:, 0:1], sm[:, 0:1], sink, ALU.max, r=[ksm] + skeys, w=[ksm])
    p.v("tensor_scalar_mul", sm[:, 1:2], sm[:, 0:1], -1.0, r=[ksm], w=[ksm])
    p.act(P[:, :W], S[:, :W], AF.Exp, bias=sm[:, 1:2], scale=1.0, accum_out=sm[:, 2:3], r=[kS, ksm], w=[kP, ksm])
    if sink is not None:
        p.act(sm[:, 3:4], sink, AF.Exp, bias=sm[:, 1:2], scale=1.0, r=[ksm] + skeys, w=[ksm])
        p.v("tensor_tensor", sm[:, 2:3], sm[:, 2:3], sm[:, 3:4], ALU.add, r=[ksm], w=[ksm])
    p.v("reciprocal", sm[:, 4:5], sm[:, 2:3], r=[ksm], w=[ksm])
    c1, keys1 = p.getps(2)
    for t in range(nt):
        p.tr(psap(k, c1 + t * 128, 128), P[:, t * 128:(t + 1) * 128], k.ident[:], r=[kP, "ident"], w=keys1)
    p.act(PT[:, :nt, :], psap(k, c1, W).rearrange("p (a b) -> p a b", b=128), AF.Copy, r=keys1, w=[kPT])
    c2, keys2 = p.getps()
    for t in range(nt):
        vap, vkey = vts[t]
        p.mm(psap(k, c2, 64), PT[:, t, :], vap, start=(t == 0), stop=(t == nt - 1), r=[kPT, vkey], w=keys2)
    p.v("tensor_scalar_mul", out, psap(k, c2, 64), sm[:, 4:5], r=keys2 + [ksm], w=[okey])


def swap_cols(k, dst, src, nh, rkeys, wkeys):
    dv = dst[:].rearrange("p kc (h two j) -> p kc h two j", two=2, j=32)
    sv = src[:].rearrange("p kc (h two j) -> p kc h two j", two=2, j=32)
    for kc in range(8):
        for two in range(2):
            k.p.g("tensor_copy", dv[:, kc, :, two, :], sv[:, kc, :, 1 - two, :], r=rkeys, w=wkeys)


def stage_mixer_c(k, l, need_ctx):
    p = k.p
    i = l // 2
    nq = NT if need_ctx else NLT
    m = p.mark()
    QT = p.sb([128, 8, T], BF16, "QT"); KT = p.sb([128, 2, T], BF16, "KT"); V = p.sb([128, NT, 128], BF16, "V")
    mA = p.mark()
    AT = p.sb([128, 8, T], BF16, "AT")
    stage_AT(k, AT, 0, 1, NT)
    atk = [("AT", t) for t in range(NT)]
    Wq = p.sb([128, 8, 1024], BF16, "Wq"); Wqs = p.sb([128, 8, 1024], BF16, "Wqs")
    Wk = p.sb([128, 8, 256], BF16, "Wk"); Wks = p.sb([128, 8, 256], BF16, "Wks"); Wv = p.sb([128, 8, 128], BF16, "Wv")
    cosF = p.sb([128, T], F32, "cosF"); sinF = p.sb([128, T], F32, "sinF")
    p.dma(cosF[:], k.cin["cosF"], w=["cosF"]); p.dma(sinF[:], k.cin["sinF"], w=["sinF"], q="act")
    w = k.inp["w_in_c"][i].rearrange("(kc p) n -> p kc n", p=128)
    for h in range(2):
        p.dma(Wq[:, :, h * 512:(h + 1) * 512], w[:, :, h * 512:(h + 1) * 512], w=["Wq"], q="pool")
    for g in range(2):
        for d in range(2):
            p.dma(Wk[:, :, g * 128 + d * 64:g * 128 + d * 64 + 64], w[:, :, 1024 + g * 64:1024 + g * 64 + 64], w=["Wk"], q="pool")
    p.dma(Wv[:], w[:, :, 1152:1280], w=["Wv"], q="pool")
    swap_cols(k, Wqs, Wq, 16, ["Wq"], ["Wqs"])
    swap_cols(k, Wks, Wk, 4, ["Wk"], ["Wks"])
    t1 = [p.sb([128, 512], F32, "t1") for _ in range(2)]
    t2 = [p.sb([128, 512], F32, "t2") for _ in range(2)]
    ri = 0
    for (Wa, Wb, ka, kb, dst, dkey, nch, ntok_lim) in ((Wq, Wqs, "Wq", "Wqs", QT, "QT", 8, nq * 128), (Wk, Wks, "Wk", "Wks", KT, "KT", 2, T)):
        for j in range(nch):
            for (t0, n) in TB5:
                if t0 >= ntok_lim:
                    continue
                ak = [("AT", t0 // 128 + a) for a in range(n // 128)]
                ca, kA = p.getps(); cb, kB = p.getps()
                for kc in range(8):
                    p.mm(psap(k, ca, n), Wa[:, kc, j * 128:(j + 1) * 128], AT[:, kc, t0:t0 + n], start=(kc == 0), stop=(kc == 7), r=[ka] + ak, w=kA)
                for kc in range(8):
                    p.mm(psap(k, cb, n), Wb[:, kc, j * 128:(j + 1) * 128], AT[:, kc, t0:t0 + n], start=(kc == 0), stop=(kc == 7), r=[kb] + ak, w=kB)
                r2 = ri % 2; ri += 1
                p.v("tensor_tensor", t1[r2][:, :n], psap(k, ca, n), cosF[:, t0:t0 + n], ALU.mult, r=kA + ["cosF"], w=[("t1", r2)])
                p.v("tensor_tensor", t2[r2][:, :n], psap(k, cb, n), sinF[:, t0:t0 + n], ALU.mult, r=kB + ["sinF"], w=[("t2", r2)])
                p.v("tensor_tensor", dst[:, j, t0:t0 + n], t1[r2][:, :n], t2[r2][:, :n], ALU.add, r=[("t1", r2), ("t2", r2)], w=[(dkey, j)])
    for tt in range(NT):
        c0, keys = p.getps()
        for kc in range(8):
            p.mm(psap(k, c0, 128), AT[:, kc, tt * 128:(tt + 1) * 128], Wv[:, kc, :], start=(kc == 0), stop=(kc == 7), r=["Wv", ("AT", tt)], w=keys)
        p.act(V[:, tt, :], psap(k, c0, 128), AF.Copy, r=keys, w=[("V", tt)])
    p.release(mA)
    MI = p.sb([128, 640], F32, "MI"); MF = p.sb([128, 512], F32, "MF"); ML = p.sb([128, 512], F32, "ML")
    p.dma(MI[:], k.cin["swa_int"], w=["MI"]); p.dma(MF[:], k.cin["swa_first"], w=["MF"]); p.dma(ML[:], k.cin["swa_last"], w=["ML"])
    SK = p.sb([128, 16], F32, "SK")
    p.dma(SK[:], k.inp["swa_sink"][i:i + 1, :].broadcast_to([128, 16]), w=["SK"])
    ab = attn_bufs(k)
    O = [p.sb([128, D], F32, "O") for _ in range(2)]
    for qt in range(nq):
        oi = qt % 2
        for h in range(16):
            j = h // 2; hp = h % 2; g = h // 8
            ps_ = slice(hp * 64, hp * 64 + 64)
            qT = QT[ps_, j, qt * 128:(qt + 1) * 128]
            ctxseg = (KT[ps_, g, NLAT:T], [("KT", g)], [(V[:, 16 + a, g * 64:(g + 1) * 64], ("V", 16 + a)) for a in range(2)])
            if qt < NLT:
                lo = max(qt - 1, 0); hi = min(qt + 1, NLT - 1)
                latseg = (KT[ps_, g, lo * 128:(hi + 1) * 128], [("KT", g)], [(V[:, t, g * 64:(g + 1) * 64], ("V", t)) for t in range(lo, hi + 1)])
                segs = [latseg, ctxseg]
                if qt == 0:
                    bias, bk = MF[:, :], ["MF"]
                elif qt == NLT - 1:
                    bias, bk = ML[:, :], ["ML"]
                else:
                    bias, bk = MI[:, :], ["MI"]
            else:
                segs = [ctxseg]; bias, bk = None, []
            attn_unit(k, ab, qT, [("QT", j)], segs, bias, bk, SK[:, h:h + 1], ["SK"], O[oi][:, h * 64:(h + 1) * 64], ("O", oi))
        p.dma(k.MIXd[qt * 128:(qt + 1) * 128, :], O[oi][:], r=[("O", oi)], w=[("MIXd", qt)], q="act")
    p.release(m)


def stage_outproj(k, l, w_out, ntiles, X2T, COMB):
    p = k.p
    m = p.mark()
    Wo = p.sb([128, 8, D], BF16, "Wo")
    wv = w_out.rearrange("(kc p) n -> p kc n", p=128)
    for h in range(2):
        p.dma(Wo[:, :, h * 512:(h + 1) * 512], wv[:, :, h * 512:(h + 1) * 512], w=[("Wo", h)], q="pool")
    Wr = p.sb([128, 8, 32], F32, "Wr")
    p.dma(Wr[:], k.inp["w_router"][l].rearrange("(kc p) n -> p kc n", p=128), w=["Wr"])
    br = p.sb([128, 32], F32, "br")
    p.dma(br[:], k.inp["b_router"][l:l + 1, :].broadcast_to([128, 32]), w=["br"])
    G1 = [p.sb([128, D], F32, "G1") for _ in range(2)]
    for s in range(2):
        load_gate(k, G1[s], s, 2, q="act", key=("G1", s))
    lng = p.sb([128, D], F32, "lng"); lnb = p.sb([128, D], F32, "lnb")
    p.dma(lng[:], k.inp["ln_g"][l, 0:1, :].broadcast_to([128, D]), w=["lng"], q="act")
    p.dma(lnb[:], k.inp["ln_b"][l, 0:1, :].broadcast_to([128, D]), w=["lnb"], q="act")
    mix = [p.sb([128, D], F32, "mix") for _ in range(2)]
    hb = [p.sb([128, D], F32, "hb2") for _ in range(2)]
    MT = [p.sb([128, 8, 128], BF16, "MT") for _ in range(2)]
    y = [p.sb([128, D], F32, "y") for _ in range(2)]
    hn = [p.sb([128, D], F32, "hn") for _ in range(2)]
    XF = [p.sb([128, 8, 128], F32, "XF") for _ in range(2)]
    st = p.sb([128, 2, 6], F32, "st"); mv = p.sb([128, 2], F32, "mv"); rs = p.sb([128, 1], F32, "rs")
    lg = p.sb([128, 32], F32, "lg"); m8 = p.sb([128, 8], F32, "m8"); msk = p.sb([128, 32], F32, "msk")
    ex = p.sb([128, 32], F32, "ex"); sm = p.sb([128, 1], F32, "sm"); nm = p.sb([128, 1], F32, "nm")
    for tt in range(ntiles):
        i = tt % 2; s = 0 if tt < NLT else 1
        p.dma(mix[i][:], k.MIXd[tt * 128:(tt + 1) * 128, :], r=[("MIXd", tt)], w=[("mix", i)])
        p.dma(hb[i][:], k.Hd[tt * 128:(tt + 1) * 128, :], r=[("Hd", tt)], w=[("hb2", i)], q="act")
        for half in range(2):
            c0, keys = p.getps()
            for q4 in range(4):
                kc = half * 4 + q4
                p.tr(psap(k, c0 + q4 * 128, 128), mix[i][:, kc * 128:(kc + 1) * 128], k.ident[:], r=[("mix", i), "ident"], w=keys)
            p.act(MT[i][:, half * 4:half * 4 + 4, :], psap(k, c0, 512).rearrange("p (a b) -> p a b", b=128), AF.Copy,
                  r=keys, w=[("MT", i)])
        c0, keys = p.getps(2)
        for half in range(2):
            for kc in range(8):
                p.mm(psap(k, c0 + half * 512, 512), MT[i][:, kc, :], Wo[:, kc, half * 512:(half + 1) * 512],
                     start=(kc == 0), stop=(kc == 7), r=[("MT", i), ("Wo", half)], w=keys)
        p.v("tensor_tensor", y[i][:], psap(k, c0, D), G1[s][:], ALU.mult, r=keys + [("G1", s)], w=[("y", i)])
        p.v("scalar_tensor_tensor", y[i][:], hb[i][:], ALPHA, y[i][:], ALU.mult, ALU.add, r=[("hb2", i), ("y", i)], w=[("y", i)])
        ln_tile(k, y[i], ("y", i), hn[i], ("hn", i), lng, lnb, ["lng", "lnb"], (st, mv, rs))
        p.dma(k.Hd[tt * 128:(tt + 1) * 128, :], hn[i][:], r=[("hn", i)], w=[("Hd", tt)])
        for half in range(2):
            c0, keys = p.getps()
            for q4 in range(4):
                kc = half * 4 + q4
                p.tr(psap(k, c0 + q4 * 128, 128), hn[i][:, kc * 128:(kc + 1) * 128], k.ident[:], r=[("hn", i), "ident"], w=keys)
            for q4 in range(4):
                kc = half * 4 + q4
                p.act(XF[i][:, kc, :], psap(k, c0 + q4 * 128, 128), AF.Identity,
                      scale=k.MODT[:, s, 32 + kc:33 + kc], bias=k.MODT[:, s, 24 + kc:25 + kc],
                      r=keys + ["MODT"], w=[("XF", i)])
        p.act(X2T[:, :, tt * 128:(tt + 1) * 128], XF[i][:], AF.Copy, r=[("XF", i)], w=[("X2T", tt)])
        c0, keys = p.getps()
        for kc in range(8):
            p.mm(psap(k, c0, 32), XF[i][:, kc, :], Wr[:, kc, :], start=(kc == 0), stop=(kc == 7), r=[("XF", i), "Wr"], w=keys)
        p.v("tensor_tensor", lg[:], psap(k, c0, 32), br[:], ALU.add, r=keys + ["br"], w=["lg"])
        p.v("max", m8[:], lg[:], r=["lg"], w=["m8"])
        p.v("tensor_scalar", msk[:], lg[:], m8[:, 3:4], None, ALU.is_ge, r=["lg", "m8"], w=["msk"])
        p.v("tensor_scalar_mul", nm[:], m8[:, 0:1], -1.0, r=["m8"], w=["nm"])
        p.act(ex[:], lg[:], AF.Exp, bias=nm[:], scale=1.0, r=["lg", "nm"], w=["ex"])
        p.v("tensor_tensor", ex[:], ex[:], msk[:], ALU.mult, r=["ex", "msk"], w=["ex"])
        p.v("reduce_sum", sm[:], ex[:], AX.X, r=["ex"], w=["sm"])
        p.v("reciprocal", sm[:], sm[:], r=["sm"], w=["sm"])
        p.v("tensor_scalar_mul", COMB[:, tt, :], ex[:], sm[:], r=["ex", "sm"], w=[("COMB", tt)])
    p.release(m)


def stage_moe(k, l, ntiles, X2T, COMB, out_dram, last):
    p = k.p
    m = p.mark()
    TB = [(t0, min(4, ntiles - t0)) for t0 in range(0, ntiles, 4)]
    F = p.sb([128, ntiles, D], F32, "F")
    BGU = p.sb([128, 16, 32], F32, "BGU")
    m1 = p.mark()
    bg_raw = p.sb([32, 2048], F32, "bgraw")
    p.dma(bg_raw[:], k.inp["b_gu"][l], w=["bgraw"])
    for c4 in range(4):
        c0, keys = p.getps()
        for q in range(4):
            c = c4 * 4 + q
            p.tr(psap(k, c0 + q * 32, 32), bg_raw[:, c * 128:(c + 1) * 128], k.ident[0:32, 0:32], r=["bgraw", "ident"], w=keys)
        p.v("tensor_copy", BGU[:, c4 * 4:c4 * 4 + 4, :], psap(k, c0, 128).rearrange("p (a b) -> p a b", b=32), r=keys, w=["BGU"])
    bd = p.sb([32, D], F32, "bd")
    p.dma(bd[:], k.inp["b_down"][l], w=["bd"])
    ct = p.sb([32, 128], F32, "ct")
    for tt in range(ntiles):
        c0, keys = p.getps()
        p.tr(psap(k, c0, 128, 32), COMB[:, tt, :], k.ident[:], r=[("COMB", tt), "ident"], w=keys)
        p.v("tensor_copy", ct[:], psap(k, c0, 128, 32), r=keys, w=["ct"])
        c0, keys = p.getps(2)
        for half in range(2):
            p.mm(psap(k, c0 + half * 512, 512), ct[:], bd[:, half * 512:(half + 1) * 512], r=["ct", "bd"], w=keys)
        p.act(F[:, tt, :], psap(k, c0, D), AF.Copy, r=keys, w=[("F", tt)])
    p.release(m1)
    m2 = p.mark()
    NB = 2
    Wg = [p.sb([128, 8, 512], BF16, "Wg") for _ in range(NB)]
    Wu = [p.sb([128, 8, 512], BF16, "Wu") for _ in range(NB)]
    Wd = [p.sb([128, 4, D], BF16, "Wd") for _ in range(NB)]
    NH_ = 2
    HT = [p.sb([128, 4, 512], BF16, "HT") for _ in range(NH_)]
    NC_ = 3
    tg = [p.sb([128, 512], F32, "tg") for _ in range(NC_)]
    ts_ = [p.sb([128, 512], F32, "ts") for _ in range(NC_)]
    tu = [p.sb([128, 512], F32, "tu") for _ in range(NC_)]
    tu2 = tu
    COMB2 = p.sb([128, ntiles, 32], F32, "COMB2")
    for tt in range(ntiles):
        p.act(COMB2[:, tt, :], COMB[:, tt, :], AF.Copy, scale=1.0 / 1.702, r=[("COMB", tt)], w=[("COMB2", tt)])
    CM7 = p.sb([128, 512], F32, "CM7")
    p.dma(CM7[:], k.cin["cm7"], w=["CM7"])
    granules = [(e, hh) for e in range(32) for hh in range(2)]

    def load_granule(g):
        e, hh = granules[g]
        b = g % NB
        wgu = k.inp["w_gu"][k.wl(l), e].rearrange("(kc p) n -> p kc n", p=128)
        wdn = k.inp["w_down"][k.wl(l), e].rearrange("(j p) n -> p j n", p=128)
        p.dma(Wg[b][:], wgu[:, :, hh * 512:(hh + 1) * 512], w=[("Wg", b)], q="pool")
        p.dma(Wu[b][:], wgu[:, :, 1024 + hh * 512:1024 + (hh + 1) * 512], w=[("Wu", b)], q="pool")
        p.dma(Wd[b][:], wdn[:, hh * 4:(hh + 1) * 4, :], w=[("Wd", b)], q="pool")

    items = [(g, t0, nt) for g in range(len(granules)) for (t0, nt) in TB]
    cstate = {"ci": 0}

    def emit_gu(idx):
        g, t0, nt = items[idx]
        e, hh = granules[g]; b = g % NB
        ntok = nt * 128
        hb = idx % NH_
        xkeys = [("X2T", t0 + a) for a in range(nt)]
        pend = []

        def flush_ht(jc):
            j_, c_ = jc
            p.v("scalar_tensor_tensor", HT[hb][:, j_, :ntok], tu[c_][:, :ntok], 1.0, ts_[c_][:, :ntok], ALU.add, ALU.mult,
                r=[("tu", c_), ("ts", c_)], w=[("HT", hb, j_)])

        for j in range(4):
            cidx = hh * 4 + j
            cg, kg = p.getps(); cu, ku = p.getps()
            for kc in range(8):
                p.mm(psap(k, cg, ntok), Wg[b][:, kc, j * 128:(j + 1) * 128], X2T[:, kc, t0 * 128:t0 * 128 + ntok],
                     start=(kc == 0), stop=(kc == 7), r=[("Wg", b)] + xkeys, w=kg)
            for kc in range(8):
                p.mm(psap(k, cu, ntok), Wu[b][:, kc, j * 128:(j + 1) * 128], X2T[:, kc, t0 * 128:t0 * 128 + ntok],
                     start=(kc == 0), stop=(kc == 7), r=[("Wu", b)] + xkeys, w=ku)
            c2 = cstate["ci"] % NC_; cstate["ci"] += 1
            p.v("tensor_scalar", tg[c2][:, :ntok], psap(k, cg, ntok), BGU[:, cidx, e:e + 1], 7.0, ALU.add, ALU.min,
                r=kg + ["BGU"], w=[("tg", c2)])
            p.act(tu[c2][:, :ntok], psap(k, cu, ntok), AF.Identity, bias=BGU[:, 8 + cidx, e:e + 1], scale=1.0,
                  r=ku + ["BGU"], w=[("tu", c2)])
            p.act(ts_[c2][:, :ntok], tg[c2][:, :ntok], AF.Silu, scale=1.702, r=[("tg", c2)], w=[("ts", c2)])
            p.v("scalar_tensor_tensor", tu[c2][:, :ntok], tu[c2][:, :ntok], 7.0, CM7[:, :ntok], ALU.min, ALU.max, r=[("tu", c2), "CM7"], w=[("tu", c2)])
            pend.append((j, c2))
            if len(pend) > 1:
                flush_ht(pend.pop(0))
        while pend:
            flush_ht(pend.pop(0))

    def emit_down(idx):
        g, t0, nt = items[idx]
        e, hh = granules[g]; b = g % NB
        hb = idx % NH_
        for a in range(nt):
            tt = t0 + a
            for half in range(2):
                cy, ky = p.getps()
                for j in range(4):
                    p.mm(psap(k, cy, 512), HT[hb][:, j, a * 128:(a + 1) * 128], Wd[b][:, j, half * 512:(half + 1) * 512],
                         start=(j == 0), stop=(j == 3), r=[("HT", hb, j), ("Wd", b)], w=ky)
                p.v("scalar_tensor_tensor", F[:, tt, half * 512:(half + 1) * 512], psap(k, cy, 512), COMB2[:, tt, e:e + 1],
                    F[:, tt, half * 512:(half + 1) * 512], ALU.mult, ALU.add, r=ky + [("COMB2", tt), ("F", tt)], w=[("F", tt)])

    load_granule(0); load_granule(1)
    for idx in range(len(items)):
        g, t0, nt = items[idx]
        emit_gu(idx)
        if idx >= 1:
            emit_down(idx - 1)
        if t0 == 0 and g >= 1 and g + 1 < len(granules):
            load_granule(g + 1)
    emit_down(len(items) - 1)
    p.release(m2)
    G2 = [p.sb([128, D], F32, "G2") for _ in range(2)]
    for s in range(2):
        load_gate(k, G2[s], s, 5, q="act", key=("G2", s))
    lng = p.sb([128, D], F32, "lng2"); lnb = p.sb([128, D], F32, "lnb2")
    p.dma(lng[:], k.inp["ln_g"][l, 1:2, :].broadcast_to([128, D]), w=["lng2"], q="act")
    p.dma(lnb[:], k.inp["ln_b"][l, 1:2, :].broadcast_to([128, D]), w=["lnb2"], q="act")
    hb3 = [p.sb([128, D], F32, "hb3") for _ in range(2)]
    ho = [p.sb([128, D], F32, "ho") for _ in range(2)]
    st = p.sb([128, 2, 6], F32, "st2"); mv = p.sb([128, 2], F32, "mv2"); rs = p.sb([128, 1], F32, "rs2")
    for tt in range(ntiles):
        i = tt % 2; s = 0 if tt < NLT else 1
        p.dma(hb3[i][:], k.Hd[tt * 128:(tt + 1) * 128, :], r=[("Hd", tt)], w=[("hb3", i)])
        p.v("tensor_tensor", F[:, tt, :], F[:, tt, :], G2[s][:], ALU.mult, r=[("F", tt), ("G2", s)], w=[("F", tt)])
        p.v("scalar_tensor_tensor", F[:, tt, :], hb3[i][:], ALPHA, F[:, tt, :], ALU.mult, ALU.add, r=[("hb3", i), ("F", tt)], w=[("F", tt)])
        ln_tile(k, F[:, tt, :], ("F", tt), ho[i], ("ho", i), lng, lnb, ["lng2", "lnb2"], (st, mv, rs))
        if last:
            p.dma(out_dram[tt * 128:(tt + 1) * 128, :], ho[i][:], r=[("ho", i)], w=[("OUT", tt)], q="act")
        else:
            p.dma(k.Hd[tt * 128:(tt + 1) * 128, :], ho[i][:], r=[("ho", i)], w=[("Hd", tt)], q="act")
    p.release(m)


def na_class(qt):
    return 0 if qt == 0 else 1 if qt == 1 else 2 if qt <= 13 else 3 if qt == 14 else 4


def stage_mixer_ab(k, l, need_ctx):
    p = k.p
    i = l // 2
    nq = NT if need_ctx else NLT
    if not hasattr(k, "QKTd"):
        k.QKTd_h = p.dram([1024, T], BF16, "QKTd"); k.QKTd = k.QKTd_h.ap()
        k.Vd = p.dram([T, 512], BF16, "Vd").ap()
        k.QKVBd = p.dram([1536, T], F32, "QKVBd").ap()
        k.Zd = p.dram([T, 512], F32, "Zd").ap()
        k.ABd = p.dram([T, 16], F32, "ABd").ap()
        k.VZ_h = p.dram([120, 64, 128], F32, "VZ"); k.VZ = k.VZ_h.ap()
    m = p.mark()
    AT = p.sb([128, 8, T], BF16, "AT")
    stage_AT(k, AT, 0, 1, NT)
    W = p.sb([128, 8, 3600], BF16, "Wab")
    w = k.inp["w_in_ab"][i].rearrange("(kc p) n -> p kc n", p=128)
    for c0 in range(0, 3600, 512):
        c1 = min(c0 + 512, 3600)
        p.dma(W[:, :, c0:c1], w[:, :, c0:c1], w=["Wab"], q="pool")
    sb16 = [p.sb([128, 512], BF16, "st16") for _ in range(2)]
    sf32 = [p.sb([128, 512], F32, "st32") for _ in range(2)]
    ui = 0
    for ch in list(range(8)) + list(range(12, 24)):
        isq = ch < 8
        for (t0, n) in TB5:
            ak = [("AT", t0 // 128 + a) for a in range(n // 128)]
            c0, keys = p.getps()
            for kc in range(8):
                p.mm(psap(k, c0, n), W[:, kc, ch * 128:(ch + 1) * 128], AT[:, kc, t0:t0 + n], start=(kc == 0), stop=(kc == 7), r=["Wab"] + ak, w=keys)
            u = ui % 2; ui += 1
            if isq:
                st, sk = sb16[u], ("st16", u)
                dst, dk = k.QKTd[ch * 128:(ch + 1) * 128, t0:t0 + n], ("QKTd", ch)
            else:
                st, sk = sf32[u], ("st32", u)
                dst, dk = k.QKVBd[(ch - 12) * 128:(ch - 11) * 128, t0:t0 + n], ("QKVBd", ch - 12)
            if ui % 2:
                p.act(st[:, :n], psap(k, c0, n), AF.Copy, r=keys, w=[sk])
            else:
                p.v("tensor_copy", st[:, :n], psap(k, c0, n), r=keys, w=[sk])
            p.dma(dst, st[:, :n], r=[sk], w=[dk], q="sp" if ui % 2 else "act")
    sab = [p.sb([128, 16], F32, "stab") for _ in range(2)]
    for tt in range(NT):
        u = tt % 2
        for (cs, n, st, sk, dst, dk) in ((1024, 512, sb16[u], ("st16", u), k.Vd[tt * 128:(tt + 1) * 128, :], ("Vd", tt)),
                                         (3072, 512, sf32[u], ("st32", u), k.Zd[tt * 128:(tt + 1) * 128, :], ("Zd", tt)),
                                         (3584, 16, sab[u], ("stab", u), k.ABd[tt * 128:(tt + 1) * 128, :], ("ABd", tt))):
            c0, keys = p.getps()
            for kc in range(8):
                p.mm(psap(k, c0, n), AT[:, kc, tt * 128:(tt + 1) * 128], W[:, kc, cs:cs + n], start=(kc == 0), stop=(kc == 7), r=["Wab", ("AT", tt)], w=keys)
            p.act(st[:, :n], psap(k, c0, n), AF.Copy, r=keys, w=[sk])
            p.dma(dst, st[:, :n], r=[sk], w=[dk], q="sp")
    p.release(m)
    if getattr(k, "do_na", True):
        stage_na(k, i, nq)
    if getattr(k, "do_dn", True):
        stage_deltanet(k, i, nq)


def stage_na(k, i, nq):
    p = k.p
    m = p.mark()
    QT4 = p.sb([128, 4, T], BF16, "QT4"); KT4 = p.sb([128, 4, T], BF16, "KT4"); V = p.sb([128, NT, 512], BF16, "V")
    for j in range(4):
        p.dma(QT4[:, j, :], k.QKTd[j * 128:(j + 1) * 128, :], r=[("QKTd", j)], w=[("QT4", j)])
        p.dma(KT4[:, j, :], k.QKTd[(4 + j) * 128:(5 + j) * 128, :], r=[("QKTd", 4 + j)], w=[("KT4", j)], q="act")
    p.dma(V[:], k.Vd.rearrange("(t p) c -> p t c", p=128), r=[("Vd", t) for t in range(NT)], w=["V"])
    CM = p.sb([128, 640], F32, "CM"); p.dma(CM[:], k.cin["na_cm"], w=["CM"])
    rp = p.sb([120, 128], F32, "rp")
    p.v("memset", rp[:], 0.0, w=["rp"])
    p.dma(rp[:, 48:79], k.inp["na_rpb"][i].rearrange("h a b -> (h a) b"), w=["rp"])
    for r0 in range(0, 64, 16):
        p.dma(k.VZ[:, r0:r0 + 16, :], rp[:].unsqueeze(1).broadcast_to([120, 16, 128]), r=["rp"], w=["VZ"])
    BI = [[p.sb([128, 896], F32, "BI") for _ in range(8)] for _ in range(2)]

    def build_bias(cls):
        s = cls % 2
        qt = {0: 0, 1: 1, 2: 2, 3: 14, 4: 15}[cls]
        nk = 5 if cls == 2 else 4
        kbase = int(np.clip(2 * qt - 4, 0, 24))
        for h in range(8):
            t = BI[s][h]; key = ("BI", s, h)
            p.v("memset", t[:, 0:nk * 128], NEG, w=[key])
            p.v("memset", t[:, nk * 128:nk * 128 + 256], 0.0, w=[key])
            for qrl in range(2):
                qr = 2 * qt + qrl
                k0 = int(np.clip(qr - 4, 0, 24))
                a_start = k0 - qr + 7
                cstart = (k0 - kbase) * 64
                src = bass.AP(tensor=k.VZ_h, offset=(h * 15 + a_start) * 8192 + 63, ap=[[127, 64], [8192, 8], [1, 64]])
                dst = t[qrl * 64:(qrl + 1) * 64, cstart:cstart + 512].rearrange("p (a b) -> p a b", b=64)
                p.dma(dst, src, r=["VZ"], w=[key], q="sp" if qrl == 0 else "act")
            p.v("tensor_tensor", t[:, 0:nk * 128], t[:, 0:nk * 128], CM[:, 0:nk * 128], ALU.add, r=[key, "CM"], w=[key])

    build_bias(0); build_bias(1)
    ab = attn_bufs(k)
    O = [p.sb([128, 512], F32, "Ona") for _ in range(2)]
    for qt in range(nq):
        oi = qt % 2
        if qt == 1:
            build_bias(2)
        if qt == 2:
            build_bias(3)
        if qt == 14:
            build_bias(4)
        for h in range(8):
            j = h // 2; hp = h % 2
            ps_ = slice(hp * 64, hp * 64 + 64)
            qT = QT4[ps_, j, qt * 128:(qt + 1) * 128]
            ctxseg = (KT4[ps_, j, NLAT:T], [("KT4", j)], [(V[:, 16 + a, h * 64:(h + 1) * 64], "V") for a in range(2)])
            if qt < NLT:
                cls = na_class(qt); nk = 5 if cls == 2 else 4
                kt0 = int(np.clip(2 * qt - 4, 0, 24)) // 2
                latseg = (KT4[ps_, j, kt0 * 128:(kt0 + nk) * 128], [("KT4", j)], [(V[:, t, h * 64:(h + 1) * 64], "V") for t in range(kt0, kt0 + nk)])
                segs = [latseg, ctxseg]
                bias, bk = BI[cls % 2][h][:, 0:nk * 128 + 256], [("BI", cls % 2, h)]
            else:
                segs = [ctxseg]; bias, bk = None, []
            attn_unit(k, ab, qT, [("QT4", j)], segs, bias, bk, None, [], O[oi][:, h * 64:(h + 1) * 64], ("Ona", oi))
        p.dma(k.MIXd[qt * 128:(qt + 1) * 128, 0:512], O[oi][:], r=[("Ona", oi)], w=[("MIXd", qt)], q="act")
    p.release(m)


def stage_deltanet(k, i, nq):
    p = k.p
    m = p.mark()
    masks = {}
    for nme in ("ut_incl", "ut_strict", "lt_incl", "lt_strict"):
        masks[nme] = p.sb([128, 128], F32, nme)
        p.dma(masks[nme][:], k.cin[nme], w=[nme])
        EPSC = p.sb([128, 1], F32, "EPSC"); p.v("memset", EPSC[:], RMS_EPS, w=["EPSC"])
    cwr = p.sb([5, 1536], F32, "cwr"); p.dma(cwr[:], k.inp["dn_conv"][i], w=["cwr"])
    CW = p.sb([128, 12, 5], F32, "CW")
    for rc in range(12):
        c0, keys = p.getps()
        p.tr(psap(k, c0, 5), cwr[:, rc * 128:(rc + 1) * 128], k.ident[0:5, 0:5], r=["cwr", "ident"], w=keys)
        p.v("tensor_copy", CW[:, rc, :], psap(k, c0, 5), r=keys, w=["CW"])
    AB = p.sb([128, NT, 16], F32, "AB")
    p.dma(AB[:], k.ABd.rearrange("(t p) c -> p t c", p=128), r=[("ABd", t) for t in range(NT)], w=["AB"])
    ALB = p.sb([128, 8], F32, "ALB"); DTB = p.sb([128, 8], F32, "DTB")
    p.dma(ALB[:], k.inp["dn_a_log"][i:i + 1].rearrange("o a b -> o (a b)").broadcast_to([128, 8]), w=["ALB"])
    p.dma(DTB[:], k.inp["dn_dt_bias"][i:i + 1].rearrange("o a b -> o (a b)").broadcast_to([128, 8]), w=["DTB"])
    p.act(ALB[:], ALB[:], AF.Exp, r=["ALB"], w=["ALB"])
    p.v("tensor_scalar_mul", ALB[:], ALB[:], -1.0, r=["ALB"], w=["ALB"])
    Gg = p.sb([128, NT, 8], F32, "Gg"); BETA = p.sb([128, NT, 8], F32, "BETA")
    tA = p.sb([128, NT, 8], F32, "tA"); tB = p.sb([128, NT, 8], F32, "tB")
    for t in range(NT):
        p.v("tensor_tensor", Gg[:, t, :], AB[:, t, 0:8], DTB[:], ALU.add, r=["AB", "DTB"], w=["Gg"])
    p.act(tA[:], Gg[:], AF.Abs, r=["Gg"], w=["tA"])
    p.act(tA[:], tA[:], AF.Exp, scale=-1.0, r=["tA"], w=["tA"])
    p.act(tA[:], tA[:], AF.Ln, bias=1.0, scale=1.0, r=["tA"], w=["tA"])
    p.v("tensor_scalar_max", tB[:], Gg[:], 0.0, r=["Gg"], w=["tB"])
    p.v("tensor_tensor", tA[:], tA[:], tB[:], ALU.add, r=["tA", "tB"], w=["tA"])
    for t in range(NT):
        p.v("tensor_tensor", Gg[:, t, :], tA[:, t, :], ALB[:], ALU.mult, r=["tA", "ALB", "Gg"], w=["Gg"])
    p.act(BETA[:], AB[:, :, 8:16], AF.Sigmoid, r=["AB"], w=["BETA"])
    OB = p.sb([128, NT, 4, 128], F32, "OB")
    written = set()
    if getattr(k, "dn_stop", "") == "gates":
        p.dma(k.out[1536:1664, 0:NT * 8], Gg[:].rearrange("p a b -> p (a b)"), r=["Gg"], w=["dbg"])
        p.dma(k.out[1664:1792, 0:NT * 8], BETA[:].rearrange("p a b -> p (a b)"), r=["BETA"], w=["dbg"])
        p.dma(k.out[0:128, 0:60], CW[:].rearrange("p a b -> p (a b)"), r=["CW"], w=["dbg"])
        p.release(m)
        return
    mH = p.mark()
    for hp2 in range(2):
        heads = [2 * hp2, 2 * hp2 + 1]
        QN = {}; KN = {}; VV = {}
        for h in heads:
            QN[h] = p.sb([128, T], F32, "QN"); KN[h] = p.sb([128, T], F32, "KN"); VV[h] = p.sb([128, T], F32, "VV")
        mP = p.mark()
        XP = [p.sb([128, 2312], F32, "XP") for _ in range(2)]
        SQ = p.sb([128, T], F32, "SQ"); RS = p.sb([128, T], F32, "RS")
        for u in range(2):
            p.v("memset", XP[u][:], 0.0, w=[("XP", u)])
        xi = 0
        for h in heads:
            for kind, dstd in (("q", QN), ("k", KN), ("v", VV)):
                rc = {"q": 0, "k": 4, "v": 8}[kind] + h
                u = xi % 2; xi += 1
                xp = XP[u]; xk = ("XP", u)
                Y = dstd[h]; yk = (kind, h)
                p.dma(xp[:, 2:2050], k.QKVBd[rc * 128:(rc + 1) * 128, 0:NLAT], r=[("QKVBd", rc)], w=[xk])
                p.dma(xp[:, 2054:2310], k.QKVBd[rc * 128:(rc + 1) * 128, NLAT:T], r=[("QKVBd", rc)], w=[xk], q="act")
                for (y0, x0, n) in ((0, 0, NLAT), (NLAT, 2052, LCTX)):
                    p.v("tensor_scalar_mul", Y[:, y0:y0 + n], xp[:, x0:x0 + n], CW[:, rc, 0:1], r=[xk, "CW"], w=[yk])
                    for kk in range(1, 5):
                        p.v("scalar_tensor_tensor", Y[:, y0:y0 + n], xp[:, x0 + kk:x0 + kk + n], CW[:, rc, kk:kk + 1], Y[:, y0:y0 + n],
                            ALU.mult, ALU.add, r=[xk, "CW", yk], w=[yk])
                p.act(Y[:], Y[:], AF.Silu, r=[yk], w=[yk])
                if kind != "v" and not getattr(k, "dn_nol2", False):
                    p.act(SQ[:], Y[:], AF.Square, r=[yk], w=["SQ"])
                    for (t0, n) in TB5:
                        c0, keys = p.getps()
                        p.mm(psap(k, c0, n), k.ones[:], SQ[:, t0:t0 + n], r=["ones", "SQ"], w=keys)
                        p.act(RS[:, t0:t0 + n], psap(k, c0, n), AF.Sqrt, bias=EPSC[:], scale=1.0, r=keys + ["EPSC"], w=["RS"])
                        p.v("reciprocal", RS[:, t0:t0 + n], RS[:, t0:t0 + n], r=["RS"], w=["RS"])
                    p.v("scalar_tensor_tensor", Y[:], Y[:], (128.0 ** -0.5) if kind == "q" else 1.0, RS[:], ALU.mult, ALU.mult, r=[yk, "RS"], w=[yk])
        p.release(mP)
        if getattr(k, "dn_stop", "") == "phase1":
            dbg = k.out
            for h in heads:
                for nm_, dd in (("q", QN), ("k", KN), ("v", VV)):
                    r0 = ({"q": 0, "k": 4, "v": 8}[nm_] + h) * 128
                    p.dma(dbg[r0:r0 + 128, :], dd[h][:, 0:D], r=[(nm_, h)], w=["dbg"])
            p.dma(dbg[1536:1664, 0:NT * 8], Gg[:].rearrange("p a b -> p (a b)"), r=["Gg"], w=["dbg"])
            p.dma(dbg[1664:1792, 0:NT * 8], BETA[:].rearrange("p a b -> p (a b)"), r=["BETA"], w=["dbg"])
            p.release(mH)
            continue
        R = 2
        pools = {}
        cnt = {}
        jobid = [0]

        def tb(kind, shape=(128, 128)):
            kind = (kind, jobid[0])
            if kind not in pools:
                pools[kind] = [p.sb(list(shape), F32, "p" + kind[0]) for _ in range(R)]
                cnt[kind] = 0
            ix = cnt[kind] % R; cnt[kind] += 1
            return pools[kind][ix], (kind, hp2, ix)

        jobs = []
        for h in heads:
            for d in range(2):
                S = p.sb([128, 128], F32, "S")
                sk = ("S", h, d)
                p.v("memset", S[:], 0.0, w=[sk])
                order = [16, 17] + list(range(16)) if d == 0 else [17, 16] + list(range(15, -1, -1))
                jobs.append((h, d, S, sk, order))

        def unit(h, d, S, sk, order, step):
            c = order[step]
            incl = masks["ut_incl" if d == 0 else "lt_incl"]; ikey = "ut_incl" if d == 0 else "lt_incl"
            strict = masks["ut_strict" if d == 0 else "lt_strict"]; skey_ = "ut_strict" if d == 0 else "lt_strict"
            last = 127 if d == 0 else 0
            cs = slice(c * 128, (c + 1) * 128)
            qT = QN[h][:, cs]; kT = KN[h][:, cs]; vT = VV[h][:, cs]
            qk = [("q", h)]; kk_ = [("k", h)]; vk = [("v", h)]
            gcol = Gg[:, c, d * 4 + h:d * 4 + h + 1]; bcol = BETA[:, c, d * 4 + h:d * 4 + h + 1]
            ktm, ktk = tb("ktm"); vtm, vtk = tb("vtm")
            c0, ks = p.getps(); p.tr(psap(k, c0, 128), kT, k.ident[:], r=kk_ + ["ident"], w=ks)
            p.act(ktm[:], psap(k, c0, 128), AF.Copy, r=ks, w=[ktk])
            c0, ks = p.getps(); p.tr(psap(k, c0, 128), vT, k.ident[:], r=vk + ["ident"], w=ks)
            p.act(vtm[:], psap(k, c0, 128), AF.Copy, r=ks, w=[vtk])
            yield
            sm, smk = tb("sm", (128, 8))
            c0, ks = p.getps(); p.mm(psap(k, c0, 8), incl[:], Gg[:, c, :], r=[ikey, "Gg"], w=ks)
            p.v("tensor_copy", sm[:, 0:1], psap(k, c0 + d * 4 + h, 1), r=ks, w=[smk])
            Gt, Gtk = tb("Gt"); p.act(Gt[:], incl[:], AF.Identity, scale=gcol, r=[ikey, "Gg"], w=[Gtk])
            Bd, Bdk = tb("Bd"); p.act(Bd[:], k.ident[:], AF.Identity, scale=bcol, r=["ident", "BETA"], w=[Bdk])
            cB, kB = p.getps(); p.mm(psap(k, cB, 128), k.ones[:], Bd[:], r=["ones", Bdk], w=kB)
            BR, BRk = tb("BR"); p.act(BR[:], psap(k, cB, 128), AF.Copy, r=kB, w=[BRk])
            cR, kR = p.getps(); p.mm(psap(k, cR, 128), k.ones[:], Gt[:], r=["ones", Gtk], w=kR)
            D1, D1k = tb("D1"); p.v("tensor_scalar", D1[:], psap(k, cR, 128), sm[:, 0:1], 0.0, ALU.subtract, ALU.min, r=kR + [smk], w=[D1k])
            egr, egrk = tb("egr"); p.act(egr[:], psap(k, cR, 128), AF.Exp, r=kR, w=[egrk])
            p.act(sm[:, 2:3], psap(k, cR + last, 1), AF.Exp, r=kR, w=[smk])
            p.v("tensor_scalar", sm[:, 3:4], psap(k, cR + last, 1), sm[:, 0:1], None, ALU.subtract, r=kR + [smk], w=[smk])
            p.act(sm[:, 3:4], sm[:, 3:4], AF.Exp, r=[smk], w=[smk])
            p.act(sm[:, 1:2], sm[:, 0:1], AF.Exp, r=[smk], w=[smk])
            E, Ek = tb("E"); p.act(E[:], D1[:], AF.Exp, r=[D1k], w=[Ek])
            DTm, DTmk = tb("DTm"); p.v("tensor_tensor", DTm[:], E[:], incl[:], ALU.mult, r=[Ek, ikey], w=[DTmk])
            DTs, DTsk = tb("DTs"); p.v("tensor_tensor", DTs[:], E[:], strict[:], ALU.mult, r=[Ek, skey_], w=[DTsk])
            yield
            cG, kG = p.getps(); p.mm(psap(k, cG, 128), kT, kT, r=kk_, w=kG)
            N1, N1k = tb("N1"); p.v("tensor_tensor", N1[:], psap(k, cG, 128), DTs[:], ALU.mult, r=kG + [DTsk], w=[N1k])
            cA, kA = p.getps(); p.mm(psap(k, cA, 128), kT, qT, r=kk_ + qk, w=kA)
            ATt, ATk = tb("ATt"); p.v("tensor_tensor", ATt[:], psap(k, cA, 128), DTm[:], ALU.mult, r=kA + [DTmk], w=[ATk])
            Nl, Nlk = tb("Na"); p.v("tensor_tensor", Nl[:], N1[:], BR[:], ALU.mult, r=[BRk, N1k], w=[Nlk])
            yield
            Ml, Mlk = tb("Ma")
            c0, ks = p.getps(); p.tr(psap(k, c0, 128), Nl[:], k.ident[:], r=[Nlk, "ident"], w=ks)
            p.act(Ml[:], psap(k, c0, 128), AF.Copy, r=ks, w=[Mlk])
            yield
            y, yk2 = tb("ya", (128, 256))
            p.act(y[:, 0:128], vtm[:], AF.Identity, scale=bcol, r=[vtk, "BETA"], w=[yk2])
            p.v("tensor_tensor", sm[:, 4:5], sm[:, 1:2], bcol, ALU.mult, r=[smk, "BETA"], w=[smk])
            p.act(y[:, 128:256], ktm[:], AF.Identity, scale=sm[:, 4:5], r=[ktk, smk], w=[yk2])
            for lev in range(7):
                yield
                c0, ks = p.getps()
                p.mm(psap(k, c0, 256), Nl[:], y[:], r=[Nlk, yk2], w=ks)
                y2, y2k = tb("yb" if lev % 2 == 0 else "ya", (128, 256))
                p.v("tensor_tensor", y2[:], y[:], psap(k, c0, 256), ALU.subtract if lev == 0 else ALU.add, r=ks + [yk2], w=[y2k])
                y, yk2 = y2, y2k
                if lev < 6:
                    N2, N2k = tb("Nb" if lev % 2 == 0 else "Na")
                    c0, ks = p.getps(); p.mm(psap(k, c0, 128), Ml[:], Nl[:], r=[Mlk, Nlk], w=ks)
                    p.act(N2[:], psap(k, c0, 128), AF.Copy, r=ks, w=[N2k])
                    if lev < 5:
                        M2, M2k = tb("Mb" if lev % 2 == 0 else "Ma")
                        c0, ks = p.getps(); p.mm(psap(k, c0, 128), Nl[:], Ml[:], r=[Mlk, Nlk], w=ks)
                        p.act(M2[:], psap(k, c0, 128), AF.Copy, r=ks, w=[M2k])
                        Ml, Mlk = M2, M2k
                    Nl, Nlk = N2, N2k
            yield
            wT, wTk = tb("wT")
            c0, ks = p.getps(); p.tr(psap(k, c0, 128), y[:, 128:256], k.ident[:], r=[yk2, "ident"], w=ks)
            p.act(wT[:], psap(k, c0, 128), AF.Copy, r=ks, w=[wTk])
            yield
            qg, qgk = tb("qg"); p.v("tensor_tensor", qg[:], qT, egr[:], ALU.mult, r=qk + [egrk], w=[qgk])
            kd, kdk = tb("kd"); p.act(kd[:], ktm[:], AF.Identity, scale=sm[:, 3:4], r=[ktk, smk], w=[kdk])
            yield
            c0, ks = p.getps(); p.mm(psap(k, c0, 128), wT[:], S[:], r=[wTk, sk], w=ks)
            vn, vnk = tb("vn"); p.v("tensor_tensor", vn[:], y[:, 0:128], psap(k, c0, 128), ALU.subtract, r=ks + [yk2], w=[vnk])
            cO, kO = p.getps()
            p.mm(psap(k, cO, 128), qg[:], S[:], start=True, stop=False, r=[qgk, sk], w=kO)
            p.mm(psap(k, cO, 128), ATt[:], vn[:], start=False, stop=True, r=[ATk, vnk], w=kO)
            cS, kS_ = p.getps(); p.mm(psap(k, cS, 128), kd[:], vn[:], r=[kdk, vnk], w=kS_)
            p.v("scalar_tensor_tensor", S[:], S[:], sm[:, 2:3], psap(k, cS, 128), ALU.mult, ALU.add, r=[sk, smk] + kS_, w=[sk])
            okey = ("OB", c, h)
            if (c, h) not in written:
                written.add((c, h))
                p.act(OB[:, c, h, :], psap(k, cO, 128), AF.Copy, r=kO, w=[okey])
            else:
                p.v("tensor_tensor", OB[:, c, h, :], OB[:, c, h, :], psap(k, cO, 128), ALU.add, r=kO + [okey], w=[okey])

        for step in range(getattr(k, "dn_steps", NT)):
            gens = []
            for ji, job in enumerate(jobs):
                gens.append((ji, unit(*job, step)))
            while gens:
                for (ji, g) in list(gens):
                    jobid[0] = ji
                    try:
                        next(g)
                    except StopIteration:
                        gens.remove((ji, g))
        p.release(mH)
    if getattr(k, "dn_stop", "") != "":
        p.release(m)
        return
    NW = p.sb([128, 4, 128], F32, "NW")
    for h in range(4):
        p.dma(NW[:, h, :], k.inp["dn_norm_w"][i:i + 1, :].broadcast_to([128, 128]), w=["NW"])
    zt = [p.sb([128, 512], F32, "zt") for _ in range(2)]
    on = [p.sb([128, 4, 128], F32, "on") for _ in range(2)]
    junk = p.sb([128, 128], F32, "junk")
    ss = [p.sb([128, 4], F32, "ss4") for _ in range(2)]
    for tt in range(nq):
        u = tt % 2
        p.dma(zt[u][:], k.Zd[tt * 128:(tt + 1) * 128, :], r=[("Zd", tt)], w=[("zt", u)])
        p.act(zt[u][:], zt[u][:], AF.Silu, r=[("zt", u)], w=[("zt", u)])
        for h in range(4):
            p.act(junk[:], OB[:, tt, h, :], AF.Square, accum_out=ss[u][:, h:h + 1], r=[("OB", tt, h)], w=["junk", ("ss4", u)])
        p.v("tensor_scalar", ss[u][:], ss[u][:], 1.0 / 128, RMS_EPS, ALU.mult, ALU.add, r=[("ss4", u)], w=[("ss4", u)])
        p.act(ss[u][:], ss[u][:], AF.Sqrt, r=[("ss4", u)], w=[("ss4", u)])
        p.v("reciprocal", ss[u][:], ss[u][:], r=[("ss4", u)], w=[("ss4", u)])
        for h in range(4):
            p.v("tensor_scalar_mul", on[u][:, h, :], OB[:, tt, h, :], ss[u][:, h:h + 1], r=[("OB", tt, h), ("ss4", u)], w=[("on", u)])
        p.v("tensor_tensor", on[u][:], on[u][:], NW[:], ALU.mult, r=[("on", u), "NW"], w=[("on", u)])
        p.v("tensor_tensor", on[u][:].rearrange("p a b -> p (a b)"), on[u][:].rearrange("p a b -> p (a b)"), zt[u][:], ALU.mult,
            r=[("on", u), ("zt", u)], w=[("on", u)])
        p.dma(k.MIXd[tt * 128:(tt + 1) * 128, 512:1024], on[u][:].rearrange("p a b -> p (a b)"), r=[("on", u)], w=[("MIXd", tt)], q="act")
    p.release(m)


from concourse.bass_utils import run_bass_kernel_spmd

N_CORES = 8


def build_program(nc, layers=(0, 1, 2, 3)):
    k = setup(nc)
    p = k.p
    stage_init(k)
    for l in layers:
        last = l == 3
        stage_mod(k, l)
        if l % 2 == 0:
            stage_mixer_ab(k, l, not last)
        else:
            stage_mixer_c(k, l, not last)
        nt = NLT if last else NT
        m = p.mark()
        X2T = p.sb([128, 8, T], BF16, "X2T"); COMB = p.sb([128, NT, 32], F32, "COMB")
        stage_outproj(k, l, k.inp["w_out_c" if l % 2 else "w_out_ab"][l // 2], nt, X2T, COMB)
        stage_moe(k, l, nt, X2T, COMB, k.out, last)
        p.release(m)
    p.finish()
    p.emit()
    return k


_CACHE = {}


def kernel(**inputs):
    consts = host_consts()
    nc = bass.Bass("TRN2", target_bir_lowering=False)
    build_program(nc)
    shared = {}
    for n in IN_SHAPES:
        if n in ("x", "ctx", "c", "c_ctx"):
            continue
        shared[n] = np.ascontiguousarray(np.asarray(inputs[n], dtype=np.float32))
    for n, v in consts.items():
        shared["k_" + n] = np.ascontiguousarray(v)
    x = np.asarray(inputs["x"], dtype=np.float32); ctx = np.asarray(inputs["ctx"], dtype=np.float32)
    c = np.asarray(inputs["c"], dtype=np.float32); cc = np.asarray(inputs["c_ctx"], dtype=np.float32)
    in_maps = []
    for b in range(N_CORES):
        m = dict(shared)
        m["x"] = np.ascontiguousarray(x[b]); m["ctx"] = np.ascontiguousarray(ctx[b])
        m["c"] = np.ascontiguousarray(c[b:b + 1]); m["c_ctx"] = np.ascontiguousarray(cc[None, :])
        in_maps.append(m)
    res = run_bass_kernel_spmd(nc, in_maps, core_ids=list(range(N_CORES)))
    return np.stack([np.asarray(r["out"], dtype=np.float32) for r in res.results], 0)
```

```python
import numpy as np
import concourse.bass as bass
import concourse.mybir as mybir

F32 = mybir.dt.float32
BF16 = mybir.dt.bfloat16
I32 = mybir.dt.int32
ALU = mybir.AluOpType
AF = mybir.ActivationFunctionType
AX = mybir.AxisListType

ENGS = ["pe", "act", "dve", "pool", "sp"]
NSLOT = 8


class Prog:
    def __init__(self, nc):
        self.nc = nc
        self.ops = {e: [] for e in ENGS}
        self.last_w = {}
        self.readers = {}
        self.known = {e: {} for e in ENGS}
        self.ndma = {e: 0 for e in ENGS}
        self.sb_off = 16512
        self.sb_hi = 16512
        self.uid = 0
        self.psrr = 0
        self.epoch = 0
        self.epoch_start = {e: 0 for e in ENGS}

    def sb(self, shape, dtype=F32, name=None):
        self.uid += 1
        nm = f"{name or 't'}_{self.uid}"
        esz = 2 if dtype == BF16 else 4
        nbytes = int(np.prod(shape[1:])) * esz
        off = (self.sb_off + 63) // 64 * 64
        assert off + nbytes <= 229344, f"SBUF overflow {off + nbytes} for {nm}"
        t = self.nc.alloc_sbuf_tensor_at(nm, list(shape), dtype, offset=off)
        self.sb_off = off + nbytes
        self.sb_hi = max(self.sb_hi, self.sb_off)
        return t

    def mark(self):
        return self.sb_off

    def release(self, mark):
        self.barrier()
        self.sb_off = mark

    def dram(self, shape, dtype=F32, name=None):
        self.uid += 1
        return self.nc.dram_tensor(f"{name or 'd'}_{self.uid}", list(shape), dtype, kind="Internal")

    def _need(self, eng, ev):
        stream, val = ev
        k = self.known[eng]
        if k.get(stream, -1) >= val:
            return False
        k[stream] = val
        return True

    def op(self, eng, fn, r=(), w=(), dma=False):
        waits = []
        deps = []
        for key in r:
            ev = self.last_w.get(key)
            if ev is not None:
                deps.append(ev)
        for key in w:
            ev = self.last_w.get(key)
            if ev is not None:
                deps.append(ev)
            deps.extend(self.readers.get(key, ()))
        idx = len(self.ops[eng])
        if dma:
            n = self.ndma[eng]
            self.ndma[eng] += 1
            slot, cnt = n % NSLOT, n // NSLOT + 1
            myev = (("d", eng, slot), cnt)
            if cnt > 1:
                deps.append((("d", eng, slot), cnt - 1))
        else:
            myev = (("c", eng, self.epoch), idx)
        for ev in deps:
            stream, val = ev
            if stream[:2] == ("c", eng) and eng == "pe":
                continue
            if self._need(eng, ev):
                waits.append(ev)
        self.ops[eng].append(dict(fn=fn, waits=waits, dma=myev if dma else None, ep=self.epoch))
        for key in r:
            self.readers.setdefault(key, []).append(myev)
        for key in w:
            self.last_w[key] = myev
            self.readers[key] = []
        return myev

    def barrier(self):
        evs = []
        for e in ENGS:
            li = None
            for i in range(len(self.ops[e]) - 1, -1, -1):
                o = self.ops[e][i]
                if o["fn"] is not None and o["dma"] is None:
                    li = i
                    break
            if li is not None:
                evs.append((("c", e, self.ops[e][li]["ep"]), li))
            n = self.ndma[e]
            for s in range(min(n, NSLOT)):
                last = (n - 1 - s) // NSLOT * NSLOT + s
                evs.append((("d", e, s), last // NSLOT + 1))
        for e in ENGS:
            waits = []
            for ev in evs:
                if ev[0][:2] == ("c", e):
                    if e == "pe":
                        continue
                if self._need(e, ev):
                    waits.append(ev)
            if waits:
                self.ops[e].append(dict(fn=None, waits=waits, dma=None, ep=self.epoch))
        if any(len(self.ops[e]) - self.epoch_start[e] > 25000 for e in ENGS):
            self.epoch += 1
            self.epoch_start = {e: len(self.ops[e]) for e in ENGS}

    def emit(self):
        nc = self.nc
        sig = {e: set() for e in ENGS}
        for e in ENGS:
            for o in self.ops[e]:
                for (stream, val) in o["waits"]:
                    if stream[0] == "c":
                        sig[stream[1]].add(val)
        cnt = {}
        used_ep = set()
        for e in ENGS:
            c = {}
            m = {}
            for i in range(len(self.ops[e])):
                if i in sig[e]:
                    ep = self.ops[e][i]["ep"]
                    c[ep] = c.get(ep, 0) + 1
                    assert c[ep] < 65000, "semaphore overflow"
                    m[i] = c[ep]
                    used_ep.add((e, ep))
            cnt[e] = m
        from contextlib import ExitStack
        with ExitStack() as st:
            csem = {(e, ep): st.enter_context(nc.semaphore(f"c_{e}_{ep}")) for (e, ep) in sorted(used_ep)}
            dsem = {(e, s): st.enter_context(nc.semaphore(f"d_{e}_{s}"))
                    for e in ENGS if self.ndma[e] for s in range(min(NSLOT, self.ndma[e]))}
            block = st.enter_context(nc.Block())

            def run(e, eng):
                for i, o in enumerate(self.ops[e]):
                    for (stream, val) in o["waits"]:
                        if stream[0] == "c":
                            eng.wait_ge(csem[(stream[1], stream[2])], cnt[stream[1]][val])
                        else:
                            eng.wait_ge(dsem[(stream[1], stream[2])], 16 * val)
                    if o["fn"] is None:
                        continue
                    ins = o["fn"](eng)
                    if o["dma"] is not None:
                        (_, q, slot), _c = o["dma"]
                        ins.then_inc(dsem[(q, slot)], 16)
                    elif i in sig[e]:
                        ins.then_inc(csem[(e, o["ep"])], 1)

            @block.tensor
            def _(eng):
                run("pe", eng)

            @block.scalar
            def _(eng):
                run("act", eng)

            @block.vector
            def _(eng):
                run("dve", eng)

            @block.gpsimd
            def _(eng):
                run("pool", eng)

            @block.sync
            def _(eng):
                run("sp", eng)

    def dma(self, out, in_, r=(), w=(), q="sp", **kw):
        return self.op(q, lambda e: e.dma_start(out=out, in_=in_, **kw), r=r, w=w, dma=True)

    def mm(self, out, lhsT, rhs, start=True, stop=True, r=(), w=(), **kw):
        return self.op("pe", lambda e: e.matmul(out, lhsT, rhs, start=start, stop=stop, **kw), r=r, w=w)

    def tr(self, out, in_, ident, r=(), w=()):
        return self.op("pe", lambda e: e.transpose(out, in_, ident), r=r, w=w)

    def act(self, out, in_, func, r=(), w=(), **kw):
        return self.op("act", lambda e: e.activation(out, in_, func, **kw), r=r, w=w)

    def v(self, name, *args, r=(), w=(), **kw):
        return self.op("dve", lambda e: getattr(e, name)(*args, **kw), r=r, w=w)

    def g(self, name, *args, r=(), w=(), **kw):
        return self.op("pool", lambda e: getattr(e, name)(*args, **kw), r=r, w=w)

    def a(self, name, *args, r=(), w=(), **kw):
        return self.op("act", lambda e: getattr(e, name)(*args, **kw), r=r, w=w)

    def getps(self, nb=1):
        if nb == 2:
            b = (self.psrr + 1) // 2 * 2 % 8
            self.psrr = (b + 2) % 8
            return b * 512, [("ps", b), ("ps", b + 1)]
        b = self.psrr % 8
        self.psrr = (b + 1) % 8
        return b * 512, [("ps", b)]

    def finish(self):
        self.barrier()

import numpy as np


D = 1024; NLAT = 2048; LCTX = 256; T = 2304; NT = 18; NLT = 16
ALPHA = 8.0 ** 0.25
NEG = -1.0e30
LN_EPS = 1e-5
RMS_EPS = 1e-6
SB_ = 0


def host_consts():
    c = {}
    c["ident"] = np.eye(128, dtype=np.float32)
    c["ones"] = np.ones((128, 128), np.float32)
    i = np.arange(128)
    c["ut_incl"] = (i[None, :] >= i[:, None]).astype(np.float32)
    c["ut_strict"] = (i[None, :] > i[:, None]).astype(np.float32)
    c["lt_incl"] = (i[None, :] <= i[:, None]).astype(np.float32)
    c["lt_strict"] = (i[None, :] < i[:, None]).astype(np.float32)
    mp = np.where(i[None, :] >= i[:, None], 0.0, NEG).astype(np.float32)
    mn = np.where(i[None, :] <= i[:, None], 0.0, NEG).astype(np.float32)
    z = np.zeros((128, 128), np.float32); zc = np.zeros((128, 256), np.float32)
    c["swa_int"] = np.concatenate([mp, z, mn, zc], 1)
    c["swa_first"] = np.concatenate([z, mn, zc], 1)
    c["swa_last"] = np.concatenate([mp, z, zc], 1)
    qc = np.arange(64); cs = np.clip(qc - 8, 0, 48)
    colin = (qc[None, :] >= cs[:, None]) & (qc[None, :] < cs[:, None] + 16)
    cm = np.where(colin, 0.0, NEG).astype(np.float32)
    c["na_cm"] = np.tile(cm, (2, 10))
    t = np.arange(NLAT)
    inv = (10000.0 ** (-np.arange(16, dtype=np.float32) / 16)).astype(np.float32)
    row = (t // 64).astype(np.float32)[:, None]; col = (t % 64).astype(np.float32)[:, None]
    ang = np.concatenate([row * inv, col * inv], -1).astype(np.float32)
    cos = np.cos(ang).astype(np.float32); sin = np.sin(ang).astype(np.float32)
    cosF = np.ones((128, T), np.float32); sinF = np.zeros((128, T), np.float32)
    for pth in range(128):
        j = pth % 64; f = j % 32
        cosF[pth, :NLAT] = cos[:, f]
        sinF[pth, :NLAT] = (-sin[:, f]) if j < 32 else sin[:, f]
    c["cosF"] = cosF; c["sinF"] = sinF
    c["cm7"] = np.full((128, 512), -7.0, np.float32)
    return c


CONST_SHAPES = {"ident": [128, 128], "ones": [128, 128], "ut_incl": [128, 128], "ut_strict": [128, 128],
                "lt_incl": [128, 128], "lt_strict": [128, 128], "swa_int": [128, 640], "swa_first": [128, 512],
                "swa_last": [128, 512], "na_cm": [128, 640], "cosF": [128, T], "sinF": [128, T], "cm7": [128, 512]}

IN_SHAPES = {
    "x": [NLAT, D], "ctx": [LCTX, D], "c": [1, D], "c_ctx": [1, D],
    "w_mod": [4, D, 6 * D], "b_mod": [4, 6 * D], "ln_g": [4, 2, D], "ln_b": [4, 2, D],
    "w_in_ab": [2, D, 3600], "na_rpb": [2, 8, 15, 31], "dn_conv": [2, 5, 1536], "dn_a_log": [2, 2, 4],
    "dn_dt_bias": [2, 2, 4], "dn_norm_w": [2, 128], "w_out_ab": [2, D, D], "w_in_c": [2, D, 1280],
    "swa_sink": [2, 16], "w_out_c": [2, D, D], "w_router": [4, D, 32], "b_router": [4, 32],
    "w_gu": [4, 32, D, 2048], "b_gu": [4, 32, 2048], "w_down": [4, 32, D, D], "b_down": [4, 32, D],
}


class Ctx:
    pass


def setup(nc, out_shape=(NLAT, D), out_name="out"):
    p = Prog(nc)
    k = Ctx()
    k.p = p; k.nc = nc; k.wl = lambda l: l
    k.inp = {n: nc.dram_tensor(n, s, F32, kind="ExternalInput").ap() for n, s in IN_SHAPES.items()}
    k.cin = {n: nc.dram_tensor("k_" + n, s, F32, kind="ExternalInput").ap() for n, s in CONST_SHAPES.items()}
    k.out = nc.dram_tensor(out_name, list(out_shape), F32, kind="ExternalOutput").ap()
    k.ps = nc.alloc_psum_tensor("psall", [128, 4096], F32)
    k.ident = p.sb([128, 128], F32, "ident"); p.dma(k.ident[:], k.cin["ident"], w=["ident"])
    k.ones = p.sb([128, 128], F32, "ones"); p.dma(k.ones[:], k.cin["ones"], w=["ones"])
    k.epsln = p.sb([128, 1], F32, "epsln"); p.v("memset", k.epsln[:], LN_EPS, w=["epsln"])
    k.Hd = p.dram([T, D], F32, "Hd").ap()
    k.MIXd = p.dram([T, D], F32, "MIXd").ap()
    k.MODROW = p.dram([2, 6 * D], F32, "MODROW").ap()
    k.MODT = p.sb([128, 2, 48], F32, "MODT")
    k.S2 = p.sb([128, 8, 2], F32, "S2")
    return k


def psap(k, col0, n, parts=128, p0=0):
    return k.ps[p0:p0 + parts, col0:col0 + n]


def stage_init(k):
    p = k.p
    m = p.mark()
    bufs = [p.sb([128, 4, D], F32, "cp") for _ in range(2)]
    xv = k.inp["x"].rearrange("(n a p) d -> n p a d", p=128, a=4)
    hv = k.Hd[0:NLAT].rearrange("(n a p) d -> n p a d", p=128, a=4)
    for n in range(4):
        b = bufs[n % 2]; key = ("cp", n % 2)
        p.dma(b[:], xv[n], w=[key])
        p.dma(hv[n], b[:], r=[key], w=[("Hd", 4 * n + a) for a in range(4)], q="act")
    b = bufs[0]; key = ("cp", 0)
    cv = k.inp["ctx"].rearrange("(a p) d -> p a d", p=128)
    p.dma(b[:, 0:2, :], cv, w=[key])
    p.dma(k.Hd[NLAT:T].rearrange("(a p) d -> p a d", p=128), b[:, 0:2, :], r=[key], w=[("Hd", 16), ("Hd", 17)], q="act")
    cs = p.sb([8, 2, 128], F32, "cs")
    p.dma(cs[:, 0, :], k.inp["c"].rearrange("o (k p) -> (o k) p", p=128), w=["cs"])
    p.dma(cs[:, 1, :], k.inp["c_ctx"].rearrange("o (k p) -> (o k) p", p=128), w=["cs"])
    cs2 = p.sb([8, 2, 128], F32, "cs2")
    p.act(cs2[:], cs[:], AF.Silu, r=["cs"], w=["cs2"])
    for s in range(2):
        c0, keys = p.getps()
        p.tr(psap(k, c0, 8), cs2[:, s, :], k.ident[0:8, 0:8], r=["cs2", "ident"], w=keys)
        p.v("tensor_copy", k.S2[:, :, s], psap(k, c0, 8), r=keys, w=["S2"])
    p.release(m)


def stage_mod(k, l):
    p = k.p
    m = p.mark()
    wbuf = [p.sb([128, 8, 512], F32, "wm") for _ in range(2)]
    rows = p.sb([2, 6 * D], F32, "modrows")
    brow = p.sb([2, 6 * D], F32, "bmodrows")
    p.dma(brow[:], k.inp["b_mod"][l:l + 1, :].broadcast_to([2, 6 * D]), w=["brow"])
    wv = k.inp["w_mod"][l].rearrange("(kc p) n -> p kc n", p=128)
    for cb in range(12):
        wb = wbuf[cb % 2]; key = ("wm", cb % 2)
        p.dma(wb[:], wv[:, :, cb * 512:(cb + 1) * 512], w=[key], q="sp" if cb % 2 == 0 else "act")
        c0, keys = p.getps()
        for kc in range(8):
            p.mm(psap(k, c0, 512, 2), k.S2[:, kc, :], wb[:, kc, :], start=(kc == 0), stop=(kc == 7),
                 r=[key, "S2"], w=keys)
        p.v("tensor_tensor", rows[:, cb * 512:(cb + 1) * 512], psap(k, c0, 512, 2), brow[:, cb * 512:(cb + 1) * 512],
            ALU.add, r=keys + ["brow"], w=["rows"])
    p.dma(k.MODROW, rows[:], r=["rows"], w=["MODROW"])
    mt = p.sb([48, 2, 128], F32, "mt")
    for s in range(2):
        p.dma(mt[:, s, :], k.MODROW[s:s + 1, :].rearrange("o (j p) -> (o j) p", p=128), r=["MODROW"], w=["mt"])
    for s in range(2):
        c0, keys = p.getps()
        p.tr(psap(k, c0, 48), mt[:, s, :], k.ident[0:48, 0:48], r=["mt", "ident"], w=keys)
        p.v("tensor_copy", k.MODT[:, s, :], psap(k, c0, 48), r=keys, w=["MODT"])
    for j0 in (8, 32):
        p.v("tensor_scalar_add", k.MODT[:, :, j0:j0 + 8], k.MODT[:, :, j0:j0 + 8], 1.0, r=["MODT"], w=["MODT"])
    p.release(m)


def load_gate(k, tile, s, j, q="sp", key=None):
    k.p.dma(tile[:], k.MODROW[s:s + 1, j * D:(j + 1) * D].broadcast_to([128, D]), r=["MODROW"], w=[key], q=q)


def stage_AT(k, AT, sh_j, sc_j, ntiles, atkey="AT", XF=None):
    p = k.p
    m = p.mark()
    hb = [p.sb([128, D], F32, "hb") for _ in range(2)]
    for tt in range(ntiles):
        b = hb[tt % 2]; key = ("hb", tt % 2)
        s = 0 if tt < NLT else 1
        p.dma(b[:], k.Hd[tt * 128:(tt + 1) * 128, :], r=[("Hd", tt)], w=[key], q="sp" if tt % 2 == 0 else "act")
        for half in range(2):
            c0, keys = p.getps()
            for q4 in range(4):
                kc = half * 4 + q4
                p.tr(psap(k, c0 + q4 * 128, 128), b[:, kc * 128:(kc + 1) * 128], k.ident[:], r=[key, "ident"], w=keys)
            for q4 in range(4):
                kc = half * 4 + q4
                p.act(AT[:, kc, tt * 128:(tt + 1) * 128], psap(k, c0 + q4 * 128, 128), AF.Identity,
                      scale=k.MODT[:, s, sc_j * 8 + kc:sc_j * 8 + kc + 1], bias=k.MODT[:, s, sh_j * 8 + kc:sh_j * 8 + kc + 1],
                      r=keys + ["MODT"], w=[(atkey, tt)])
    p.release(m)


def ln_tile(k, y, ykey, out, outkey, lng, lnb, gkeys, scratch):
    p = k.p
    st, mv, rs = scratch
    for h in range(2):
        p.v("bn_stats", st[:, h, :], y[:, h * 512:(h + 1) * 512], r=[ykey], w=["lnst"])
    p.v("bn_aggr", mv[:], st[:].rearrange("p a b -> p (a b)"), r=["lnst"], w=["lnmv"])
    p.act(rs[:], mv[:, 1:2], AF.Sqrt, bias=k.epsln[:], scale=1.0, r=["lnmv", "epsln"], w=["lnrs"])
    p.v("reciprocal", rs[:], rs[:], r=["lnrs"], w=["lnrs"])
    p.v("tensor_scalar", y[:], y[:], mv[:, 0:1], rs[:], ALU.subtract, ALU.mult, r=[ykey, "lnmv", "lnrs"], w=[ykey])
    p.v("tensor_tensor", y[:], y[:], lng[:], ALU.mult, r=[ykey] + gkeys, w=[ykey])
    p.v("tensor_tensor", out[:], y[:], lnb[:], ALU.add, r=[ykey] + gkeys, w=[outkey])


TB5 = [(0, 512), (512, 512), (1024, 512), (1536, 512), (2048, 256)]


def attn_bufs(k):
    p = k.p
    b = Ctx()
    b.S = [p.sb([128, 896], F32, "aS") for _ in range(2)]
    b.P = [p.sb([128, 896], F32, "aP") for _ in range(2)]
    b.PT = [p.sb([128, 7, 128], BF16, "aPT") for _ in range(2)]
    b.sm = [p.sb([128, 8], F32, "asm") for _ in range(2)]
    b.i = 0
    return b


def attn_unit(k, b, qT, qkeys, segs, bias, bkeys, sink, skeys, out, okey):
    p = k.p
    i = b.i % 2; b.i += 1
    S = b.S[i]; P = b.P[i]; PT = b.PT[i]; sm = b.sm[i]
    kS = ("aS", i); kP = ("aP", i); kPT = ("aPT", i); ksm = ("asm", i)
    c0, keys = p.getps(2)
    col = 0
    vts = []
    for (kT, kkeys, vlist) in segs:
        n = kT.shape[-1]
        off = 0
        while off < n:
            take = min(n - off, 512 - (col % 512))
            p.mm(psap(k, c0 + col, take), qT, kT[:, off:off + take], r=qkeys + kkeys, w=keys)
            off += take; col += take
        vts.extend(vlist)
    W = col
    nt = W // 128
    if bias is not None:
        p.v("scalar_tensor_tensor", S[:, :W], psap(k, c0, W), 0.125, bias, ALU.mult, ALU.add, r=keys + bkeys, w=[kS])
    else:
        p.v("tensor_scalar_mul", S[:, :W], psap(k, c0, W), 0.125, r=keys, w=[kS])
    p.v("reduce_max", sm[:, 0:1], S[:, :W], AX.X, r=[kS], w=[ksm])
    if sink is not None:
        p.v("tensor_tensor", sm[:, 0:1], sm[:, 0:1], sink, ALU.max, r=[ksm] + skeys, w=[ksm])
    p.v("tensor_scalar_mul", sm[:, 1:2], sm[:, 0:1], -1.0, r=[ksm], w=[ksm])
    p.act(P[:, :W], S[:, :W], AF.Exp, bias=sm[:, 1:2], scale=1.0, accum_out=sm[:, 2:3], r=[kS, ksm], w=[kP, ksm])
    if sink is not None:
        p.act(sm[:, 3:4], sink, AF.Exp, bias=sm[:, 1:2], scale=1.0, r=[ksm] + skeys, w=[ksm])
        p.v("tensor_tensor", sm[:, 2:3], sm[:, 2:3], sm[:, 3:4], ALU.add, r=[ksm], w=[ksm])
    p.v("reciprocal", sm[:, 4:5], sm[:, 2:3], r=[ksm], w=[ksm])
    c1, keys1 = p.getps(2)
    for t in range(nt):
        p.tr(psap(k, c1 + t * 128, 128), P[:, t * 128:(t + 1) * 128], k.ident[:], r=[kP, "ident"], w=keys1)
    p.act(PT[:, :nt, :], psap(k, c1, W).rearrange("p (a b) -> p a b", b=128), AF.Copy, r=keys1, w=[kPT])
    c2, keys2 = p.getps()
    for t in range(nt):
        vap, vkey = vts[t]
        p.mm(psap(k, c2, 64), PT[:, t, :], vap, start=(t == 0), stop=(t == nt - 1), r=[kPT, vkey], w=keys2)
    p.v("tensor_scalar_mul", out, psap(k, c2, 64), sm[:, 4:5], r=keys2 + [ksm], w=[okey])


def swap_cols(k, dst, src, nh, rkeys, wkeys):
    dv = dst[:].rearrange("p kc (h two j) -> p kc h two j", two=2, j=32)
    sv = src[:].rearrange("p kc (h two j) -> p kc h two j", two=2, j=32)
    for kc in range(8):
        for two in range(2):
            k.p.g("tensor_copy", dv[:, kc, :, two, :], sv[:, kc, :, 1 - two, :], r=rkeys, w=wkeys)


def stage_mixer_c(k, l, need_ctx):
    p = k.p
    i = l // 2
    nq = NT if need_ctx else NLT
    m = p.mark()
    QT = p.sb([128, 8, T], BF16, "QT"); KT = p.sb([128, 2, T], BF16, "KT"); V = p.sb([128, NT, 128], BF16, "V")
    mA = p.mark()
    AT = p.sb([128, 8, T], BF16, "AT")
    stage_AT(k, AT, 0, 1, NT)
    atk = [("AT", t) for t in range(NT)]
    Wq = p.sb([128, 8, 1024], BF16, "Wq"); Wqs = p.sb([128, 8, 1024], BF16, "Wqs")
    Wk = p.sb([128, 8, 256], BF16, "Wk"); Wks = p.sb([128, 8, 256], BF16, "Wks"); Wv = p.sb([128, 8, 128], BF16, "Wv")
    cosF = p.sb([128, T], F32, "cosF"); sinF = p.sb([128, T], F32, "sinF")
    p.dma(cosF[:], k.cin["cosF"], w=["cosF"]); p.dma(sinF[:], k.cin["sinF"], w=["sinF"], q="act")
    w = k.inp["w_in_c"][i].rearrange("(kc p) n -> p kc n", p=128)
    for h in range(2):
        p.dma(Wq[:, :, h * 512:(h + 1) * 512], w[:, :, h * 512:(h + 1) * 512], w=["Wq"], q="pool")
    for g in range(2):
        for d in range(2):
            p.dma(Wk[:, :, g * 128 + d * 64:g * 128 + d * 64 + 64], w[:, :, 1024 + g * 64:1024 + g * 64 + 64], w=["Wk"], q="pool")
    p.dma(Wv[:], w[:, :, 1152:1280], w=["Wv"], q="pool")
    wq5 = w[:, :, 0:1024].rearrange("p kc (h two j) -> p kc h two j", two=2, j=32)
    dq5 = Wqs[:].rearrange("p kc (h two j) -> p kc h two j", two=2, j=32)
    for kc in range(8):
        for two in range(2):
            p.dma(dq5[:, kc, :, two, :], wq5[:, kc, :, 1 - two, :], w=["Wqs"], q="pool")
    for g in range(2):
        for d in range(2):
            for two in range(2):
                c_dst = g * 128 + d * 64 + two * 32
                c_src = 1024 + g * 64 + (1 - two) * 32
                p.dma(Wks[:, :, c_dst:c_dst + 32], w[:, :, c_src:c_src + 32], w=["Wks"], q="pool")
    t1 = [p.sb([128, 512], F32, "t1") for _ in range(2)]
    t2 = [p.sb([128, 512], F32, "t2") for _ in range(2)]
    ri = 0
    for (Wa, Wb, ka, kb, dst, dkey, nch, ntok_lim) in ((Wq, Wqs, "Wq", "Wqs", QT, "QT", 8, nq * 128), (Wk, Wks, "Wk", "Wks", KT, "KT", 2, T)):
        for j in range(nch):
            for (t0, n) in TB5:
                if t0 >= ntok_lim:
                    continue
                ak = [("AT", t0 // 128 + a) for a in range(n // 128)]
                ca, kA = p.getps(); cb, kB = p.getps()
                for kc in range(8):
                    p.mm(psap(k, ca, n), Wa[:, kc, j * 128:(j + 1) * 128], AT[:, kc, t0:t0 + n], start=(kc == 0), stop=(kc == 7), r=[ka] + ak, w=kA)
                for kc in range(8):
                    p.mm(psap(k, cb, n), Wb[:, kc, j * 128:(j + 1) * 128], AT[:, kc, t0:t0 + n], start=(kc == 0), stop=(kc == 7), r=[kb] + ak, w=kB)
                r2 = ri % 2; ri += 1
                p.v("tensor_tensor", t1[r2][:, :n], psap(k, ca, n), cosF[:, t0:t0 + n], ALU.mult, r=kA + ["cosF"], w=[("t1", r2)])
                p.v("tensor_tensor", t2[r2][:, :n], psap(k, cb, n), sinF[:, t0:t0 + n], ALU.mult, r=kB + ["sinF"], w=[("t2", r2)])
                p.v("tensor_tensor", dst[:, j, t0:t0 + n], t1[r2][:, :n], t2[r2][:, :n], ALU.add, r=[("t1", r2), ("t2", r2)], w=[(dkey, j)])
    for tt in range(NT):
        c0, keys = p.getps()
        for kc in range(8):
            p.mm(psap(k, c0, 128), AT[:, kc, tt * 128:(tt + 1) * 128], Wv[:, kc, :], start=(kc == 0), stop=(kc == 7), r=["Wv", ("AT", tt)], w=keys)
        p.act(V[:, tt, :], psap(k, c0, 128), AF.Copy, r=keys, w=[("V", tt)])
    p.release(mA)
    MI = p.sb([128, 640], F32, "MI"); MF = p.sb([128, 512], F32, "MF"); ML = p.sb([128, 512], F32, "ML")
    p.dma(MI[:], k.cin["swa_int"], w=["MI"]); p.dma(MF[:], k.cin["swa_first"], w=["MF"]); p.dma(ML[:], k.cin["swa_last"], w=["ML"])
    SK = p.sb([128, 16], F32, "SK")
    p.dma(SK[:], k.inp["swa_sink"][i:i + 1, :].broadcast_to([128, 16]), w=["SK"])
    ab = attn_bufs(k)
    O = [p.sb([128, D], F32, "O") for _ in range(2)]
    for qt in range(nq):
        oi = qt % 2
        for h in range(16):
            j = h // 2; hp = h % 2; g = h // 8
            ps_ = slice(hp * 64, hp * 64 + 64)
            qT = QT[ps_, j, qt * 128:(qt + 1) * 128]
            ctxseg = (KT[ps_, g, NLAT:T], [("KT", g)], [(V[:, 16 + a, g * 64:(g + 1) * 64], ("V", 16 + a)) for a in range(2)])
            if qt < NLT:
                lo = max(qt - 1, 0); hi = min(qt + 1, NLT - 1)
                latseg = (KT[ps_, g, lo * 128:(hi + 1) * 128], [("KT", g)], [(V[:, t, g * 64:(g + 1) * 64], ("V", t)) for t in range(lo, hi + 1)])
                segs = [latseg, ctxseg]
                if qt == 0:
                    bias, bk = MF[:, :], ["MF"]
                elif qt == NLT - 1:
                    bias, bk = ML[:, :], ["ML"]
                else:
                    bias, bk = MI[:, :], ["MI"]
            else:
                segs = [ctxseg]; bias, bk = None, []
            attn_unit(k, ab, qT, [("QT", j)], segs, bias, bk, SK[:, h:h + 1], ["SK"], O[oi][:, h * 64:(h + 1) * 64], ("O", oi))
        p.dma(k.MIXd[qt * 128:(qt + 1) * 128, :], O[oi][:], r=[("O", oi)], w=[("MIXd", qt)], q="act")
    p.release(m)


def stage_outproj(k, l, w_out, ntiles, X2T, COMB):
    p = k.p
    m = p.mark()
    Wo = p.sb([128, 8, D], BF16, "Wo")
    wv = w_out.rearrange("(kc p) n -> p kc n", p=128)
    for h in range(2):
        p.dma(Wo[:, :, h * 512:(h + 1) * 512], wv[:, :, h * 512:(h + 1) * 512], w=[("Wo", h)], q="pool")
    Wr = p.sb([128, 8, 32], F32, "Wr")
    p.dma(Wr[:], k.inp["w_router"][l].rearrange("(kc p) n -> p kc n", p=128), w=["Wr"])
    br = p.sb([128, 32], F32, "br")
    p.dma(br[:], k.inp["b_router"][l:l + 1, :].broadcast_to([128, 32]), w=["br"])
    G1 = [p.sb([128, D], F32, "G1") for _ in range(2)]
    for s in range(2):
        load_gate(k, G1[s], s, 2, q="act", key=("G1", s))
    lng = p.sb([128, D], F32, "lng"); lnb = p.sb([128, D], F32, "lnb")
    p.dma(lng[:], k.inp["ln_g"][l, 0:1, :].broadcast_to([128, D]), w=["lng"], q="act")
    p.dma(lnb[:], k.inp["ln_b"][l, 0:1, :].broadcast_to([128, D]), w=["lnb"], q="act")
    mix = [p.sb([128, D], F32, "mix") for _ in range(2)]
    hb = [p.sb([128, D], F32, "hb2") for _ in range(2)]
    MT = [p.sb([128, 8, 128], BF16, "MT") for _ in range(2)]
    y = [p.sb([128, D], F32, "y") for _ in range(2)]
    hn = [p.sb([128, D], F32, "hn") for _ in range(2)]
    XF = [p.sb([128, 8, 128], F32, "XF") for _ in range(2)]
    st = p.sb([128, 2, 6], F32, "st"); mv = p.sb([128, 2], F32, "mv"); rs = p.sb([128, 1], F32, "rs")
    lg = p.sb([128, 32], F32, "lg"); m8 = p.sb([128, 8], F32, "m8"); msk = p.sb([128, 32], F32, "msk")
    ex = p.sb([128, 32], F32, "ex"); sm = p.sb([128, 1], F32, "sm"); nm = p.sb([128, 1], F32, "nm")
    for tt in range(ntiles):
        i = tt % 2; s = 0 if tt < NLT else 1
        p.dma(mix[i][:], k.MIXd[tt * 128:(tt + 1) * 128, :], r=[("MIXd", tt)], w=[("mix", i)])
        p.dma(hb[i][:], k.Hd[tt * 128:(tt + 1) * 128, :], r=[("Hd", tt)], w=[("hb2", i)], q="act")
        for half in range(2):
            c0, keys = p.getps()
            for q4 in range(4):
                kc = half * 4 + q4
                p.tr(psap(k, c0 + q4 * 128, 128), mix[i][:, kc * 128:(kc + 1) * 128], k.ident[:], r=[("mix", i), "ident"], w=keys)
            p.act(MT[i][:, half * 4:half * 4 + 4, :], psap(k, c0, 512).rearrange("p (a b) -> p a b", b=128), AF.Copy,
                  r=keys, w=[("MT", i)])
        c0, keys = p.getps(2)
        for half in range(2):
            for kc in range(8):
                p.mm(psap(k, c0 + half * 512, 512), MT[i][:, kc, :], Wo[:, kc, half * 512:(half + 1) * 512],
                     start=(kc == 0), stop=(kc == 7), r=[("MT", i), ("Wo", half)], w=keys)
        p.v("tensor_tensor", y[i][:], psap(k, c0, D), G1[s][:], ALU.mult, r=keys + [("G1", s)], w=[("y", i)])
        p.v("scalar_tensor_tensor", y[i][:], hb[i][:], ALPHA, y[i][:], ALU.mult, ALU.add, r=[("hb2", i), ("y", i)], w=[("y", i)])
        ln_tile(k, y[i], ("y", i), hn[i], ("hn", i), lng, lnb, ["lng", "lnb"], (st, mv, rs))
        p.dma(k.Hd[tt * 128:(tt + 1) * 128, :], hn[i][:], r=[("hn", i)], w=[("Hd", tt)])
        for half in range(2):
            c0, keys = p.getps()
            for q4 in range(4):
                kc = half * 4 + q4
                p.tr(psap(k, c0 + q4 * 128, 128), hn[i][:, kc * 128:(kc + 1) * 128], k.ident[:], r=[("hn", i), "ident"], w=keys)
            for q4 in range(4):
                kc = half * 4 + q4
                p.act(XF[i][:, kc, :], psap(k, c0 + q4 * 128, 128), AF.Identity,
                      scale=k.MODT[:, s, 32 + kc:33 + kc], bias=k.MODT[:, s, 24 + kc:25 + kc],
                      r=keys + ["MODT"], w=[("XF", i)])
        p.act(X2T[:, :, tt * 128:(tt + 1) * 128], XF[i][:], AF.Copy, r=[("XF", i)], w=[("X2T", tt)])
        c0, keys = p.getps()
        for kc in range(8):
            p.mm(psap(k, c0, 32), XF[i][:, kc, :], Wr[:, kc, :], start=(kc == 0), stop=(kc == 7), r=[("XF", i), "Wr"], w=keys)
        p.v("tensor_tensor", lg[:], psap(k, c0, 32), br[:], ALU.add, r=keys + ["br"], w=["lg"])
        p.v("max", m8[:], lg[:], r=["lg"], w=["m8"])
        p.v("tensor_scalar", msk[:], lg[:], m8[:, 3:4], None, ALU.is_ge, r=["lg", "m8"], w=["msk"])
        p.v("tensor_scalar_mul", nm[:], m8[:, 0:1], -1.0, r=["m8"], w=["nm"])
        p.act(ex[:], lg[:], AF.Exp, bias=nm[:], scale=1.0, r=["lg", "nm"], w=["ex"])
        p.v("tensor_tensor", ex[:], ex[:], msk[:], ALU.mult, r=["ex", "msk"], w=["ex"])
        p.v("reduce_sum", sm[:], ex[:], AX.X, r=["ex"], w=["sm"])
        p.v("reciprocal", sm[:], sm[:], r=["sm"], w=["sm"])
        p.v("tensor_scalar_mul", COMB[:, tt, :], ex[:], sm[:], r=["ex", "sm"], w=[("COMB", tt)])
    p.release(m)


def stage_moe(k, l, ntiles, X2T, COMB, out_dram, last):
    p = k.p
    m = p.mark()
    TB = [(t0, min(4, ntiles - t0)) for t0 in range(0, ntiles, 4)]
    F = p.sb([128, ntiles, D], F32, "F")
    BGU = p.sb([128, 16, 32], F32, "BGU")
    m1 = p.mark()
    bg_raw = p.sb([32, 2048], F32, "bgraw")
    p.dma(bg_raw[:], k.inp["b_gu"][l], w=["bgraw"])
    for c4 in range(4):
        c0, keys = p.getps()
        for q in range(4):
            c = c4 * 4 + q
            p.tr(psap(k, c0 + q * 32, 32), bg_raw[:, c * 128:(c + 1) * 128], k.ident[0:32, 0:32], r=["bgraw", "ident"], w=keys)
        p.v("tensor_copy", BGU[:, c4 * 4:c4 * 4 + 4, :], psap(k, c0, 128).rearrange("p (a b) -> p a b", b=32), r=keys, w=["BGU"])
    bd = p.sb([32, D], F32, "bd")
    p.dma(bd[:], k.inp["b_down"][l], w=["bd"])
    ct = p.sb([32, 128], F32, "ct")
    for tt in range(ntiles):
        c0, keys = p.getps()
        p.tr(psap(k, c0, 128, 32), COMB[:, tt, :], k.ident[:], r=[("COMB", tt), "ident"], w=keys)
        p.v("tensor_copy", ct[:], psap(k, c0, 128, 32), r=keys, w=["ct"])
        c0, keys = p.getps(2)
        for half in range(2):
            p.mm(psap(k, c0 + half * 512, 512), ct[:], bd[:, half * 512:(half + 1) * 512], r=["ct", "bd"], w=keys)
        p.act(F[:, tt, :], psap(k, c0, D), AF.Copy, r=keys, w=[("F", tt)])
    p.release(m1)
    m2 = p.mark()
    NB = 2
    Wg = [p.sb([128, 8, 512], BF16, "Wg") for _ in range(NB)]
    Wu = [p.sb([128, 8, 512], BF16, "Wu") for _ in range(NB)]
    Wd = [p.sb([128, 4, D], BF16, "Wd") for _ in range(NB)]
    NH_ = 2
    HT = [p.sb([128, 4, 512], BF16, "HT") for _ in range(NH_)]
    NC_ = 3
    tg = [p.sb([128, 512], F32, "tg") for _ in range(NC_)]
    ts_ = [p.sb([128, 512], F32, "ts") for _ in range(NC_)]
    tu = [p.sb([128, 512], F32, "tu") for _ in range(NC_)]
    tu2 = tu
    COMB2 = p.sb([128, ntiles, 32], F32, "COMB2")
    for tt in range(ntiles):
        p.act(COMB2[:, tt, :], COMB[:, tt, :], AF.Copy, scale=1.0 / 1.702, r=[("COMB", tt)], w=[("COMB2", tt)])
    CM7 = p.sb([128, 512], F32, "CM7")
    p.dma(CM7[:], k.cin["cm7"], w=["CM7"])
    granules = [(e, hh) for e in range(32) for hh in range(2)]

    def load_granule(g):
        e, hh = granules[g]
        b = g % NB
        wgu = k.inp["w_gu"][k.wl(l), e].rearrange("(kc p) n -> p kc n", p=128)
        wdn = k.inp["w_down"][k.wl(l), e].rearrange("(j p) n -> p j n", p=128)
        p.dma(Wg[b][:], wgu[:, :, hh * 512:(hh + 1) * 512], w=[("Wg", b)], q="pool")
        p.dma(Wu[b][:], wgu[:, :, 1024 + hh * 512:1024 + (hh + 1) * 512], w=[("Wu", b)], q="pool")
        p.dma(Wd[b][:], wdn[:, hh * 4:(hh + 1) * 4, :], w=[("Wd", b)], q="pool")

    items = [(g, t0, nt) for g in range(len(granules)) for (t0, nt) in TB]
    cstate = {"ci": 0}

    def emit_gu(idx):
        g, t0, nt = items[idx]
        e, hh = granules[g]; b = g % NB
        ntok = nt * 128
        hb = idx % NH_
        xkeys = [("X2T", t0 + a) for a in range(nt)]
        pend = []

        def flush_ht(jc):
            j_, c_ = jc
            p.v("scalar_tensor_tensor", HT[hb][:, j_, :ntok], tu[c_][:, :ntok], 1.0, ts_[c_][:, :ntok], ALU.add, ALU.mult,
                r=[("tu", c_), ("ts", c_)], w=[("HT", hb, j_)])

        for j in range(4):
            cidx = hh * 4 + j
            cg, kg = p.getps(); cu, ku = p.getps()
            for kc in range(8):
                p.mm(psap(k, cg, ntok), Wg[b][:, kc, j * 128:(j + 1) * 128], X2T[:, kc, t0 * 128:t0 * 128 + ntok],
                     start=(kc == 0), stop=(kc == 7), r=[("Wg", b)] + xkeys, w=kg)
            for kc in range(8):
                p.mm(psap(k, cu, ntok), Wu[b][:, kc, j * 128:(j + 1) * 128], X2T[:, kc, t0 * 128:t0 * 128 + ntok],
                     start=(kc == 0), stop=(kc == 7), r=[("Wu", b)] + xkeys, w=ku)
            c2 = cstate["ci"] % NC_; cstate["ci"] += 1
            p.v("tensor_scalar", tg[c2][:, :ntok], psap(k, cg, ntok), BGU[:, cidx, e:e + 1], 7.0, ALU.add, ALU.min,
                r=kg + ["BGU"], w=[("tg", c2)])
            p.act(tu[c2][:, :ntok], psap(k, cu, ntok), AF.Identity, bias=BGU[:, 8 + cidx, e:e + 1], scale=1.0,
                  r=ku + ["BGU"], w=[("tu", c2)])
            p.act(ts_[c2][:, :ntok], tg[c2][:, :ntok], AF.Silu, scale=1.702, r=[("tg", c2)], w=[("ts", c2)])
            p.v("scalar_tensor_tensor", tu[c2][:, :ntok], tu[c2][:, :ntok], 7.0, CM7[:, :ntok], ALU.min, ALU.max, r=[("tu", c2), "CM7"], w=[("tu", c2)])
            pend.append((j, c2))
            if len(pend) > 1:
                flush_ht(pend.pop(0))
        while pend:
            flush_ht(pend.pop(0))

    def emit_down(idx):
        g, t0, nt = items[idx]
        e, hh = granules[g]; b = g % NB
        hb = idx % NH_
        for a in range(nt):
            tt = t0 + a
            for half in range(2):
                cy, ky = p.getps()
                for j in range(4):
                    p.mm(psap(k, cy, 512), HT[hb][:, j, a * 128:(a + 1) * 128], Wd[b][:, j, half * 512:(half + 1) * 512],
                         start=(j == 0), stop=(j == 3), r=[("HT", hb, j), ("Wd", b)], w=ky)
                p.v("scalar_tensor_tensor", F[:, tt, half * 512:(half + 1) * 512], psap(k, cy, 512), COMB2[:, tt, e:e + 1],
                    F[:, tt, half * 512:(half + 1) * 512], ALU.mult, ALU.add, r=ky + [("COMB2", tt), ("F", tt)], w=[("F", tt)])

    load_granule(0); load_granule(1)
    for idx in range(len(items)):
        g, t0, nt = items[idx]
        emit_gu(idx)
        if idx >= 1:
            emit_down(idx - 1)
        if t0 == 0 and g >= 1 and g + 1 < len(granules):
            load_granule(g + 1)
    emit_down(len(items) - 1)
    p.release(m2)
    G2 = [p.sb([128, D], F32, "G2") for _ in range(2)]
    for s in range(2):
        load_gate(k, G2[s], s, 5, q="act", key=("G2", s))
    lng = p.sb([128, D], F32, "lng2"); lnb = p.sb([128, D], F32, "lnb2")
    p.dma(lng[:], k.inp["ln_g"][l, 1:2, :].broadcast_to([128, D]), w=["lng2"], q="act")
    p.dma(lnb[:], k.inp["ln_b"][l, 1:2, :].broadcast_to([128, D]), w=["lnb2"], q="act")
    hb3 = [p.sb([128, D], F32, "hb3") for _ in range(2)]
    ho = [p.sb([128, D], F32, "ho") for _ in range(2)]
    st = p.sb([128, 2, 6], F32, "st2"); mv = p.sb([128, 2], F32, "mv2"); rs = p.sb([128, 1], F32, "rs2")
    for tt in range(ntiles):
        i = tt % 2; s = 0 if tt < NLT else 1
        p.dma(hb3[i][:], k.Hd[tt * 128:(tt + 1) * 128, :], r=[("Hd", tt)], w=[("hb3", i)])
        p.v("tensor_tensor", F[:, tt, :], F[:, tt, :], G2[s][:], ALU.mult, r=[("F", tt), ("G2", s)], w=[("F", tt)])
        p.v("scalar_tensor_tensor", F[:, tt, :], hb3[i][:], ALPHA, F[:, tt, :], ALU.mult, ALU.add, r=[("hb3", i), ("F", tt)], w=[("F", tt)])
        ln_tile(k, F[:, tt, :], ("F", tt), ho[i], ("ho", i), lng, lnb, ["lng2", "lnb2"], (st, mv, rs))
        if last:
            p.dma(out_dram[tt * 128:(tt + 1) * 128, :], ho[i][:], r=[("ho", i)], w=[("OUT", tt)], q="act")
        else:
            p.dma(k.Hd[tt * 128:(tt + 1) * 128, :], ho[i][:], r=[("ho", i)], w=[("Hd", tt)], q="act")
    p.release(m)


def na_class(qt):
    return 0 if qt == 0 else 1 if qt == 1 else 2 if qt <= 13 else 3 if qt == 14 else 4


def stage_mixer_ab(k, l, need_ctx):
    p = k.p
    i = l // 2
    nq = NT if need_ctx else NLT
    if not hasattr(k, "QKTd"):
        k.QKTd_h = p.dram([1024, T], BF16, "QKTd"); k.QKTd = k.QKTd_h.ap()
        k.Vd = p.dram([T, 512], BF16, "Vd").ap()
        k.QKVBd = p.dram([1536, T], F32, "QKVBd").ap()
        k.Zd = p.dram([T, 512], F32, "Zd").ap()
        k.ABd = p.dram([T, 16], F32, "ABd").ap()
        k.VZ_h = p.dram([120, 64, 128], F32, "VZ"); k.VZ = k.VZ_h.ap()
    m = p.mark()
    AT = p.sb([128, 8, T], BF16, "AT")
    stage_AT(k, AT, 0, 1, NT)
    W = p.sb([128, 8, 3600], BF16, "Wab")
    w = k.inp["w_in_ab"][i].rearrange("(kc p) n -> p kc n", p=128)
    for c0 in range(0, 3600, 512):
        c1 = min(c0 + 512, 3600)
        p.dma(W[:, :, c0:c1], w[:, :, c0:c1], w=["Wab"], q="pool")
    sb16 = [p.sb([128, 512], BF16, "st16") for _ in range(2)]
    sf32 = [p.sb([128, 512], F32, "st32") for _ in range(2)]
    ui = 0
    for ch in list(range(8)) + list(range(12, 24)):
        isq = ch < 8
        for (t0, n) in TB5:
            ak = [("AT", t0 // 128 + a) for a in range(n // 128)]
            c0, keys = p.getps()
            for kc in range(8):
                p.mm(psap(k, c0, n), W[:, kc, ch * 128:(ch + 1) * 128], AT[:, kc, t0:t0 + n], start=(kc == 0), stop=(kc == 7), r=["Wab"] + ak, w=keys)
            u = ui % 2; ui += 1
            if isq:
                st, sk = sb16[u], ("st16", u)
                dst, dk = k.QKTd[ch * 128:(ch + 1) * 128, t0:t0 + n], ("QKTd", ch)
            else:
                st, sk = sf32[u], ("st32", u)
                dst, dk = k.QKVBd[(ch - 12) * 128:(ch - 11) * 128, t0:t0 + n], ("QKVBd", ch - 12)
            if ui % 2:
                p.act(st[:, :n], psap(k, c0, n), AF.Copy, r=keys, w=[sk])
            else:
                p.v("tensor_copy", st[:, :n], psap(k, c0, n), r=keys, w=[sk])
            p.dma(dst, st[:, :n], r=[sk], w=[dk], q="sp" if ui % 2 else "act")
    sab = [p.sb([128, 16], F32, "stab") for _ in range(2)]
    for tt in range(NT):
        u = tt % 2
        for (cs, n, st, sk, dst, dk) in ((1024, 512, sb16[u], ("st16", u), k.Vd[tt * 128:(tt + 1) * 128, :], ("Vd", tt)),
                                         (3072, 512, sf32[u], ("st32", u), k.Zd[tt * 128:(tt + 1) * 128, :], ("Zd", tt)),
                                         (3584, 16, sab[u], ("stab", u), k.ABd[tt * 128:(tt + 1) * 128, :], ("ABd", tt))):
            c0, keys = p.getps()
            for kc in range(8):
                p.mm(psap(k, c0, n), AT[:, kc, tt * 128:(tt + 1) * 128], W[:, kc, cs:cs + n], start=(kc == 0), stop=(kc == 7), r=["Wab", ("AT", tt)], w=keys)
            p.act(st[:, :n], psap(k, c0, n), AF.Copy, r=keys, w=[sk])
            p.dma(dst, st[:, :n], r=[sk], w=[dk], q="sp")
    p.release(m)
    if getattr(k, "do_na", True):
        stage_na(k, i, nq)
    if getattr(k, "do_dn", True):
        stage_deltanet(k, i, nq)


def stage_na(k, i, nq):
    p = k.p
    m = p.mark()
    QT4 = p.sb([128, 4, T], BF16, "QT4"); KT4 = p.sb([128, 4, T], BF16, "KT4"); V = p.sb([128, NT, 512], BF16, "V")
    for j in range(4):
        p.dma(QT4[:, j, :], k.QKTd[j * 128:(j + 1) * 128, :], r=[("QKTd", j)], w=[("QT4", j)])
        p.dma(KT4[:, j, :], k.QKTd[(4 + j) * 128:(5 + j) * 128, :], r=[("QKTd", 4 + j)], w=[("KT4", j)], q="act")
    p.dma(V[:], k.Vd.rearrange("(t p) c -> p t c", p=128), r=[("Vd", t) for t in range(NT)], w=["V"])
    CM = p.sb([128, 640], F32, "CM"); p.dma(CM[:], k.cin["na_cm"], w=["CM"])
    rp = p.sb([120, 128], F32, "rp")
    p.v("memset", rp[:], 0.0, w=["rp"])
    p.dma(rp[:, 48:79], k.inp["na_rpb"][i].rearrange("h a b -> (h a) b"), w=["rp"])
    for r0 in range(0, 64, 16):
        p.dma(k.VZ[:, r0:r0 + 16, :], rp[:].unsqueeze(1).broadcast_to([120, 16, 128]), r=["rp"], w=["VZ"])
    BI = [[p.sb([128, 896], F32, "BI") for _ in range(8)] for _ in range(2)]

    def build_bias(cls):
        s = cls % 2
        qt = {0: 0, 1: 1, 2: 2, 3: 14, 4: 15}[cls]
        nk = 5 if cls == 2 else 4
        kbase = int(np.clip(2 * qt - 4, 0, 24))
        for h in range(8):
            t = BI[s][h]; key = ("BI", s, h)
            p.v("memset", t[:, 0:nk * 128], NEG, w=[key])
            p.v("memset", t[:, nk * 128:nk * 128 + 256], 0.0, w=[key])
            for qrl in range(2):
                qr = 2 * qt + qrl
                k0 = int(np.clip(qr - 4, 0, 24))
                a_start = k0 - qr + 7
                cstart = (k0 - kbase) * 64
                src = bass.AP(tensor=k.VZ_h, offset=(h * 15 + a_start) * 8192 + 63, ap=[[127, 64], [8192, 8], [1, 64]])
                dst = t[qrl * 64:(qrl + 1) * 64, cstart:cstart + 512].rearrange("p (a b) -> p a b", b=64)
                p.dma(dst, src, r=["VZ"], w=[key], q="sp" if qrl == 0 else "act")
            p.v("tensor_tensor", t[:, 0:nk * 128], t[:, 0:nk * 128], CM[:, 0:nk * 128], ALU.add, r=[key, "CM"], w=[key])

    build_bias(0); build_bias(1)
    ab = attn_bufs(k)
    O = [p.sb([128, 512], F32, "Ona") for _ in range(2)]
    for qt in range(nq):
        oi = qt % 2
        if qt == 1:
            build_bias(2)
        if qt == 2:
            build_bias(3)
        if qt == 14:
            build_bias(4)
        for h in range(8):
            j = h // 2; hp = h % 2
            ps_ = slice(hp * 64, hp * 64 + 64)
            qT = QT4[ps_, j, qt * 128:(qt + 1) * 128]
            ctxseg = (KT4[ps_, j, NLAT:T], [("KT4", j)], [(V[:, 16 + a, h * 64:(h + 1) * 64], "V") for a in range(2)])
            if qt < NLT:
                cls = na_class(qt); nk = 5 if cls == 2 else 4
                kt0 = int(np.clip(2 * qt - 4, 0, 24)) // 2
                latseg = (KT4[ps_, j, kt0 * 128:(kt0 + nk) * 128], [("KT4", j)], [(V[:, t, h * 64:(h + 1) * 64], "V") for t in range(kt0, kt0 + nk)])
                segs = [latseg, ctxseg]
                bias, bk = BI[cls % 2][h][:, 0:nk * 128 + 256], [("BI", cls % 2, h)]
            else:
                segs = [ctxseg]; bias, bk = None, []
            attn_unit(k, ab, qT, [("QT4", j)], segs, bias, bk, None, [], O[oi][:, h * 64:(h + 1) * 64], ("Ona", oi))
        p.dma(k.MIXd[qt * 128:(qt + 1) * 128, 0:512], O[oi][:], r=[("Ona", oi)], w=[("MIXd", qt)], q="act")
    p.release(m)


def stage_deltanet(k, i, nq):
    p = k.p
    m = p.mark()
    masks = {}
    for nme in ("ut_incl", "ut_strict", "lt_incl", "lt_strict"):
        masks[nme] = p.sb([128, 128], F32, nme)
        p.dma(masks[nme][:], k.cin[nme], w=[nme])
        EPSC = p.sb([128, 1], F32, "EPSC"); p.v("memset", EPSC[:], RMS_EPS, w=["EPSC"])
    cwr = p.sb([5, 1536], F32, "cwr"); p.dma(cwr[:], k.inp["dn_conv"][i], w=["cwr"])
    CW = p.sb([128, 12, 5], F32, "CW")
    for rc in range(12):
        c0, keys = p.getps()
        p.tr(psap(k, c0, 5), cwr[:, rc * 128:(rc + 1) * 128], k.ident[0:5, 0:5], r=["cwr", "ident"], w=keys)
        p.v("tensor_copy", CW[:, rc, :], psap(k, c0, 5), r=keys, w=["CW"])
    AB = p.sb([128, NT, 16], F32, "AB")
    p.dma(AB[:], k.ABd.rearrange("(t p) c -> p t c", p=128), r=[("ABd", t) for t in range(NT)], w=["AB"])
    ALB = p.sb([128, 8], F32, "ALB"); DTB = p.sb([128, 8], F32, "DTB")
    p.dma(ALB[:], k.inp["dn_a_log"][i:i + 1].rearrange("o a b -> o (a b)").broadcast_to([128, 8]), w=["ALB"])
    p.dma(DTB[:], k.inp["dn_dt_bias"][i:i + 1].rearrange("o a b -> o (a b)").broadcast_to([128, 8]), w=["DTB"])
    p.act(ALB[:], ALB[:], AF.Exp, r=["ALB"], w=["ALB"])
    p.v("tensor_scalar_mul", ALB[:], ALB[:], -1.0, r=["ALB"], w=["ALB"])
    Gg = p.sb([128, NT, 8], F32, "Gg"); BETA = p.sb([128, NT, 8], F32, "BETA")
    tA = p.sb([128, NT, 8], F32, "tA"); tB = p.sb([128, NT, 8], F32, "tB")
    for t in range(NT):
        p.v("tensor_tensor", Gg[:, t, :], AB[:, t, 0:8], DTB[:], ALU.add, r=["AB", "DTB"], w=["Gg"])
    p.act(tA[:], Gg[:], AF.Abs, r=["Gg"], w=["tA"])
    p.act(tA[:], tA[:], AF.Exp, scale=-1.0, r=["tA"], w=["tA"])
    p.act(tA[:], tA[:], AF.Ln, bias=1.0, scale=1.0, r=["tA"], w=["tA"])
    p.v("tensor_scalar_max", tB[:], Gg[:], 0.0, r=["Gg"], w=["tB"])
    p.v("tensor_tensor", tA[:], tA[:], tB[:], ALU.add, r=["tA", "tB"], w=["tA"])
    for t in range(NT):
        p.v("tensor_tensor", Gg[:, t, :], tA[:, t, :], ALB[:], ALU.mult, r=["tA", "ALB", "Gg"], w=["Gg"])
    p.act(BETA[:], AB[:, :, 8:16], AF.Sigmoid, r=["AB"], w=["BETA"])
    OB = p.sb([128, NT, 4, 128], F32, "OB")
    written = set()
    if getattr(k, "dn_stop", "") == "gates":
        p.dma(k.out[1536:1664, 0:NT * 8], Gg[:].rearrange("p a b -> p (a b)"), r=["Gg"], w=["dbg"])
        p.dma(k.out[1664:1792, 0:NT * 8], BETA[:].rearrange("p a b -> p (a b)"), r=["BETA"], w=["dbg"])
        p.dma(k.out[0:128, 0:60], CW[:].rearrange("p a b -> p (a b)"), r=["CW"], w=["dbg"])
        p.release(m)
        return
    mH = p.mark()
    for hp2 in range(2):
        heads = [2 * hp2, 2 * hp2 + 1]
        QN = {}; KN = {}; VV = {}
        for h in heads:
            QN[h] = p.sb([128, T], F32, "QN"); KN[h] = p.sb([128, T], F32, "KN"); VV[h] = p.sb([128, T], F32, "VV")
        mP = p.mark()
        XP = [p.sb([128, 2312], F32, "XP") for _ in range(2)]
        SQ = p.sb([128, T], F32, "SQ"); RS = p.sb([128, T], F32, "RS")
        for u in range(2):
            p.v("memset", XP[u][:], 0.0, w=[("XP", u)])
        xi = 0
        for h in heads:
            for kind, dstd in (("q", QN), ("k", KN), ("v", VV)):
                rc = {"q": 0, "k": 4, "v": 8}[kind] + h
                u = xi % 2; xi += 1
                xp = XP[u]; xk = ("XP", u)
                Y = dstd[h]; yk = (kind, h)
                p.dma(xp[:, 2:2050], k.QKVBd[rc * 128:(rc + 1) * 128, 0:NLAT], r=[("QKVBd", rc)], w=[xk])
                p.dma(xp[:, 2054:2310], k.QKVBd[rc * 128:(rc + 1) * 128, NLAT:T], r=[("QKVBd", rc)], w=[xk], q="act")
                for (y0, x0, n) in ((0, 0, NLAT), (NLAT, 2052, LCTX)):
                    p.v("tensor_scalar_mul", Y[:, y0:y0 + n], xp[:, x0:x0 + n], CW[:, rc, 0:1], r=[xk, "CW"], w=[yk])
                    for kk in range(1, 5):
                        p.v("scalar_tensor_tensor", Y[:, y0:y0 + n], xp[:, x0 + kk:x0 + kk + n], CW[:, rc, kk:kk + 1], Y[:, y0:y0 + n],
                            ALU.mult, ALU.add, r=[xk, "CW", yk], w=[yk])
                p.act(Y[:], Y[:], AF.Silu, r=[yk], w=[yk])
                if kind != "v" and not getattr(k, "dn_nol2", False):
                    p.act(SQ[:], Y[:], AF.Square, r=[yk], w=["SQ"])
                    for (t0, n) in TB5:
                        c0, keys = p.getps()
                        p.mm(psap(k, c0, n), k.ones[:], SQ[:, t0:t0 + n], r=["ones", "SQ"], w=keys)
                        p.act(RS[:, t0:t0 + n], psap(k, c0, n), AF.Sqrt, bias=EPSC[:], scale=1.0, r=keys + ["EPSC"], w=["RS"])
                        p.v("reciprocal", RS[:, t0:t0 + n], RS[:, t0:t0 + n], r=["RS"], w=["RS"])
                    p.v("scalar_tensor_tensor", Y[:], Y[:], (128.0 ** -0.5) if kind == "q" else 1.0, RS[:], ALU.mult, ALU.mult, r=[yk, "RS"], w=[yk])
        p.release(mP)
        if getattr(k, "dn_stop", "") == "phase1":
            dbg = k.out
            for h in heads:
                for nm_, dd in (("q", QN), ("k", KN), ("v", VV)):
                    r0 = ({"q": 0, "k": 4, "v": 8}[nm_] + h) * 128
                    p.dma(dbg[r0:r0 + 128, :], dd[h][:, 0:D], r=[(nm_, h)], w=["dbg"])
            p.dma(dbg[1536:1664, 0:NT * 8], Gg[:].rearrange("p a b -> p (a b)"), r=["Gg"], w=["dbg"])
            p.dma(dbg[1664:1792, 0:NT * 8], BETA[:].rearrange("p a b -> p (a b)"), r=["BETA"], w=["dbg"])
            p.release(mH)
            continue
        R = 2
        pools = {}
        cnt = {}
        jobid = [0]

        def tb(kind, shape=(128, 128)):
            kind = (kind, jobid[0])
            if kind not in pools:
                pools[kind] = [p.sb(list(shape), F32, "p" + kind[0]) for _ in range(R)]
                cnt[kind] = 0
            ix = cnt[kind] % R; cnt[kind] += 1
            return pools[kind][ix], (kind, hp2, ix)

        jobs = []
        for h in heads:
            for d in range(2):
                S = p.sb([128, 128], F32, "S")
                sk = ("S", h, d)
                p.v("memset", S[:], 0.0, w=[sk])
                order = [16, 17] + list(range(16)) if d == 0 else [17, 16] + list(range(15, -1, -1))
                jobs.append((h, d, S, sk, order))

        def unit(h, d, S, sk, order, step):
            c = order[step]
            incl = masks["ut_incl" if d == 0 else "lt_incl"]; ikey = "ut_incl" if d == 0 else "lt_incl"
            strict = masks["ut_strict" if d == 0 else "lt_strict"]; skey_ = "ut_strict" if d == 0 else "lt_strict"
            last = 127 if d == 0 else 0
            cs = slice(c * 128, (c + 1) * 128)
            qT = QN[h][:, cs]; kT = KN[h][:, cs]; vT = VV[h][:, cs]
            qk = [("q", h)]; kk_ = [("k", h)]; vk = [("v", h)]
            gcol = Gg[:, c, d * 4 + h:d * 4 + h + 1]; bcol = BETA[:, c, d * 4 + h:d * 4 + h + 1]
            ktm, ktk = tb("ktm"); vtm, vtk = tb("vtm")
            c0, ks = p.getps(); p.tr(psap(k, c0, 128), kT, k.ident[:], r=kk_ + ["ident"], w=ks)
            p.act(ktm[:], psap(k, c0, 128), AF.Copy, r=ks, w=[ktk])
            c0, ks = p.getps(); p.tr(psap(k, c0, 128), vT, k.ident[:], r=vk + ["ident"], w=ks)
            p.act(vtm[:], psap(k, c0, 128), AF.Copy, r=ks, w=[vtk])
            yield
            sm, smk = tb("sm", (128, 8))
            c0, ks = p.getps(); p.mm(psap(k, c0, 8), incl[:], Gg[:, c, :], r=[ikey, "Gg"], w=ks)
            p.v("tensor_copy", sm[:, 0:1], psap(k, c0 + d * 4 + h, 1), r=ks, w=[smk])
            Gt, Gtk = tb("Gt"); p.act(Gt[:], incl[:], AF.Identity, scale=gcol, r=[ikey, "Gg"], w=[Gtk])
            Bd, Bdk = tb("Bd"); p.act(Bd[:], k.ident[:], AF.Identity, scale=bcol, r=["ident", "BETA"], w=[Bdk])
            cB, kB = p.getps(); p.mm(psap(k, cB, 128), k.ones[:], Bd[:], r=["ones", Bdk], w=kB)
            BR, BRk = tb("BR"); p.act(BR[:], psap(k, cB, 128), AF.Copy, r=kB, w=[BRk])
            cR, kR = p.getps(); p.mm(psap(k, cR, 128), k.ones[:], Gt[:], r=["ones", Gtk], w=kR)
            D1, D1k = tb("D1"); p.v("tensor_scalar", D1[:], psap(k, cR, 128), sm[:, 0:1], 0.0, ALU.subtract, ALU.min, r=kR + [smk], w=[D1k])
            egr, egrk = tb("egr"); p.act(egr[:], psap(k, cR, 128), AF.Exp, r=kR, w=[egrk])
            p.act(sm[:, 2:3], psap(k, cR + last, 1), AF.Exp, r=kR, w=[smk])
            p.v("tensor_scalar", sm[:, 3:4], psap(k, cR + last, 1), sm[:, 0:1], None, ALU.subtract, r=kR + [smk], w=[smk])
            p.act(sm[:, 3:4], sm[:, 3:4], AF.Exp, r=[smk], w=[smk])
            p.act(sm[:, 1:2], sm[:, 0:1], AF.Exp, r=[smk], w=[smk])
            E, Ek = tb("E"); p.act(E[:], D1[:], AF.Exp, r=[D1k], w=[Ek])
            DTm, DTmk = tb("DTm"); p.v("tensor_tensor", DTm[:], E[:], incl[:], ALU.mult, r=[Ek, ikey], w=[DTmk])
            DTs, DTsk = tb("DTs"); p.v("tensor_tensor", DTs[:], E[:], strict[:], ALU.mult, r=[Ek, skey_], w=[DTsk])
            yield
            cG, kG = p.getps(); p.mm(psap(k, cG, 128), kT, kT, r=kk_, w=kG)
            N1, N1k = tb("N1"); p.v("tensor_tensor", N1[:], psap(k, cG, 128), DTs[:], ALU.mult, r=kG + [DTsk], w=[N1k])
            cA, kA = p.getps(); p.mm(psap(k, cA, 128), kT, qT, r=kk_ + qk, w=kA)
            ATt, ATk = tb("ATt"); p.v("tensor_tensor", ATt[:], psap(k, cA, 128), DTm[:], ALU.mult, r=kA + [DTmk], w=[ATk])
            Nl, Nlk = tb("Na"); p.v("tensor_tensor", Nl[:], N1[:], BR[:], ALU.mult, r=[BRk, N1k], w=[Nlk])
            yield
            Ml, Mlk = tb("Ma")
            c0, ks = p.getps(); p.tr(psap(k, c0, 128), Nl[:], k.ident[:], r=[Nlk, "ident"], w=ks)
            p.act(Ml[:], psap(k, c0, 128), AF.Copy, r=ks, w=[Mlk])
            yield
            y, yk2 = tb("ya", (128, 256))
            p.act(y[:, 0:128], vtm[:], AF.Identity, scale=bcol, r=[vtk, "BETA"], w=[yk2])
            p.v("tensor_tensor", sm[:, 4:5], sm[:, 1:2], bcol, ALU.mult, r=[smk, "BETA"], w=[smk])
            p.act(y[:, 128:256], ktm[:], AF.Identity, scale=sm[:, 4:5], r=[ktk, smk], w=[yk2])
            for lev in range(7):
                yield
                c0, ks = p.getps()
                p.mm(psap(k, c0, 256), Nl[:], y[:], r=[Nlk, yk2], w=ks)
                y2, y2k = tb("yb" if lev % 2 == 0 else "ya", (128, 256))
                p.v("tensor_tensor", y2[:], y[:], psap(k, c0, 256), ALU.subtract if lev == 0 else ALU.add, r=ks + [yk2], w=[y2k])
                y, yk2 = y2, y2k
                if lev < 6:
                    N2, N2k = tb("Nb" if lev % 2 == 0 else "Na")
                    c0, ks = p.getps(); p.mm(psap(k, c0, 128), Ml[:], Nl[:], r=[Mlk, Nlk], w=ks)
                    p.act(N2[:], psap(k, c0, 128), AF.Copy, r=ks, w=[N2k])
                    if lev < 5:
                        M2, M2k = tb("Mb" if lev % 2 == 0 else "Ma")
                        c0, ks = p.getps(); p.mm(psap(k, c0, 128), Nl[:], Ml[:], r=[Mlk, Nlk], w=ks)
                        p.act(M2[:], psap(k, c0, 128), AF.Copy, r=ks, w=[M2k])
                        Ml, Mlk = M2, M2k
                    Nl, Nlk = N2, N2k
            yield
            wT, wTk = tb("wT")
            c0, ks = p.getps(); p.tr(psap(k, c0, 128), y[:, 128:256], k.ident[:], r=[yk2, "ident"], w=ks)
            p.act(wT[:], psap(k, c0, 128), AF.Copy, r=ks, w=[wTk])
            yield
            qg, qgk = tb("qg"); p.v("tensor_tensor", qg[:], qT, egr[:], ALU.mult, r=qk + [egrk], w=[qgk])
            kd, kdk = tb("kd"); p.act(kd[:], ktm[:], AF.Identity, scale=sm[:, 3:4], r=[ktk, smk], w=[kdk])
            yield
            c0, ks = p.getps(); p.mm(psap(k, c0, 128), wT[:], S[:], r=[wTk, sk], w=ks)
            vn, vnk = tb("vn"); p.v("tensor_tensor", vn[:], y[:, 0:128], psap(k, c0, 128), ALU.subtract, r=ks + [yk2], w=[vnk])
            cO, kO = p.getps()
            p.mm(psap(k, cO, 128), qg[:], S[:], start=True, stop=False, r=[qgk, sk], w=kO)
            p.mm(psap(k, cO, 128), ATt[:], vn[:], start=False, stop=True, r=[ATk, vnk], w=kO)
            cS, kS_ = p.getps(); p.mm(psap(k, cS, 128), kd[:], vn[:], r=[kdk, vnk], w=kS_)
            p.v("scalar_tensor_tensor", S[:], S[:], sm[:, 2:3], psap(k, cS, 128), ALU.mult, ALU.add, r=[sk, smk] + kS_, w=[sk])
            okey = ("OB", c, h)
            if (c, h) not in written:
                written.add((c, h))
                p.act(OB[:, c, h, :], psap(k, cO, 128), AF.Copy, r=kO, w=[okey])
            else:
                p.v("tensor_tensor", OB[:, c, h, :], OB[:, c, h, :], psap(k, cO, 128), ALU.add, r=kO + [okey], w=[okey])

        for step in range(getattr(k, "dn_steps", NT)):
            gens = []
            for ji, job in enumerate(jobs):
                gens.append((ji, unit(*job, step)))
            while gens:
                for (ji, g) in list(gens):
                    jobid[0] = ji
                    try:
                        next(g)
                    except StopIteration:
                        gens.remove((ji, g))
        p.release(mH)
    if getattr(k, "dn_stop", "") != "":
        p.release(m)
        return
    NW = p.sb([128, 4, 128], F32, "NW")
    for h in range(4):
        p.dma(NW[:, h, :], k.inp["dn_norm_w"][i:i + 1, :].broadcast_to([128, 128]), w=["NW"])
    zt = [p.sb([128, 512], F32, "zt") for _ in range(2)]
    on = [p.sb([128, 4, 128], F32, "on") for _ in range(2)]
    junk = p.sb([128, 128], F32, "junk")
    ss = [p.sb([128, 4], F32, "ss4") for _ in range(2)]
    for tt in range(nq):
        u = tt % 2
        p.dma(zt[u][:], k.Zd[tt * 128:(tt + 1) * 128, :], r=[("Zd", tt)], w=[("zt", u)])
        p.act(zt[u][:], zt[u][:], AF.Silu, r=[("zt", u)], w=[("zt", u)])
        for h in range(4):
            p.act(junk[:], OB[:, tt, h, :], AF.Square, accum_out=ss[u][:, h:h + 1], r=[("OB", tt, h)], w=["junk", ("ss4", u)])
        p.v("tensor_scalar", ss[u][:], ss[u][:], 1.0 / 128, RMS_EPS, ALU.mult, ALU.add, r=[("ss4", u)], w=[("ss4", u)])
        p.act(ss[u][:], ss[u][:], AF.Sqrt, r=[("ss4", u)], w=[("ss4", u)])
        p.v("reciprocal", ss[u][:], ss[u][:], r=[("ss4", u)], w=[("ss4", u)])
        for h in range(4):
            p.v("tensor_scalar_mul", on[u][:, h, :], OB[:, tt, h, :], ss[u][:, h:h + 1], r=[("OB", tt, h), ("ss4", u)], w=[("on", u)])
        p.v("tensor_tensor", on[u][:], on[u][:], NW[:], ALU.mult, r=[("on", u), "NW"], w=[("on", u)])
        p.v("tensor_tensor", on[u][:].rearrange("p a b -> p (a b)"), on[u][:].rearrange("p a b -> p (a b)"), zt[u][:], ALU.mult,
            r=[("on", u), ("zt", u)], w=[("on", u)])
        p.dma(k.MIXd[tt * 128:(tt + 1) * 128, 512:1024], on[u][:].rearrange("p a b -> p (a b)"), r=[("on", u)], w=[("MIXd", tt)], q="act")
    p.release(m)


from concourse.bass_utils import run_bass_kernel_spmd

N_CORES = 8


def build_program(nc, layers=(0, 1, 2, 3)):
    k = setup(nc)
    p = k.p
    stage_init(k)
    for l in layers:
        last = l == 3
        stage_mod(k, l)
        if l % 2 == 0:
            stage_mixer_ab(k, l, not last)
        else:
            stage_mixer_c(k, l, not last)
        nt = NLT if last else NT
        m = p.mark()
        X2T = p.sb([128, 8, T], BF16, "X2T"); COMB = p.sb([128, NT, 32], F32, "COMB")
        stage_outproj(k, l, k.inp["w_out_c" if l % 2 else "w_out_ab"][l // 2], nt, X2T, COMB)
        stage_moe(k, l, nt, X2T, COMB, k.out, last)
        p.release(m)
    p.finish()
    p.emit()
    return k


_CACHE = {}


def kernel(**inputs):
    consts = host_consts()
    nc = bass.Bass("TRN2", target_bir_lowering=False)
    build_program(nc)
    shared = {}
    for n in IN_SHAPES:
        if n in ("x", "ctx", "c", "c_ctx"):
            continue
        shared[n] = np.ascontiguousarray(np.asarray(inputs[n], dtype=np.float32))
    for n, v in consts.items():
        shared["k_" + n] = np.ascontiguousarray(v)
    x = np.asarray(inputs["x"], dtype=np.float32); ctx = np.asarray(inputs["ctx"], dtype=np.float32)
    c = np.asarray(inputs["c"], dtype=np.float32); cc = np.asarray(inputs["c_ctx"], dtype=np.float32)
    in_maps = []
    for b in range(N_CORES):
        m = dict(shared)
        m["x"] = np.ascontiguousarray(x[b]); m["ctx"] = np.ascontiguousarray(ctx[b])
        m["c"] = np.ascontiguousarray(c[b:b + 1]); m["c_ctx"] = np.ascontiguousarray(cc[None, :])
        in_maps.append(m)
    res = run_bass_kernel_spmd(nc, in_maps, core_ids=list(range(N_CORES)))
    return np.stack([np.asarray(r["out"], dtype=np.float32) for r in res.results], 0)
```
